# Optimizing a Trainium2 kernel written in Bass

```python
import math
import jax
import jax.numpy as jnp
from jax import lax
import numpy as np

D_MODEL = 2048
BATCH = 2
SEQ = 16384
DEPTH = 2

PLE_DIM = 256
N_BRANCH = 4
BRANCH_WIDTH = 1024

SSD_HEADS = 16
SSD_HEAD_DIM = 64
SSD_INNER = SSD_HEADS * SSD_HEAD_DIM
SSD_GROUPS = 2
SSD_STATE = 128
SSD_CONV = 4
SSD_CONV_DIM = SSD_INNER + 2 * SSD_GROUPS * SSD_STATE
SSD_CHUNK = 128

S5_WIDTH = 1024
S5_GROUP = 16
S5_GROUPS = S5_WIDTH // S5_GROUP
S5_STATE = 64
S5_DT_MIN = 1e-3
S5_DT_MAX = 1e-1

RET_HEADS = 8
RET_QK_DIM = 64
RET_V_DIM = 128
RET_QK = RET_HEADS * RET_QK_DIM
RET_WIDTH = RET_HEADS * RET_V_DIM
RET_CHUNK = 128
ROPE_BASE = 10000.0

GLA_HEADS = 4
GLA_QK_DIM = 128
GLA_V_DIM = 256
GLA_QK = GLA_HEADS * GLA_QK_DIM
GLA_WIDTH = GLA_HEADS * GLA_V_DIM
GLA_GATE_RANK = 16
GLA_TAU = 16.0
GLA_CHUNK = 64

N_EXPERTS = 32
TOP_K = 4
D_EXPERT = 1024
SWIGLU_ALPHA = 1.702
SWIGLU_LIMIT = 7.0
MOE_BLOCK = 256

DEEPNORM_ALPHA = (2 * DEPTH) ** 0.25
DEEPNORM_BETA = (8 * DEPTH) ** -0.25
LN_EPS = 1e-5
RMS_EPS = 1e-6

IN_SPLITS = (SSD_INNER, SSD_CONV_DIM, SSD_HEADS,
             S5_WIDTH,
             RET_QK, RET_QK, RET_WIDTH, RET_WIDTH,
             GLA_QK, GLA_QK, GLA_WIDTH, GLA_WIDTH,
             GLA_GATE_RANK,
             N_BRANCH * D_MODEL)
N_IN = sum(IN_SPLITS)

kernel_name = 'hybrid_ssd_s5_retention_gla_moe_trunk'


def layer_norm(x, w, b):
    xf = x.astype(jnp.float32)
    mu = jnp.mean(xf, -1, keepdims=True)
    var = jnp.mean(jnp.square(xf - mu), -1, keepdims=True)
    return ((xf - mu) * lax.rsqrt(var + LN_EPS) * w + b).astype(x.dtype)


def rms_norm(x, w):
    xf = x.astype(jnp.float32)
    return xf * lax.rsqrt(jnp.mean(jnp.square(xf), -1, keepdims=True) + RMS_EPS) * w


def chunk_recurrence(chunk_inputs, chunk_decay):
    inputs = jnp.moveaxis(chunk_inputs, 1, 0)
    decays = jnp.moveaxis(chunk_decay, 1, 0)

    def step(h, inp):
        s, d = inp
        return h * d + s, h

    _, h_prev = lax.scan(step, jnp.zeros_like(inputs[0]), (inputs, decays))
    return jnp.moveaxis(h_prev, 0, 1)


def causal_depthwise_conv(x, w, b):
    k, c = w.shape
    y = lax.conv_general_dilated(x, w[:, None, :], (1,), [(k - 1, 0)],
                                 dimension_numbers=('NWC', 'WIO', 'NWC'), feature_group_count=c)
    return y + b


def ssd_mixer(z, xbc, dt_raw, conv_w, conv_b, dt_bias, a_log, d_skip, norm_w):
    f32 = jnp.float32
    bsz, seq, _ = z.shape
    nc, L = seq // SSD_CHUNK, SSD_CHUNK
    R = SSD_HEADS // SSD_GROUPS
    xbc = jax.nn.silu(causal_depthwise_conv(xbc.astype(f32), conv_w.astype(f32), conv_b.astype(f32)))
    xs, bm, cm = jnp.split(xbc, [SSD_INNER, SSD_INNER + SSD_GROUPS * SSD_STATE], axis=-1)
    dt = jax.nn.softplus(dt_raw.astype(f32) + dt_bias.astype(f32))
    log_a = dt * (-jnp.exp(a_log.astype(f32)))
    xdt = xs.reshape(bsz, nc, L, SSD_GROUPS, R, SSD_HEAD_DIM) * dt.reshape(bsz, nc, L, SSD_GROUPS, R, 1)
    bm = bm.reshape(bsz, nc, L, SSD_GROUPS, SSD_STATE)
    cm = cm.reshape(bsz, nc, L, SSD_GROUPS, SSD_STATE)
    a_cs = jnp.cumsum(log_a.reshape(bsz, nc, L, SSD_GROUPS, R), axis=2)
    causal = jnp.tril(jnp.ones((L, L), bool))
    seg = a_cs[:, :, :, None] - a_cs[:, :, None, :]
    decay = jnp.exp(jnp.where(causal[None, None, :, :, None, None], seg, -jnp.inf))
    cb = jnp.einsum('bclgn,bcsgn->bclsg', cm, bm)
    y_diag = jnp.einsum('bclsg,bclsgr,bcsgrp->bclgrp', cb, decay, xdt)
    to_end = jnp.exp(a_cs[:, :, -1:] - a_cs)
    states = jnp.einsum('bclgn,bclgr,bclgrp->bcgrpn', bm, to_end, xdt)
    h_in = chunk_recurrence(states, jnp.exp(a_cs[:, :, -1])[..., None, None])
    y_off = jnp.einsum('bclgn,bcgrpn,bclgr->bclgrp', cm, h_in, jnp.exp(a_cs))
    y = (y_diag + y_off).reshape(bsz, seq, SSD_HEADS, SSD_HEAD_DIM)
    y = y + d_skip.astype(f32)[:, None] * xs.reshape(bsz, seq, SSD_HEADS, SSD_HEAD_DIM)
    group = SSD_INNER // SSD_GROUPS
    y = y.reshape(bsz, seq, SSD_GROUPS, group) * jax.nn.silu(z.astype(f32)).reshape(bsz, seq, SSD_GROUPS, group)
    return rms_norm(y, norm_w.astype(f32).reshape(SSD_GROUPS, group)).reshape(bsz, seq, SSD_INNER)


def complex_affine_combine(earlier, later):
    a1r, a1i, b1r, b1i = earlier
    a2r, a2i, b2r, b2i = later
    return (a2r * a1r - a2i * a1i, a2r * a1i + a2i * a1r,
            a2r * b1r - a2i * b1i + b2r, a2r * b1i + a2i * b1r + b2i)


def s5_mixer(u, lam_re, lam_im, log_dt, b_re, b_im, c_re, c_im, d_skip, w_glu, b_glu):
    f32 = jnp.float32
    bsz, seq, _ = u.shape
    uf = u.astype(f32)
    ug = uf.reshape(bsz, seq, S5_GROUPS, S5_GROUP)
    lr = jnp.minimum(lam_re.astype(f32), -1e-4)[None, :]
    li = lam_im.astype(f32)[None, :]
    dt = jnp.exp(log_dt.astype(f32))[:, None]
    mag = jnp.exp(lr * dt)
    ab_re, ab_im = mag * jnp.cos(li * dt), mag * jnp.sin(li * dt)
    den = lr * lr + li * li
    nr, ni = ab_re - 1.0, ab_im
    f_re, f_im = (nr * lr + ni * li) / den, (ni * lr - nr * li) / den
    b_re, b_im = b_re.astype(f32), b_im.astype(f32)
    bb_re = f_re[..., None] * b_re - f_im[..., None] * b_im
    bb_im = f_re[..., None] * b_im + f_im[..., None] * b_re
    bu_re = jnp.einsum('bsgc,gpc->sbgp', ug, bb_re)
    bu_im = jnp.einsum('bsgc,gpc->sbgp', ug, bb_im)
    a_re = jnp.broadcast_to(ab_re[None, None], (seq, 1, S5_GROUPS, S5_STATE))
    a_im = jnp.broadcast_to(ab_im[None, None], (seq, 1, S5_GROUPS, S5_STATE))
    _, _, h_re, h_im = lax.associative_scan(complex_affine_combine, (a_re, a_im, bu_re, bu_im), axis=0)
    y = (jnp.einsum('sbgp,gcp->bsgc', h_re, c_re.astype(f32))
         - jnp.einsum('sbgp,gcp->bsgc', h_im, c_im.astype(f32)))
    y = jax.nn.gelu(y.reshape(bsz, seq, S5_WIDTH) + d_skip.astype(f32) * uf)
    return y * jax.nn.sigmoid(y @ w_glu.astype(f32) + b_glu.astype(f32))


def apply_rotary(x, positions):
    half = x.shape[-1] // 2
    inv_freq = 1.0 / (ROPE_BASE ** (jnp.arange(half, dtype=jnp.float32) / half))
    ang = positions.astype(jnp.float32)[..., None] * inv_freq
    cos, sin = jnp.cos(ang)[:, :, None, :], jnp.sin(ang)[:, :, None, :]
    x1, x2 = x[..., :half], x[..., half:]
    return jnp.concatenate([x1 * cos - x2 * sin, x2 * cos + x1 * sin], -1)


def retention_mixer(q, k, v, g, positions, norm_w):
    f32 = jnp.float32
    bsz, seq, _ = q.shape
    nc, L = seq // RET_CHUNK, RET_CHUNK
    q = apply_rotary(q.astype(f32).reshape(bsz, seq, RET_HEADS, RET_QK_DIM), positions)
    k = apply_rotary(k.astype(f32).reshape(bsz, seq, RET_HEADS, RET_QK_DIM), positions) * RET_QK_DIM ** -0.5
    q = q.reshape(bsz, nc, L, RET_HEADS, RET_QK_DIM)
    k = k.reshape(bsz, nc, L, RET_HEADS, RET_QK_DIM)
    v = v.astype(f32).reshape(bsz, nc, L, RET_HEADS, RET_V_DIM)
    log_gamma = jnp.log1p(-jnp.exp2(-5.0 - jnp.arange(RET_HEADS, dtype=f32)))
    pos = jnp.arange(L, dtype=f32)
    rel = (pos[:, None] - pos[None, :])[..., None]
    intra = jnp.exp(jnp.where(rel >= 0, rel * log_gamma, -jnp.inf))
    scores = jnp.einsum('bclhd,bcshd->bclsh', q, k) * intra
    y = jnp.einsum('bclsh,bcshe->bclhe', scores, v)
    kv = jnp.einsum('bclhd,lh,bclhe->bchde', k, jnp.exp((L - 1 - pos)[:, None] * log_gamma), v)
    chunk_decay = jnp.broadcast_to(jnp.exp(L * log_gamma)[:, None, None], (1, nc, RET_HEADS, 1, 1))
    r_in = chunk_recurrence(kv, chunk_decay)
    y = y + jnp.einsum('bclhd,bchde,lh->bclhe', q, r_in, jnp.exp((pos + 1)[:, None] * log_gamma))
    y = y.reshape(bsz, seq, RET_HEADS, RET_V_DIM)
    mu = jnp.mean(y, -1, keepdims=True)
    var = jnp.mean(jnp.square(y - mu), -1, keepdims=True)
    y = ((y - mu) * lax.rsqrt(var + LN_EPS)).reshape(bsz, seq, RET_WIDTH) * norm_w.astype(f32)
    return jax.nn.silu(g.astype(f32)) * y


def gla_mixer(q, k, v, r, gate_code, w_alpha, b_alpha, norm_w):
    f32 = jnp.float32
    bsz, seq, _ = q.shape
    nc, L = seq // GLA_CHUNK, GLA_CHUNK
    log_alpha = jax.nn.log_sigmoid(gate_code.astype(f32) @ w_alpha.astype(f32) + b_alpha.astype(f32)) / GLA_TAU
    shp = (bsz, nc, L, GLA_HEADS, GLA_QK_DIM)
    q = q.astype(f32).reshape(shp) * GLA_QK_DIM ** -0.5
    k = k.astype(f32).reshape(shp)
    v = v.astype(f32).reshape(bsz, nc, L, GLA_HEADS, GLA_V_DIM)
    b = jnp.cumsum(log_alpha.reshape(shp), axis=2)
    q_dec = q * jnp.exp(b)
    k_inv = k * jnp.exp(-b)
    causal = jnp.tril(jnp.ones((L, L), bool))[..., None]
    attn = jnp.where(causal, jnp.einsum('bclhd,bcshd->bclsh', q_dec, k_inv), 0.0)
    y = jnp.einsum('bclsh,bcshe->bclhe', attn, v)
    kv = jnp.einsum('bclhd,bclhe->bchde', k * jnp.exp(b[:, :, -1:] - b), v)
    s_in = chunk_recurrence(kv, jnp.exp(b[:, :, -1])[..., None])
    y = y + jnp.einsum('bclhd,bchde->bclhe', q_dec, s_in)
    y = rms_norm(y.reshape(bsz, seq, GLA_HEADS, GLA_V_DIM), norm_w.astype(f32).reshape(GLA_HEADS, GLA_V_DIM))
    return y.reshape(bsz, seq, GLA_WIDTH) * jax.nn.silu(r.astype(f32))


def hybrid_token_mixer(h, positions, w_in, ssd_conv_w, ssd_conv_b, ssd_dt_bias, ssd_a_log, ssd_d, ssd_norm_w,
                       s5_lambda_re, s5_lambda_im, s5_log_dt, s5_b_re, s5_b_im, s5_c_re, s5_c_im, s5_d,
                       s5_w_glu, s5_b_glu, ret_norm_w, gla_w_alpha, gla_b_alpha, gla_norm_w, w_branch, w_out):
    bsz, seq, _ = h.shape
    proj = h @ w_in
    (z, xbc, dt_raw, u, rq, rk, rv, rg, gq, gk, gv, gr, g_code, gate_logits) = jnp.split(
        proj, np.cumsum(IN_SPLITS)[:-1].tolist(), axis=-1)
    y_a = ssd_mixer(z, xbc, dt_raw, ssd_conv_w, ssd_conv_b, ssd_dt_bias, ssd_a_log, ssd_d, ssd_norm_w)
    y_b = s5_mixer(u, s5_lambda_re, s5_lambda_im, s5_log_dt, s5_b_re, s5_b_im, s5_c_re, s5_c_im, s5_d,
                   s5_w_glu, s5_b_glu)
    y_c = retention_mixer(rq, rk, rv, rg, positions, ret_norm_w)
    y_d = gla_mixer(gq, gk, gv, gr, g_code, gla_w_alpha, gla_b_alpha, gla_norm_w)
    ys = jnp.stack([y_a, y_b, y_c, y_d], axis=2)
    branches = jnp.einsum('bsnc,ncd->bsnd', ys, w_branch.astype(jnp.float32))
    gates = jax.nn.sigmoid(gate_logits.astype(jnp.float32).reshape(bsz, seq, N_BRANCH, D_MODEL))
    merged = jnp.sum(gates * branches, axis=2)
    return (merged @ w_out.astype(jnp.float32)).astype(h.dtype)


def moe_ffn(h, router_w, router_b, w_gate_up, b_gate_up, w_down, b_down):
    f32 = jnp.float32
    bsz, seq, d = h.shape
    n_tok = bsz * seq
    xf = h.reshape(n_tok, d)
    logits = (xf @ router_w + router_b).astype(f32)
    top_logits, top_idx = lax.top_k(logits, TOP_K)
    gate = jax.nn.softmax(top_logits, axis=-1)
    n_assign = n_tok * TOP_K
    e_flat = top_idx.reshape(n_assign)
    order = jnp.argsort(e_flat)
    e_sorted = e_flat[order]
    tok_sorted = (order // TOP_K).astype(jnp.int32)
    counts = jnp.bincount(e_flat, length=N_EXPERTS)
    padded = (counts + MOE_BLOCK - 1) // MOE_BLOCK * MOE_BLOCK
    pad_end = jnp.cumsum(padded)
    pad_start = pad_end - padded
    grp_start = jnp.cumsum(counts) - counts
    dest = pad_start[e_sorted] + jnp.arange(n_assign) - grp_start[e_sorted]
    n_blocks = -(-n_assign // MOE_BLOCK) + N_EXPERTS
    n_rows = n_blocks * MOE_BLOCK
    row_token = jnp.zeros((n_rows,), jnp.int32).at[dest].set(tok_sorted)
    xb = xf[row_token].reshape(n_blocks, MOE_BLOCK, d)
    block_expert = jnp.minimum(jnp.searchsorted(pad_end // MOE_BLOCK, jnp.arange(n_blocks), side='right'),
                               N_EXPERTS - 1)

    def expert_block(args):
        xblk, e = args
        hgu = xblk @ w_gate_up[e] + b_gate_up[e]
        g, u = hgu[:, :D_EXPERT], hgu[:, D_EXPERT:]
        g = jnp.minimum(g, SWIGLU_LIMIT)
        u = jnp.clip(u, -SWIGLU_LIMIT, SWIGLU_LIMIT)
        act = (u + 1.0) * (g * jax.nn.sigmoid(SWIGLU_ALPHA * g))
        return act @ w_down[e] + b_down[e]

    yb = lax.map(expert_block, (xb, block_expert))
    y_rows = yb.reshape(n_rows, d)[dest].astype(f32)
    w_sorted = gate.reshape(n_assign)[order]
    out = jnp.zeros((n_tok, d), f32).at[tok_sorted].add(y_rows * w_sorted[:, None])
    return out.reshape(bsz, seq, d).astype(h.dtype)


def per_layer_embedding(h, p_i, w_gate, w_proj):
    return (jax.nn.sigmoid(h @ w_gate) * (p_i @ w_proj)).astype(h.dtype)


def setup_inputs(seed: int = 0) -> dict:
    key = jax.random.key(seed)
    ks = iter(jax.random.split(key, 64))
    f32 = jnp.float32
    L = DEPTH

    def nrm(shape, scale):
        return jax.random.normal(next(ks), shape, f32) * scale

    def unif(shape, lo, hi):
        return jax.random.uniform(next(ks), shape, f32, lo, hi)

    x = nrm((BATCH, SEQ, D_MODEL), 1.0)
    p = nrm((DEPTH, BATCH, SEQ, PLE_DIM), 1.0)
    offsets = jax.random.randint(next(ks), (BATCH, 1), 0, 4096, jnp.int32)
    positions = offsets + jnp.arange(SEQ, dtype=jnp.int32)[None, :]
    w_in = nrm((L, D_MODEL, N_IN), D_MODEL ** -0.5)
    ssd_conv_w = nrm((L, SSD_CONV, SSD_CONV_DIM), SSD_CONV ** -0.5)
    ssd_conv_b = nrm((L, SSD_CONV_DIM), 0.02)
    dt0 = jnp.exp(unif((L, SSD_HEADS), math.log(1e-3), math.log(1e-1)))
    ssd_dt_bias = dt0 + jnp.log(-jnp.expm1(-dt0))
    ssd_a_log = jnp.log(unif((L, SSD_HEADS), 1.0, 16.0))
    ssd_d = 1.0 + nrm((L, SSD_HEADS), 0.1)
    ssd_norm_w = 1.0 + nrm((L, SSD_INNER), 0.02)
    n = jnp.arange(S5_STATE, dtype=f32)
    s5_lambda_re = -0.5 + nrm((L, S5_STATE), 0.01)
    s5_lambda_im = math.pi * n + nrm((L, S5_STATE), 0.01)
    s5_log_dt = unif((L, S5_GROUPS), math.log(S5_DT_MIN), math.log(S5_DT_MAX))
    s5_b_re = nrm((L, S5_GROUPS, S5_STATE, S5_GROUP), (2 * S5_GROUP) ** -0.5)
    s5_b_im = nrm((L, S5_GROUPS, S5_STATE, S5_GROUP), (2 * S5_GROUP) ** -0.5)
    s5_c_re = nrm((L, S5_GROUPS, S5_GROUP, S5_STATE), (2 * S5_STATE) ** -0.5)
    s5_c_im = nrm((L, S5_GROUPS, S5_GROUP, S5_STATE), (2 * S5_STATE) ** -0.5)
    s5_d = nrm((L, S5_WIDTH), 1.0)
    s5_w_glu = nrm((L, S5_WIDTH, S5_WIDTH), S5_WIDTH ** -0.5)
    s5_b_glu = nrm((L, S5_WIDTH), 0.02)
    ret_norm_w = 1.0 + nrm((L, RET_WIDTH), 0.02)
    gla_w_alpha = nrm((L, GLA_GATE_RANK, GLA_QK), GLA_GATE_RANK ** -0.5)
    gla_b_alpha = nrm((L, GLA_QK), 0.5)
    gla_norm_w = 1.0 + nrm((L, GLA_WIDTH), 0.02)
    w_branch = nrm((L, N_BRANCH, BRANCH_WIDTH, D_MODEL), BRANCH_WIDTH ** -0.5)
    w_out = nrm((L, D_MODEL, D_MODEL), DEEPNORM_BETA * D_MODEL ** -0.5)
    ln1_w = 1.0 + nrm((L, D_MODEL), 0.02)
    ln1_b = nrm((L, D_MODEL), 0.02)
    router_w = nrm((L, D_MODEL, N_EXPERTS), D_MODEL ** -0.5)
    router_b = nrm((L, N_EXPERTS), 0.01)
    moe_w_gate_up = nrm((L, N_EXPERTS, D_MODEL, 2 * D_EXPERT), D_MODEL ** -0.5)
    moe_b_gate_up = nrm((L, N_EXPERTS, 2 * D_EXPERT), 0.02)
    moe_w_down = nrm((L, N_EXPERTS, D_EXPERT, D_MODEL), DEEPNORM_BETA * D_EXPERT ** -0.5)
    moe_b_down = nrm((L, N_EXPERTS, D_MODEL), 0.02)
    ln2_w = 1.0 + nrm((L, D_MODEL), 0.02)
    ln2_b = nrm((L, D_MODEL), 0.02)
    ple_w_gate = nrm((L, D_MODEL, D_MODEL), D_MODEL ** -0.5)
    ple_w_proj = nrm((L, PLE_DIM, D_MODEL), DEEPNORM_BETA * PLE_DIM ** -0.5)
    ln3_w = 1.0 + nrm((L, D_MODEL), 0.02)
    ln3_b = nrm((L, D_MODEL), 0.02)
    return {
        'x': x, 'p': p, 'positions': positions, 'w_in': w_in,
        'ssd_conv_w': ssd_conv_w, 'ssd_conv_b': ssd_conv_b, 'ssd_dt_bias': ssd_dt_bias,
        'ssd_a_log': ssd_a_log, 'ssd_d': ssd_d, 'ssd_norm_w': ssd_norm_w,
        's5_lambda_re': s5_lambda_re, 's5_lambda_im': s5_lambda_im, 's5_log_dt': s5_log_dt,
        's5_b_re': s5_b_re, 's5_b_im': s5_b_im, 's5_c_re': s5_c_re, 's5_c_im': s5_c_im,
        's5_d': s5_d, 's5_w_glu': s5_w_glu, 's5_b_glu': s5_b_glu,
        'ret_norm_w': ret_norm_w,
        'gla_w_alpha': gla_w_alpha, 'gla_b_alpha': gla_b_alpha, 'gla_norm_w': gla_norm_w,
        'w_branch': w_branch, 'w_out': w_out, 'ln1_w': ln1_w, 'ln1_b': ln1_b,
        'router_w': router_w, 'router_b': router_b, 'moe_w_gate_up': moe_w_gate_up,
        'moe_b_gate_up': moe_b_gate_up, 'moe_w_down': moe_w_down, 'moe_b_down': moe_b_down,
        'ln2_w': ln2_w, 'ln2_b': ln2_b,
        'ple_w_gate': ple_w_gate, 'ple_w_proj': ple_w_proj, 'ln3_w': ln3_w, 'ln3_b': ln3_b,
    }


def reference(x, p, positions, w_in, ssd_conv_w, ssd_conv_b, ssd_dt_bias, ssd_a_log, ssd_d, ssd_norm_w,
              s5_lambda_re, s5_lambda_im, s5_log_dt, s5_b_re, s5_b_im, s5_c_re, s5_c_im, s5_d, s5_w_glu,
              s5_b_glu, ret_norm_w, gla_w_alpha, gla_b_alpha, gla_norm_w, w_branch, w_out, ln1_w, ln1_b,
              router_w, router_b, moe_w_gate_up, moe_b_gate_up, moe_w_down, moe_b_down, ln2_w, ln2_b,
              ple_w_gate, ple_w_proj, ln3_w, ln3_b):
    h = x
    for i in range(DEPTH):
        mix = hybrid_token_mixer(h, positions, w_in[i], ssd_conv_w[i], ssd_conv_b[i], ssd_dt_bias[i],
                                 ssd_a_log[i], ssd_d[i], ssd_norm_w[i], s5_lambda_re[i], s5_lambda_im[i],
                                 s5_log_dt[i], s5_b_re[i], s5_b_im[i], s5_c_re[i], s5_c_im[i], s5_d[i],
                                 s5_w_glu[i], s5_b_glu[i], ret_norm_w[i], gla_w_alpha[i], gla_b_alpha[i],
                                 gla_norm_w[i], w_branch[i], w_out[i])
        h = layer_norm(DEEPNORM_ALPHA * h + mix, ln1_w[i], ln1_b[i])
        ffn = moe_ffn(h, router_w[i], router_b[i], moe_w_gate_up[i], moe_b_gate_up[i], moe_w_down[i], moe_b_down[i])
        h = layer_norm(DEEPNORM_ALPHA * h + ffn, ln2_w[i], ln2_b[i])
        ple = per_layer_embedding(h, p[i], ple_w_gate[i], ple_w_proj[i])
        h = layer_norm(DEEPNORM_ALPHA * h + ple, ln3_w[i], ln3_b[i])
    return h
```

```python
import numpy as np
import concourse.bass as bass
import concourse.mybir as mybir

F32 = mybir.dt.float32
F32R = mybir.dt.float32r
BF16 = mybir.dt.bfloat16
I32 = mybir.dt.int32
AF = mybir.ActivationFunctionType
ALU = mybir.AluOpType
AX = mybir.AxisListType

SEM_ROLL = 12000


class Ent:
    __slots__ = ("name", "lastw", "reads", "dsem", "dcount")

    def __init__(self, name):
        self.name = name
        self.lastw = None
        self.reads = {}
        self.dsem = None
        self.dcount = 0


class Sched:
    def __init__(self, nc, stack):
        self.nc = nc
        self.stack = stack
        self.eng = {"pe": nc.tensor, "dve": nc.vector, "act": nc.scalar, "pool": nc.gpsimd, "sp": nc.sync}
        self.sems = {}
        self.cur = {}
        self.epoch = {e: 0 for e in self.eng}
        self.seen = {e: {} for e in self.eng}
        self.nsem = 0
        self.ninst = 0
        for e in self.eng:
            self._newsem(e)

    def _alloc(self, name):
        self.nsem += 1
        return self.stack.enter_context(self.nc.semaphore(name))

    def _newsem(self, e):
        key = f"{e}{self.epoch[e]}"
        self.epoch[e] += 1
        self.sems[key] = self._alloc("s_" + key)
        self.cur[e] = [key, 0]

    def ent(self, name):
        return Ent(name)

    def _deps(self, e, reads, writes):
        deps = {}
        def add(ev):
            if ev is None:
                return
            k, v = ev
            if deps.get(k, 0) < v:
                deps[k] = v
        for r in reads:
            add(r.lastw)
        for w in writes:
            add(w.lastw)
            for k, v in w.reads.items():
                add((k, v))
        return deps

    def _wait(self, e, deps, skip_self_pe=False):
        seen = self.seen[e]
        engine = self.eng[e]
        for k, v in deps.items():
            if skip_self_pe and k.startswith("pe") and e == "pe":
                continue
            if seen.get(k, 0) >= v:
                continue
            engine.wait_ge(self.sems[k], v)
            seen[k] = v

    def _mark(self, ev, reads, writes):
        k, v = ev
        for r in reads:
            if r.reads.get(k, 0) < v:
                r.reads[k] = v
        for w in writes:
            w.lastw = ev
            w.reads = {}

    def op(self, e, fn, reads=(), writes=(), pe_chain=False):
        deps = self._deps(e, reads, writes)
        self._wait(e, deps, skip_self_pe=pe_chain)
        inst = fn(self.eng[e])
        cur = self.cur[e]
        cur[1] += 1
        inst.then_inc(self.sems[cur[0]], 1)
        ev = (cur[0], cur[1])
        self._mark(ev, reads, writes)
        self.ninst += 1
        if cur[1] >= SEM_ROLL:
            self._newsem(e)
        return inst

    def dma(self, q, out, in_, reads=(), writes=(), sem_ent=None, **kw):
        deps = self._deps(q, reads, writes)
        self._wait(q, deps)
        se = sem_ent if sem_ent is not None else writes[0]
        if se.dsem is None:
            se.dsem = f"d{self.nsem}_{se.name}"
            self.sems[se.dsem] = self._alloc(se.dsem)
        se.dcount += 16
        self.eng[q].dma_start(out=out, in_=in_, **kw).then_inc(self.sems[se.dsem], 16)
        ev = (se.dsem, se.dcount)
        self._mark(ev, reads, writes)
        self.ninst += 1

    def wait_all(self, e, ents):
        deps = self._deps(e, ents, ents)
        self._wait(e, deps)


import contextlib
from concourse.bass_utils import run_bass_kernel_spmd

D = 2048
ALPHA = 4.0 ** 0.25
LN_EPS = 1e-5
RMS_EPS = 1e-6


class Ctx:
    def __init__(self):
        self.nc = bass.Bass("TRN2", target_bir_lowering=False)
        self.st = contextlib.ExitStack()
        self.S = Sched(self.nc, self.st)
        self.n = 0

    def din(self, name, shape, dt=F32):
        return self.nc.dram_tensor(name, list(shape), dt, kind="ExternalInput").ap()

    def dout(self, name, shape, dt=F32):
        return self.nc.dram_tensor(name, list(shape), dt, kind="ExternalOutput").ap()

    def sb(self, name, shape, dt=F32):
        t = self.st.enter_context(self.nc.sbuf_tensor(name, list(shape), dt))
        return t

    def ps(self, name, shape=(128, 512), dt=F32):
        return self.st.enter_context(self.nc.psum_tensor(name, list(shape), dt))

    def ent(self, name):
        return self.S.ent(name)


class Tiles:
    def __init__(self, cx, name, n, w, dt=F32):
        self.t = cx.sb(name, [128, n, w], dt)
        self.e = [cx.ent(f"{name}{i}") for i in range(n)]
        self.n = n

    def __getitem__(self, i):
        return self.t[:, i, :]


def build_rest(NT, T=512, NE=32):
    cx = Ctx(); nc = cx.nc; S = cx.S
    NB = NT // T
    hT = cx.din("hT", [D, NT]); ysT = cx.din("ysT", [4, 1024, NT]); pT = cx.din("pT", [256, NT])
    w_gate = cx.din("w_gate", [D, 4 * D]); w_branch = cx.din("w_branch", [4, 1024, D]); w_out = cx.din("w_out", [D, D])
    ssd_norm_w = cx.din("ssd_norm_w", [1024]); w_glu = cx.din("s5_w_glu", [1024, 1024]); b_glu = cx.din("s5_b_glu", [1024])
    lnw = [cx.din(f"ln{i}_w", [D]) for i in (1, 2, 3)]; lnb = [cx.din(f"ln{i}_b", [D]) for i in (1, 2, 3)]
    router_w = cx.din("router_w", [D, NE]); router_b = cx.din("router_b", [NE])
    w_gu = cx.din("moe_w_gate_up", [NE, D, D]); b_gu = cx.din("moe_b_gate_up", [NE, D])
    w_dn = cx.din("moe_w_down", [NE, 1024, D]); b_dn = cx.din("moe_b_down", [NE, D])
    ple_wg = cx.din("ple_w_gate", [D, D]); ple_wp = cx.din("ple_w_proj", [256, D])
    c_ones = cx.din("c_ones", [128, 128]); c_ident = cx.din("c_ident", [128, 128]); c_sel = cx.din("c_sel", [32, 32 * 128])
    oT = cx.dout("oT", [D, NT])
    eo = cx.ent("oT")

    ec = cx.ent("consts")
    ones = cx.sb("ones", [128, 128]); ident = cx.sb("ident", [128, 128])
    lnw_t = cx.sb("lnw_t", [128, 3, 16]); lnb_t = cx.sb("lnb_t", [128, 3, 16])
    snw_t = cx.sb("snw_t", [128, 8]); bglu_t = cx.sb("bglu_t", [128, 8])
    bgu_t = cx.sb("bgu_t", [128, NE, 16]); bdn_t = cx.sb("bdn_t", [NE, D])
    rw_t = cx.sb("rw_t", [128, 16, NE]); rb_t = cx.sb("rb_t", [128, NE])
    wp_t = [cx.sb(f"wp_t{i}", [128, 2, 128]) for i in range(2)]; ewp = [cx.ent(f"wp{i}") for i in range(2)]
    S.dma("sp", ones[:], c_ones, writes=[ec]); S.dma("sp", ident[:], c_ident, writes=[ec])
    for i in range(3):
        S.dma("sp", lnw_t[:, i, :], lnw[i].rearrange("(t p) -> p t", p=128), writes=[ec], allow_slow_non_contiguous=True)
        S.dma("sp", lnb_t[:, i, :], lnb[i].rearrange("(t p) -> p t", p=128), writes=[ec], allow_slow_non_contiguous=True)
    S.dma("sp", snw_t[:], ssd_norm_w.rearrange("(t p) -> p t", p=128), writes=[ec], allow_slow_non_contiguous=True)
    S.dma("sp", bglu_t[:], b_glu.rearrange("(t p) -> p t", p=128), writes=[ec], allow_slow_non_contiguous=True)
    for e8 in range(NE // 8):
        S.dma("sp", bgu_t[:, e8 * 8:(e8 + 1) * 8, :], b_gu[e8 * 8:(e8 + 1) * 8].rearrange("e (t p) -> p e t", p=128), writes=[ec], allow_slow_non_contiguous=True)
    S.dma("sp", bdn_t[:], b_dn, writes=[ec])
    S.dma("sp", rw_t[:], router_w.rearrange("(k p) e -> p k e", p=128), writes=[ec])
    S.dma("sp", rb_t[:], router_b.partition_broadcast(128), writes=[ec])

    h = Tiles(cx, "h", 16, T); u = Tiles(cx, "u", 16, T); mg = Tiles(cx, "mg", 16, T)
    ysA = Tiles(cx, "ysA", 8, T); ysB = u
    ppt = Tiles(cx, "ppt", 2, T)
    tmp = Tiles(cx, "tmp", 6, T)
    mean = cx.sb("mean", [128, T]); e_mean = cx.ent("mean")
    rstd = cx.sb("rstd", [128, T]); e_rstd = cx.ent("rstd")
    gT = cx.sb("gT", [NE, T]); e_gT = cx.ent("gT")
    gbc = cx.sb("gbc", [128, T]); e_gbc = cx.ent("gbc")
    rs = cx.sb("rs", [128, 160]); e_rs = cx.ent("rs")
    NWA = 3
    wa = [cx.sb(f"wa{i}", [128, 16, 128]) for i in range(NWA)]; ewa = [cx.ent(f"wa{i}") for i in range(NWA)]
    wb = [cx.sb(f"wb{i}", [128, 8, 128]) for i in range(NWA)]; ewb = [cx.ent(f"wb{i}") for i in range(NWA)]
    cnt = {"wa": 0, "wb": 0, "tmp": 0, "ps": 0, "q": 0}
    psA = [cx.ps(f"psA{i}") for i in range(3)]; epsA = [cx.ent(f"psA{i}") for i in range(3)]
    psB = [cx.ps(f"psB{i}") for i in range(3)]; epsB = [cx.ent(f"psB{i}") for i in range(3)]
    psS = cx.ps("psS"); epsS = cx.ent("psS")
    psQ = cx.ps("psQ"); epsQ = cx.ent("psQ")
    pc = {"A": 0, "B": 0}

    def nextq():
        cnt["q"] += 1
        return "sp" if cnt["q"] % 2 else "pool"

    def load_wa(src2d, c0):
        i = cnt["wa"] % NWA; cnt["wa"] += 1
        S.dma(nextq(), wa[i][:], src2d[:, c0:c0 + 128].rearrange("(k p) c -> p k c", p=128), writes=[ewa[i]])
        return wa[i], ewa[i]

    def load_wb(src2d, c0):
        i = cnt["wb"] % NWA; cnt["wb"] += 1
        S.dma(nextq(), wb[i][:], src2d[:, c0:c0 + 128].rearrange("(k p) c -> p k c", p=128), writes=[ewb[i]])
        return wb[i], ewb[i]

    def getps(kind):
        lst, el = (psA, epsA) if kind == "A" else (psB, epsB)
        i = pc[kind] % 3; pc[kind] += 1
        return lst[i], el[i]

    def gettmp():
        i = cnt["tmp"] % 6; cnt["tmp"] += 1
        return tmp[i], tmp.e[i]

    def mm_acc(ps, eps, w, ew, acts, nk):
        for k in range(nk):
            S.op("pe", lambda e, k=k: e.matmul(ps[:], w[:, k, :], acts[k], start=(k == 0), stop=(k == nk - 1)),
                 reads=[ew, acts.e[k]], writes=[eps], pe_chain=(k > 0))

    def stats(src, idxs, nfeat, eps_val):
        n = len(idxs)
        for j, k in enumerate(idxs):
            S.op("pe", lambda e, k=k, j=j: e.matmul(psS[:], ones[:], src[k], start=(j == 0), stop=(j == n - 1)),
                 reads=[ec, src.e[k]], writes=[epsS], pe_chain=(j > 0))
        for j, k in enumerate(idxs):
            t, et = gettmp()
            S.op("act", lambda e, k=k, t=t: e.activation(out=t, in_=src[k], func=AF.Square), reads=[src.e[k]], writes=[et])
            S.op("pe", lambda e, t=t, j=j: e.matmul(psQ[:], ones[:], t, start=(j == 0), stop=(j == n - 1)),
                 reads=[ec, et], writes=[epsQ], pe_chain=(j > 0))
        return n

    def layernorm(src, dst, li):
        stats(src, list(range(16)), D, LN_EPS)
        S.op("act", lambda e: e.activation(out=mean[:], in_=psS[:], func=AF.Copy, scale=1.0 / D), reads=[epsS], writes=[e_mean])
        t, et = gettmp()
        S.op("dve", lambda e: e.tensor_tensor(t, mean[:], mean[:], ALU.mult), reads=[e_mean], writes=[et])
        S.op("dve", lambda e: e.scalar_tensor_tensor(out=rstd[:], in0=psQ[:], scalar=1.0 / D, in1=t, op0=ALU.mult, op1=ALU.subtract),
             reads=[epsQ, et], writes=[e_rstd])
        S.op("dve", lambda e: e.tensor_scalar(rstd[:], rstd[:], LN_EPS, None, ALU.add), reads=[e_rstd], writes=[e_rstd])
        S.op("act", lambda e: e.activation(out=rstd[:], in_=rstd[:], func=AF.Sqrt), reads=[e_rstd], writes=[e_rstd])
        S.op("dve", lambda e: e.reciprocal(rstd[:], rstd[:]), reads=[e_rstd], writes=[e_rstd])
        for k in range(16):
            t, et = gettmp()
            S.op("dve", lambda e, k=k, t=t: e.tensor_tensor(t, src[k], mean[:], ALU.subtract), reads=[src.e[k], e_mean], writes=[et])
            S.op("pool", lambda e, t=t: e.tensor_tensor(t, t, rstd[:], ALU.mult), reads=[et, e_rstd], writes=[et])
            S.op("act", lambda e, k=k, t=t: e.activation(out=dst[k], in_=t, func=AF.Identity, scale=lnw_t[:, li, k:k + 1], bias=lnb_t[:, li, k:k + 1]),
                 reads=[et, ec], writes=[dst.e[k]])

    for blk in range(NB):
        tk = slice(blk * T, (blk + 1) * T)
        for k in range(16):
            S.dma(nextq(), h[k], hT[k * 128:(k + 1) * 128, tk], writes=[h.e[k]])
        for k in range(2):
            S.dma(nextq(), ppt[k], pT[k * 128:(k + 1) * 128, tk], writes=[ppt.e[k]])
        for n in range(4):
            for k in range(8):
                S.dma(nextq(), ysA[k], ysT[n, k * 128:(k + 1) * 128, tk], writes=[ysA.e[k]])
            ysrc = ysA
            if n == 0:
                for g in range(2):
                    stats(ysA, [4 * g + i for i in range(4)], 512, RMS_EPS)
                    S.op("dve", lambda e: e.tensor_scalar(rstd[:], psQ[:], 1.0 / 512, RMS_EPS, ALU.mult, ALU.add), reads=[epsQ], writes=[e_rstd])
                    S.op("act", lambda e: e.activation(out=rstd[:], in_=rstd[:], func=AF.Sqrt), reads=[e_rstd], writes=[e_rstd])
                    S.op("dve", lambda e: e.reciprocal(rstd[:], rstd[:]), reads=[e_rstd], writes=[e_rstd])
                    for i in range(4):
                        k = 4 * g + i
                        S.op("dve", lambda e, k=k: e.scalar_tensor_tensor(out=ysA[k], in0=ysA[k], scalar=snw_t[:, k:k + 1], in1=rstd[:], op0=ALU.mult, op1=ALU.mult),
                             reads=[ysA.e[k], e_rstd, ec], writes=[ysA.e[k]])
            if n == 1:
                for m in range(8):
                    w, ew = load_wb(w_glu, m * 128)
                    ps, eps = getps("A")
                    mm_acc(ps, eps, w, ew, ysA, 8)
                    t, et = gettmp()
                    S.op("act", lambda e, t=t, ps=ps, m=m: e.activation(out=t, in_=ps[:], func=AF.Sigmoid, bias=bglu_t[:, m:m + 1]), reads=[eps, ec], writes=[et])
                    S.op("dve", lambda e, t=t, m=m: e.tensor_tensor(ysB[m], ysA[m], t, ALU.mult), reads=[ysA.e[m], et], writes=[ysB.e[m]])
                ysrc = ysB
            for m in range(16):
                w, ew = load_wa(w_gate, n * D + m * 128)
                psg, epsg = getps("A")
                mm_acc(psg, epsg, w, ew, h, 16)
                w2, ew2 = load_wb(w_branch[n], m * 128)
                psb, epsb = getps("B")
                mm_acc(psb, epsb, w2, ew2, ysrc, 8)
                t, et = gettmp()
                S.op("act", lambda e, t=t, psg=psg: e.activation(out=t, in_=psg[:], func=AF.Sigmoid), reads=[epsg], writes=[et])
                if n == 0:
                    S.op("dve", lambda e, t=t, psb=psb, m=m: e.tensor_tensor(mg[m], t, psb[:], ALU.mult), reads=[et, epsb], writes=[mg.e[m]])
                else:
                    S.op("dve", lambda e, t=t, psb=psb: e.tensor_tensor(t, t, psb[:], ALU.mult), reads=[et, epsb], writes=[et])
                    S.op("pool", lambda e, t=t, m=m: e.tensor_tensor(mg[m], mg[m], t, ALU.add), reads=[et, mg.e[m]], writes=[mg.e[m]])
        for m in range(16):
            w, ew = load_wa(w_out, m * 128)
            ps, eps = getps("A")
            mm_acc(ps, eps, w, ew, mg, 16)
            S.op("dve", lambda e, ps=ps, m=m: e.scalar_tensor_tensor(out=u[m], in0=h[m], scalar=ALPHA, in1=ps[:], op0=ALU.mult, op1=ALU.add),
                 reads=[h.e[m], eps], writes=[u.e[m]])
        layernorm(u, h, 0)
        for tt in range(T // 128):
            ts_ = slice(tt * 128, (tt + 1) * 128)
            ps, eps = getps("B")
            for k in range(16):
                S.op("pe", lambda e, k=k, ps=ps: e.matmul(ps[:, 0:NE], h.t[:, k, ts_], rw_t[:, k, :], start=(k == 0), stop=(k == 15)),
                     reads=[h.e[k], ec], writes=[eps], pe_chain=(k > 0))
            lg = rs[:, 0:NE]; m8 = rs[:, 32:40]; msk = rs[:, 40:40 + NE]; ex = rs[:, 72:72 + NE]; sm = rs[:, 104:105]; nmx = rs[:, 105:106]; gw = rs[:, 112:112 + NE]
            S.op("dve", lambda e, ps=ps: e.tensor_tensor(lg, ps[:, 0:NE], rb_t[:], ALU.add), reads=[eps, ec], writes=[e_rs])
            S.op("dve", lambda e: e.max(m8, lg), reads=[e_rs], writes=[e_rs])
            S.op("dve", lambda e: e.tensor_scalar(msk, lg, rs[:, 35:36], None, ALU.is_ge), reads=[e_rs], writes=[e_rs])
            S.op("dve", lambda e: e.tensor_scalar(nmx, rs[:, 32:33], -1.0, None, ALU.mult), reads=[e_rs], writes=[e_rs])
            S.op("act", lambda e: e.activation(out=ex, in_=lg, func=AF.Exp, bias=nmx), reads=[e_rs], writes=[e_rs])
            S.op("dve", lambda e: e.tensor_tensor(ex, ex, msk, ALU.mult), reads=[e_rs], writes=[e_rs])
            S.op("dve", lambda e: e.reduce_sum(sm, ex, AX.X), reads=[e_rs], writes=[e_rs])
            S.op("dve", lambda e: e.reciprocal(sm, sm), reads=[e_rs], writes=[e_rs])
            S.op("dve", lambda e: e.tensor_scalar(gw, ex, sm, None, ALU.mult), reads=[e_rs], writes=[e_rs])
            ps2, eps2 = getps("B")
            S.op("pe", lambda e, ps2=ps2: e.transpose(ps2[0:NE, 0:128], gw, ident[:]), reads=[e_rs, ec], writes=[eps2])
            S.op("act", lambda e, ps2=ps2: e.activation(out=gT[:, ts_], in_=ps2[0:NE, 0:128], func=AF.Copy), reads=[eps2], writes=[e_gT])
        for m in range(16):
            ps, eps = getps("A")
            S.op("pe", lambda e, ps=ps, m=m: e.matmul(ps[:], bdn_t[:, m * 128:(m + 1) * 128], gT[:], start=True, stop=True), reads=[ec, e_gT], writes=[eps])
            S.op("act", lambda e, ps=ps, m=m: e.activation(out=mg[m], in_=ps[:], func=AF.Copy), reads=[eps], writes=[mg.e[m]])
        for ex_i in range(NE):
            ps, eps = getps("B")
            S.op("pe", lambda e, ps=ps: e.matmul(ps[:], ident[0:NE, ex_i:ex_i + 1].to_broadcast([NE, 128]), gT[:], start=True, stop=True), reads=[ec, e_gT], writes=[eps])
            S.op("act", lambda e, ps=ps: e.activation(out=gbc[:], in_=ps[:], func=AF.Copy), reads=[eps], writes=[e_gbc])
            for f in range(8):
                w, ew = load_wa(w_gu[ex_i], f * 128)
                psg, epsg = getps("A")
                mm_acc(psg, epsg, w, ew, h, 16)
                w2, ew2 = load_wa(w_gu[ex_i], 1024 + f * 128)
                psu, epsu = getps("B")
                mm_acc(psu, epsu, w2, ew2, h, 16)
                gc, egc = gettmp(); sg, esg = gettmp(); uc, euc = gettmp()
                S.op("dve", lambda e: e.tensor_scalar(gc, psg[:], bgu_t[:, ex_i, f:f + 1], 7.0, ALU.add, ALU.min), reads=[epsg, ec], writes=[egc])
                S.op("act", lambda e: e.activation(out=sg, in_=gc, func=AF.Sigmoid, scale=1.702), reads=[egc], writes=[esg])
                S.op("dve", lambda e: e.tensor_scalar(uc, psu[:], bgu_t[:, ex_i, 8 + f:9 + f], 7.0, ALU.add, ALU.min), reads=[epsu, ec], writes=[euc])
                S.op("dve", lambda e: e.tensor_scalar(uc, uc, -7.0, 1.0, ALU.max, ALU.add), reads=[euc], writes=[euc])
                S.op("pool", lambda e: e.tensor_tensor(gc, gc, sg, ALU.mult), reads=[egc, esg], writes=[egc])
                S.op("pool", lambda e: e.tensor_tensor(uc, uc, gc, ALU.mult), reads=[egc, euc], writes=[euc])
                S.op("pool", lambda e: e.tensor_tensor(ysA[f], uc, gbc[:], ALU.mult), reads=[euc, e_gbc], writes=[ysA.e[f]])
            for m in range(16):
                w, ew = load_wb(w_dn[ex_i], m * 128)
                ps, eps = getps("A")
                mm_acc(ps, eps, w, ew, ysA, 8)
                S.op("dve", lambda e, ps=ps, m=m: e.tensor_tensor(mg[m], mg[m], ps[:], ALU.add), reads=[mg.e[m], eps], writes=[mg.e[m]])
        for m in range(16):
            S.op("dve", lambda e, m=m: e.scalar_tensor_tensor(out=u[m], in0=h[m], scalar=ALPHA, in1=mg[m], op0=ALU.mult, op1=ALU.add),
                 reads=[h.e[m], mg.e[m]], writes=[u.e[m]])
        layernorm(u, h, 1)
        for m in range(16):
            w, ew = load_wa(ple_wg, m * 128)
            psg, epsg = getps("A")
            mm_acc(psg, epsg, w, ew, h, 16)
            psb, epsb = getps("B")
            wpt, ewpt = wp_t[m % 2], ewp[m % 2]
            S.dma(nextq(), wpt[:], ple_wp[:, m * 128:(m + 1) * 128].rearrange("(k p) d -> p k d", p=128), writes=[ewpt])
            for k in range(2):
                S.op("pe", lambda e, k=k, psb=psb, wpt=wpt: e.matmul(psb[:], wpt[:, k, :], ppt[k], start=(k == 0), stop=(k == 1)),
                     reads=[ewpt, ppt.e[k]], writes=[epsb], pe_chain=(k > 0))
            t, et = gettmp()
            S.op("act", lambda e, t=t, psg=psg: e.activation(out=t, in_=psg[:], func=AF.Sigmoid), reads=[epsg], writes=[et])
            S.op("dve", lambda e, t=t, psb=psb: e.tensor_tensor(t, t, psb[:], ALU.mult), reads=[et, epsb], writes=[et])
            S.op("dve", lambda e, t=t, m=m: e.scalar_tensor_tensor(out=u[m], in0=h[m], scalar=ALPHA, in1=t, op0=ALU.mult, op1=ALU.add),
                 reads=[h.e[m], et], writes=[u.e[m]])
        layernorm(u, mg, 2)
        for k in range(16):
            S.dma(nextq(), oT[k * 128:(k + 1) * 128, tk], mg[k], reads=[mg.e[k]], writes=[eo])
    S.wait_all("sp", [eo])
    cx.st.close()
    return nc


NWT = 22
TWO_PI = 2.0 * np.pi


def build_mixer(S_len, T=512):
    cx = Ctx(); nc = cx.nc; S = cx.S
    NB = S_len // T
    hT = cx.din("hT", [D, S_len]); Wm = cx.din("Wm", [D, NWT * 128]); pos = cx.din("pos", [S_len], I32)
    cpar = cx.din("cpar", [128, 64])
    c_ident = cx.din("c_ident", [128, 128]); c_ones = cx.din("c_ones", [128, 128]); c_wide = cx.din("c_wide", [128, 255])
    c_sel = cx.din("c_sel", [128, 2, 128]); c_rot = cx.din("c_rot", [128, 128]); c_tau = cx.din("c_tau", [128, T])
    w_alpha = cx.din("w_alpha", [128, 128])
    s5B = cx.din("s5B", [2, 128, 8, 128]); s5C = cx.din("s5C", [2, 128, 8, 128])
    yT = cx.dout("yT", [4, 256, S_len]); eo = cx.ent("yT")
    ec = cx.ent("consts")
    par = cx.sb("par", [128, 64]); ident = cx.sb("ident", [128, 128]); ones = cx.sb("ones", [128, 128]); wide = cx.sb("wide", [128, 255])
    sel = cx.sb("sel", [128, 2, 128]); rot = cx.sb("rot", [128, 128]); tau = cx.sb("tau", [128, T]); wal = cx.sb("wal", [128, 128])
    Bl = cx.sb("Bl", [128, 2, 8, 128]); Cl = cx.sb("Cl", [128, 2, 8, 128])
    for dst, src in ((par, cpar), (ident, c_ident), (ones, c_ones), (wide, c_wide), (sel, c_sel), (rot, c_rot), (tau, c_tau), (wal, w_alpha)):
        S.dma("sp", dst[:], src, writes=[ec])
    for ri in range(2):
        S.dma("sp", Bl[:, ri], s5B[ri], writes=[ec]); S.dma("sp", Cl[:, ri], s5C[ri], writes=[ec])
    CW = 0
    CB = 16
    DTB, ALOG, DSK = 20, 21, 22
    S5D = 24
    LRE, LIM = 26, 27
    LDT = 28
    RNW = 36
    INVF = 38
    LGAM = 39
    BAL = 40
    GNW = 41
    def pc(c):
        return par[:, c:c + 1]

    P = Tiles(cx, "P", NWT, T)
    xe = cx.sb("xe", [128, 4, T + 3]); e_xe = [cx.ent(f"xe{i}") for i in range(4)]
    hb = cx.sb("hb", [128, 16, T]); e_hb = cx.ent("hb")
    wa = [cx.sb(f"mwa{i}", [128, 16, 128]) for i in range(2)]; ewa = [cx.ent(f"mwa{i}") for i in range(2)]
    tmp = Tiles(cx, "mt", 12, T)
    Lg = Tiles(cx, "lg", 7, T)
    cnt = {"t": 0, "q": 0, "pa": 0, "pb": 0}
    psA = [cx.ps(f"mpA{i}") for i in range(2)]; epsA = [cx.ent(f"mpA{i}") for i in range(2)]
    psB = [cx.ps(f"mpB{i}") for i in range(2)]; epsB = [cx.ent(f"mpB{i}") for i in range(2)]
    psY = [cx.ps(f"mpY{i}") for i in range(2)]; epsY = [cx.ent(f"mpY{i}") for i in range(2)]
    psZ = [cx.ps(f"mpZ{i}") for i in range(2)]; epsZ = [cx.ent(f"mpZ{i}") for i in range(2)]
    carry = cx.sb("carry", [128, 3, 256]); e_carry = [cx.ent(f"carry{i}") for i in range(3)]
    s5c = cx.sb("s5c", [128, 2, 8]); e_s5c = cx.ent("s5c")
    posf = cx.sb("posf", [128, T]); posi = cx.sb("posi", [128, T], I32); e_pos = cx.ent("pos")
    kint = cx.sb("kint", [128, T], I32); e_kint = cx.ent("kint")
    TS = T // 2
    Tp = cx.sb("Tp", [128, 2, 8, TS]); Tn = cx.sb("Tn", [128, 2, 8, TS]); e_tab = cx.ent("tab")
    sm = cx.sb("sm", [128, 64]); e_sm = cx.ent("sm")

    def gt():
        i = cnt["t"] % 12; cnt["t"] += 1
        return tmp[i], tmp.e[i]

    def gp(kind):
        lst, el = (psA, epsA) if kind == "A" else (psB, epsB)
        key = "pa" if kind == "A" else "pb"
        i = cnt[key] % 2; cnt[key] += 1
        return lst[i], el[i]

    def nextq():
        cnt["q"] += 1
        return "sp" if cnt["q"] % 2 else "pool"

    def dve(fn, r, w): S.op("dve", fn, reads=r, writes=w)
    def act(fn, r, w): S.op("act", fn, reads=r, writes=w)
    def pool(fn, r, w): S.op("pool", fn, reads=r, writes=w)
    def pe(fn, r, w, chain=False): S.op("pe", fn, reads=r, writes=w, pe_chain=chain)

    def sincos(ang, eang, out_sin, eout, shift):
        t, et = gt()
        dve(lambda e: e.tensor_scalar(t, ang, shift, 1.0 / TWO_PI, ALU.add, ALU.mult), [eang], [et])
        dve(lambda e: e.tensor_copy(kint[:], t), [et], [e_kint])
        dve(lambda e: e.tensor_copy(t, kint[:]), [e_kint], [et])
        t2, et2 = gt()
        dve(lambda e: e.tensor_scalar(t2, ang, shift, None, ALU.add), [eang], [et2])
        dve(lambda e: e.scalar_tensor_tensor(out=t2, in0=t, scalar=-6.28125, in1=t2, op0=ALU.mult, op1=ALU.add), [et2, et], [et2])
        dve(lambda e: e.scalar_tensor_tensor(out=t2, in0=t, scalar=-1.9353071795864769e-3, in1=t2, op0=ALU.mult, op1=ALU.add), [et2, et], [et2])
        dve(lambda e: e.tensor_scalar(t, t2, float(np.pi), -TWO_PI, ALU.is_gt, ALU.mult), [et2], [et])
        t3, et3 = gt()
        dve(lambda e: e.tensor_scalar(t3, t2, -float(np.pi), TWO_PI, ALU.is_lt, ALU.mult), [et2], [et3])
        dve(lambda e: e.tensor_tensor(t2, t2, t, ALU.add), [et2, et], [et2])
        dve(lambda e: e.tensor_tensor(t2, t2, t3, ALU.add), [et2, et3], [et2])
        dve(lambda e: e.tensor_scalar(t2, t2, float(np.pi), -float(np.pi), ALU.min, ALU.max), [et2], [et2])
        act(lambda e: e.activation(out=out_sin, in_=t2, func=AF.Sin), [et2], [eout])

    S.wait_all("dve", [ec]); S.wait_all("act", [ec])
    lr = sm[:, 0:1]; dtc = sm[:, 8:16]; acol = sm[:, 16:24]; thcol = sm[:, 24:32]
    dve(lambda e: e.tensor_scalar(lr, pc(LRE), -1e-4, None, ALU.min), [ec], [e_sm])
    act(lambda e: e.activation(out=dtc, in_=par[:, LDT:LDT + 8], func=AF.Exp), [ec], [e_sm])
    dve(lambda e: e.tensor_scalar(acol, dtc, lr, None, ALU.mult), [e_sm], [e_sm])
    dve(lambda e: e.tensor_scalar(thcol, dtc, pc(LIM), None, ALU.mult), [e_sm, ec], [e_sm])
    nacol = sm[:, 32:40]
    dve(lambda e: e.tensor_scalar(nacol, acol, -1.0, None, ALU.mult), [e_sm], [e_sm])
    for j in range(8):
        ang, eang = gt()
        dve(lambda e: e.tensor_scalar(ang, tau[:], thcol[:, j:j + 1], None, ALU.mult), [ec, e_sm], [eang])
        sn, esn = gt(); cs, ecs = gt(); mg_, emg = gt(); mn, emn = gt()
        sincos(ang, eang, sn, esn, 0.0)
        sincos(ang, eang, cs, ecs, float(np.pi / 2))
        act(lambda e: e.activation(out=mg_, in_=tau[:], func=AF.Exp, scale=acol[:, j:j + 1]), [ec, e_sm], [emg])
        act(lambda e: e.activation(out=mn, in_=tau[:], func=AF.Exp, scale=nacol[:, j:j + 1]), [ec, e_sm], [emn])
        dve(lambda e: e.tensor_tensor(Tp[:, 0, j, :], mg_[:, 0:TS], cs[:, 0:TS], ALU.mult), [emg, ecs], [e_tab])
        dve(lambda e: e.tensor_tensor(Tp[:, 1, j, :], mg_[:, 0:TS], sn[:, 0:TS], ALU.mult), [emg, esn], [e_tab])
        dve(lambda e: e.tensor_tensor(Tn[:, 0, j, :], mn[:, 0:TS], cs[:, 0:TS], ALU.mult), [emn, ecs], [e_tab])
        dve(lambda e: e.scalar_tensor_tensor(out=Tn[:, 1, j, :], in0=mn[:, 0:TS], scalar=-1.0, in1=sn[:, 0:TS], op0=ALU.mult, op1=ALU.mult), [emn, esn], [e_tab])
    abr = sm[:, 40:48]; abi = sm[:, 48:56]; fre = sm[:, 56:64]
    sm2 = cx.sb("sm2", [128, 64]); e_sm2 = cx.ent("sm2")
    fim = sm2[:, 0:8]; den = sm2[:, 8:9]; t8 = sm2[:, 16:24]; lbr = sm2[:, 24:32]; lbi = sm2[:, 32:40]
    dve(lambda e: e.tensor_copy(lbr, Tp[:, 0, :, 1]), [e_tab], [e_sm2])
    dve(lambda e: e.tensor_copy(lbi, Tp[:, 1, :, 1]), [e_tab], [e_sm2])
    dve(lambda e: e.tensor_scalar(abr, lbr, -1.0, None, ALU.add), [e_sm2], [e_sm])
    dve(lambda e: e.tensor_tensor(den, lr, lr, ALU.mult), [e_sm], [e_sm2])
    dve(lambda e: e.scalar_tensor_tensor(out=den, in0=pc(LIM), scalar=pc(LIM), in1=den, op0=ALU.mult, op1=ALU.add), [ec, e_sm2], [e_sm2])
    dve(lambda e: e.reciprocal(den, den), [e_sm2], [e_sm2])
    dve(lambda e: e.tensor_scalar(t8, lbi, pc(LIM), None, ALU.mult), [e_sm2, ec], [e_sm2])
    dve(lambda e: e.scalar_tensor_tensor(out=fre, in0=abr, scalar=lr, in1=t8, op0=ALU.mult, op1=ALU.add), [e_sm, e_sm2], [e_sm])
    dve(lambda e: e.tensor_scalar(fre, fre, den, None, ALU.mult), [e_sm, e_sm2], [e_sm])
    dve(lambda e: e.tensor_scalar(t8, abr, pc(LIM), None, ALU.mult), [e_sm, ec], [e_sm2])
    dve(lambda e: e.scalar_tensor_tensor(out=fim, in0=lbi, scalar=lr, in1=t8, op0=ALU.mult, op1=ALU.subtract), [e_sm, e_sm2], [e_sm2])
    dve(lambda e: e.tensor_scalar(fim, fim, den, None, ALU.mult), [e_sm2], [e_sm2])
    for j in range(8):
        a_, ea = gt(); b_, eb = gt(); a_ = a_[:, 0:TS]; b_ = b_[:, 0:TS]
        dve(lambda e: e.tensor_scalar(a_, Tn[:, 0, j, :], fre[:, j:j + 1], None, ALU.mult), [e_tab, e_sm], [ea])
        dve(lambda e: e.tensor_scalar(b_, Tn[:, 1, j, :], fre[:, j:j + 1], None, ALU.mult), [e_tab, e_sm], [eb])
        dve(lambda e: e.scalar_tensor_tensor(out=b_, in0=Tn[:, 0, j, :], scalar=fim[:, j:j + 1], in1=b_, op0=ALU.mult, op1=ALU.add), [e_tab, e_sm2, eb], [eb])
        dve(lambda e: e.scalar_tensor_tensor(out=t8[:, 0:1].to_broadcast([128, 1]) if False else a_, in0=Tn[:, 1, j, :], scalar=fim[:, j:j + 1], in1=a_, op0=ALU.mult, op1=ALU.subtract), [e_tab, e_sm2, ea], [ea])
        dve(lambda e: e.tensor_scalar(Tn[:, 0, j, :], a_, -1.0, None, ALU.mult), [ea], [e_tab])
        dve(lambda e: e.tensor_copy(Tn[:, 1, j, :], b_), [eb], [e_tab])
    for i in range(3):
        dve(lambda e, i=i: e.memset(carry[:, i, :], 0.0), [], [e_carry[i]])
    dve(lambda e: e.memset(s5c[:], 0.0), [], [e_s5c])
    for i in range(4):
        dve(lambda e, i=i: e.memset(xe[:, i, 0:3], 0.0), [], [e_xe[i]])
    nbal = sm2[:, 40:41]; nalog = sm2[:, 41:42]
    dve(lambda e: e.tensor_scalar(nbal, pc(BAL), -1.0, None, ALU.mult), [ec], [e_sm2])
    act(lambda e: e.activation(out=nalog, in_=pc(ALOG), func=AF.Exp), [ec], [e_sm2])
    dve(lambda e: e.tensor_scalar(nalog, nalog, -1.0, None, ALU.mult), [e_sm2], [e_sm2])
    gam = sm2[:, 42:43]
    act(lambda e: e.activation(out=gam, in_=pc(LGAM), func=AF.Exp), [ec], [e_sm2])

    def chan_scan(mix, nch, k_ap, ek, q_ap, eq, a_of, v_of, out_of):
        for e_ in range(nch):
            vt, evt, row, halves = v_of(e_)
            pb, epb = gp("B")
            if halves is None:
                pe(lambda e: e.matmul(pb[:], ident[:, row:row + 1].to_broadcast([128, 128]), vt, start=True, stop=True), [ec, evt], [epb])
            else:
                (v0, ev0), (v1, ev1) = halves
                pe(lambda e: e.matmul(pb[0:64, :], ident[:, row:row + 1].to_broadcast([128, 64]), v0, start=True, stop=True), [ec, ev0], [epb])
                pe(lambda e: e.matmul(pb[64:128, :], ident[:, row:row + 1].to_broadcast([128, 64]), v1, start=True, stop=True), [ec, ev1], [epb])
            d1, ed1 = gt()
            dve(lambda e: e.tensor_tensor(d1, k_ap, pb[:], ALU.mult), [ek, epb], [ed1])
            a_ap, ea = a_of(e_)
            st, est = gt()
            dve(lambda e: e.tensor_tensor_scan(st, a_ap, d1, carry[:, mix, e_:e_ + 1], ALU.mult, ALU.add), [ea, ed1, e_carry[mix]], [est])
            act(lambda e: e.activation(out=carry[:, mix, e_:e_ + 1], in_=st[:, T - 1:T], func=AF.Copy), [est], [e_carry[mix]])
            pool(lambda e: e.tensor_tensor(st, st, q_ap, ALU.mult), [est, eq], [est])
            out_of(e_, st, est)

    for blk in range(NB):
        tk = slice(blk * T, (blk + 1) * T)
        S.dma("sp", hb[:], hT.rearrange("(k p) t -> p k t", p=128)[:, :, tk], writes=[e_hb])
        S.dma("pool", posi[:], pos[tk].partition_broadcast(128), writes=[e_pos])
        for wt in range(NWT):
            i = wt % 2
            S.dma(nextq(), wa[i][:], Wm[:, wt * 128:(wt + 1) * 128].rearrange("(k p) c -> p k c", p=128), writes=[ewa[i]])
            ps, eps = gp("A")
            for k in range(16):
                pe(lambda e, k=k: e.matmul(ps[:], wa[i][:, k, :], hb[:, k, :], start=(k == 0), stop=(k == 15)), [ewa[i], e_hb], [eps], chain=(k > 0))
            if 2 <= wt <= 5:
                ci = wt - 2
                act(lambda e: e.activation(out=xe[:, ci, 3:T + 3], in_=ps[:], func=AF.Copy), [eps], [e_xe[ci]])
            else:
                act(lambda e: e.activation(out=P[wt], in_=ps[:], func=AF.Copy), [eps], [P.e[wt]])
        for ci in range(4):
            wt = ci + 2
            dve(lambda e: e.tensor_scalar(P[wt], xe[:, ci, 3:T + 3], pc(CW + 4 * ci + 3), None, ALU.mult), [e_xe[ci], ec], [P.e[wt]])
            for k in range(3):
                dve(lambda e, k=k: e.scalar_tensor_tensor(out=P[wt], in0=xe[:, ci, k:T + k], scalar=pc(CW + 4 * ci + k), in1=P[wt], op0=ALU.mult, op1=ALU.add),
                    [e_xe[ci], ec, P.e[wt]], [P.e[wt]])
            dve(lambda e: e.tensor_copy(xe[:, ci, 0:3], xe[:, ci, T:T + 3]), [e_xe[ci]], [e_xe[ci]])
            act(lambda e: e.activation(out=P[wt], in_=P[wt], func=AF.Silu, bias=pc(CB + ci)), [P.e[wt], ec], [P.e[wt]])
        act(lambda e: e.activation(out=P[6], in_=P[6], func=AF.Exp, bias=pc(DTB)), [P.e[6], ec], [P.e[6]])
        act(lambda e: e.activation(out=P[6], in_=P[6], func=AF.Ln, bias=1.0), [P.e[6]], [P.e[6]])
        arow, earow = Lg[6], Lg.e[6]
        act(lambda e: e.activation(out=arow, in_=P[6], func=AF.Exp, scale=nalog), [P.e[6], e_sm2], [earow])
        abc = []
        for r in range(4):
            pb, epb = gp("B")
            pe(lambda e: e.matmul(pb[:], ident[:, r:r + 1].to_broadcast([128, 128]), arow, start=True, stop=True), [ec, earow], [epb])
            t, et = Lg[r], Lg.e[r]
            act(lambda e: e.activation(out=t, in_=pb[:], func=AF.Copy), [epb], [et])
            abc.append((t, et))
        xdt = []
        for i in range(2):
            pb, epb = gp("B")
            pe(lambda e: e.matmul(pb[:], sel[:, i, :], P[6], start=True, stop=True), [ec, P.e[6]], [epb])
            t, et = Lg[4 + i], Lg.e[4 + i]
            dve(lambda e: e.tensor_tensor(t, P[2 + i], pb[:], ALU.mult), [P.e[2 + i], epb], [et])
            xdt.append((t, et))
        def ssd_out(e_, st, est):
            i, el = divmod(e_, 128)
            pe(lambda e: e.matmul(psY[i][:], wide[:, 127 - el:255 - el], st, start=(el == 0), stop=(el == 127)), [ec, est], [epsY[i]], chain=(el > 0))
        chan_scan(0, 256, P[4], P.e[4], P[5], P.e[5], lambda e_: abc[e_ // 64], lambda e_: (xdt[e_ // 128][0], xdt[e_ // 128][1], e_ % 128, None), ssd_out)
        for i in range(2):
            t, et = gt()
            dve(lambda e: e.scalar_tensor_tensor(out=t, in0=P[2 + i], scalar=pc(DSK + i), in1=psY[i][:], op0=ALU.mult, op1=ALU.add), [P.e[2 + i], ec, epsY[i]], [et])
            act(lambda e: e.activation(out=P[i], in_=P[i], func=AF.Silu), [P.e[i]], [P.e[i]])
            dve(lambda e: e.tensor_tensor(t, t, P[i], ALU.mult), [et, P.e[i]], [et])
            S.dma(nextq(), yT[0, i * 128:(i + 1) * 128, tk], t, reads=[et], writes=[eo])
        dve(lambda e: e.tensor_copy(posf[:], posi[:]), [e_pos], [e_pos])
        ang, eang = gt()
        dve(lambda e: e.tensor_scalar(ang, posf[:], pc(INVF), None, ALU.mult), [e_pos, ec], [eang])
        sn, esn = gt(); cs, ecs = gt()
        sincos(ang, eang, sn, esn, 0.0); sincos(ang, eang, cs, ecs, float(np.pi / 2))
        for wt, scale in ((9, 1.0), (10, 0.125)):
            pb, epb = gp("B")
            pe(lambda e: e.matmul(pb[:], rot[:], P[wt], start=True, stop=True), [ec, P.e[wt]], [epb])
            t, et = gt()
            dve(lambda e: e.tensor_tensor(t, pb[:], sn, ALU.mult), [epb, esn], [et])
            dve(lambda e: e.tensor_tensor(P[wt], P[wt], cs, ALU.mult), [P.e[wt], ecs], [P.e[wt]])
            dve(lambda e: e.tensor_tensor(P[wt], P[wt], t, ALU.add), [P.e[wt], et], [P.e[wt]])
            if scale != 1.0:
                dve(lambda e: e.tensor_scalar(P[wt], P[wt], scale, None, ALU.mult), [P.e[wt]], [P.e[wt]])
        gam_t, egam = Lg[0], Lg.e[0]
        dve(lambda e: e.tensor_scalar(gam_t, tau[:], 0.0, gam, ALU.mult, ALU.add), [ec, e_sm2], [egam])
        def ret_out(e_, st, est):
            for hh in range(2):
                pe(lambda e, hh=hh: e.matmul(psY[hh][:], wide[hh * 64:(hh + 1) * 64, 127 - e_:255 - e_], st[hh * 64:(hh + 1) * 64, :], start=(e_ == 0), stop=(e_ == 127)),
                   [ec, est], [epsY[hh]], chain=(e_ > 0))
        chan_scan(1, 128, P[10], P.e[10], P[9], P.e[9], lambda e_: (gam_t, egam),
                  lambda e_: (None, None, e_, ((P[11], P.e[11]), (P[12], P.e[12]))), ret_out)
        for hh in range(2):
            y, ey = gt()
            act(lambda e: e.activation(out=y, in_=psY[hh][:], func=AF.Copy), [epsY[hh]], [ey])
            sq, esq = gt()
            act(lambda e: e.activation(out=sq, in_=y, func=AF.Square), [ey], [esq])
            pa, epa = gp("A"); pb, epb = gp("B")
            pe(lambda e: e.matmul(pa[:], ones[:], y, start=True, stop=True), [ec, ey], [epa])
            pe(lambda e: e.matmul(pb[:], ones[:], sq, start=True, stop=True), [ec, esq], [epb])
            mu, emu = gt(); rs_, ers = gt()
            act(lambda e: e.activation(out=mu, in_=pa[:], func=AF.Copy, scale=1.0 / 128), [epa], [emu])
            dve(lambda e: e.tensor_tensor(rs_, mu, mu, ALU.mult), [emu], [ers])
            dve(lambda e: e.scalar_tensor_tensor(out=rs_, in0=pb[:], scalar=1.0 / 128, in1=rs_, op0=ALU.mult, op1=ALU.subtract), [epb, ers], [ers])
            dve(lambda e: e.tensor_scalar(rs_, rs_, LN_EPS, None, ALU.add), [ers], [ers])
            act(lambda e: e.activation(out=rs_, in_=rs_, func=AF.Sqrt), [ers], [ers])
            dve(lambda e: e.reciprocal(rs_, rs_), [ers], [ers])
            dve(lambda e: e.tensor_tensor(y, y, mu, ALU.subtract), [ey, emu], [ey])
            dve(lambda e: e.scalar_tensor_tensor(out=y, in0=y, scalar=pc(RNW + hh), in1=rs_, op0=ALU.mult, op1=ALU.mult), [ey, ers, ec], [ey])
            act(lambda e: e.activation(out=P[13 + hh], in_=P[13 + hh], func=AF.Silu), [P.e[13 + hh]], [P.e[13 + hh]])
            dve(lambda e: e.tensor_tensor(y, y, P[13 + hh], ALU.mult), [ey, P.e[13 + hh]], [ey])
            S.dma(nextq(), yT[2, hh * 128:(hh + 1) * 128, tk], y, reads=[ey], writes=[eo])
        pb, epb = gp("B")
        pe(lambda e: e.matmul(pb[:], wal[:], P[21], start=True, stop=True), [ec, P.e[21]], [epb])
        al, eal = Lg[1], Lg.e[1]
        act(lambda e: e.activation(out=al, in_=pb[:], func=AF.Exp, scale=-1.0, bias=nbal), [epb, e_sm2], [eal])
        act(lambda e: e.activation(out=al, in_=al, func=AF.Ln, bias=1.0), [eal], [eal])
        act(lambda e: e.activation(out=al, in_=al, func=AF.Exp, scale=-1.0 / 16.0), [eal], [eal])
        dve(lambda e: e.tensor_scalar(P[15], P[15], float(128 ** -0.5), None, ALU.mult), [P.e[15]], [P.e[15]])
        def gla_out(e_, st, est):
            i, el = divmod(e_, 128)
            pe(lambda e: e.matmul(psY[i][:], wide[:, 127 - el:255 - el], st, start=(el == 0), stop=(el == 127)), [ec, est], [epsY[i]], chain=(el > 0))
        chan_scan(2, 256, P[16], P.e[16], P[15], P.e[15], lambda e_: (al, eal), lambda e_: (P[17 + e_ // 128], P.e[17 + e_ // 128], e_ % 128, None), gla_out)
        ys_ = []
        pa, epa = gp("A")
        for i in range(2):
            y, ey = gt(); sq, esq = gt()
            act(lambda e: e.activation(out=y, in_=psY[i][:], func=AF.Copy), [epsY[i]], [ey])
            act(lambda e: e.activation(out=sq, in_=y, func=AF.Square), [ey], [esq])
            pe(lambda e, i=i: e.matmul(pa[:], ones[:], sq, start=(i == 0), stop=(i == 1)), [ec, esq], [epa], chain=(i > 0))
            ys_.append((y, ey))
        rs_, ers = gt()
        dve(lambda e: e.tensor_scalar(rs_, pa[:], 1.0 / 256, RMS_EPS, ALU.mult, ALU.add), [epa], [ers])
        act(lambda e: e.activation(out=rs_, in_=rs_, func=AF.Sqrt), [ers], [ers])
        dve(lambda e: e.reciprocal(rs_, rs_), [ers], [ers])
        for i in range(2):
            y, ey = ys_[i]
            dve(lambda e: e.scalar_tensor_tensor(out=y, in0=y, scalar=pc(GNW + i), in1=rs_, op0=ALU.mult, op1=ALU.mult), [ey, ers, ec], [ey])
            act(lambda e: e.activation(out=P[19 + i], in_=P[19 + i], func=AF.Silu), [P.e[19 + i]], [P.e[19 + i]])
            dve(lambda e: e.tensor_tensor(y, y, P[19 + i], ALU.mult), [ey, P.e[19 + i]], [ey])
            S.dma(nextq(), yT[3, i * 128:(i + 1) * 128, tk], y, reads=[ey], writes=[eo])
        for hf in range(2):
         hs = slice(hf * TS, (hf + 1) * TS)
         for i in range(2):
             for jj in range(4):
                 j = 4 * i + jj
                 xr, exr = gt(); xi, exi = gt(); xr = xr[:, 0:TS]; xi = xi[:, 0:TS]
                 for ri, (xx, exx) in enumerate(((xr, exr), (xi, exi))):
                     pb, epb = gp("B")
                     pe(lambda e: e.matmul(pb[:, 0:TS], Bl[:, ri, j, :], P.t[:, 7 + i, hs], start=True, stop=True), [ec, P.e[7 + i]], [epb])
                     act(lambda e: e.activation(out=xx, in_=pb[:, 0:TS], func=AF.Copy), [epb], [exx])
                 wr, ewr = gt(); wi, ewi = gt(); t1, et1 = gt(); wr = wr[:, 0:TS]; wi = wi[:, 0:TS]; t1 = t1[:, 0:TS]
                 dve(lambda e: e.tensor_tensor(wr, xr, Tn[:, 0, j, :], ALU.mult), [exr, e_tab], [ewr])
                 pool(lambda e: e.tensor_tensor(t1, xi, Tn[:, 1, j, :], ALU.mult), [exi, e_tab], [et1])
                 dve(lambda e: e.tensor_tensor(wr, wr, t1, ALU.subtract), [ewr, et1], [ewr])
                 pool(lambda e: e.tensor_tensor(wi, xr, Tn[:, 1, j, :], ALU.mult), [exr, e_tab], [ewi])
                 dve(lambda e: e.tensor_tensor(t1, xi, Tn[:, 0, j, :], ALU.mult), [exi, e_tab, ewr], [et1])
                 dve(lambda e: e.tensor_tensor(wi, wi, t1, ALU.add), [ewi, et1], [ewi])
                 dve(lambda e: e.tensor_tensor_scan(wr, ones[:, 0:1].to_broadcast([128, TS]), wr, s5c[:, 0, j:j + 1], ALU.mult, ALU.add), [ec, ewr, e_s5c], [ewr])
                 dve(lambda e: e.tensor_tensor_scan(wi, ones[:, 0:1].to_broadcast([128, TS]), wi, s5c[:, 1, j:j + 1], ALU.mult, ALU.add), [ec, ewi, e_s5c], [ewi])
                 hr, ehr = xr, exr; hi, ehi = xi, exi
                 dve(lambda e: e.tensor_tensor(hr, wr, Tp[:, 0, j, :], ALU.mult), [ewr, e_tab], [ehr])
                 pool(lambda e: e.tensor_tensor(t1, wi, Tp[:, 1, j, :], ALU.mult), [ewi, e_tab], [et1])
                 dve(lambda e: e.tensor_tensor(hr, hr, t1, ALU.subtract), [ehr, et1], [ehr])
                 pool(lambda e: e.tensor_tensor(hi, wr, Tp[:, 1, j, :], ALU.mult), [ewr, e_tab], [ehi])
                 dve(lambda e: e.tensor_tensor(t1, wi, Tp[:, 0, j, :], ALU.mult), [ewi, e_tab, ehr], [et1])
                 dve(lambda e: e.tensor_tensor(hi, hi, t1, ALU.add), [ehi, et1], [ehi])
                 c1 = sm2[:, 48:49]
                 dve(lambda e: e.tensor_scalar(c1, hi[:, TS - 1:TS], lbi[:, j:j + 1], None, ALU.mult), [ehi, e_sm2], [e_sm2])
                 dve(lambda e: e.scalar_tensor_tensor(out=s5c[:, 0, j:j + 1], in0=hr[:, TS - 1:TS], scalar=lbr[:, j:j + 1], in1=c1, op0=ALU.mult, op1=ALU.subtract), [ehr, e_sm2], [e_s5c])
                 dve(lambda e: e.tensor_scalar(c1, hi[:, TS - 1:TS], lbr[:, j:j + 1], None, ALU.mult), [ehi, e_sm2], [e_sm2])
                 dve(lambda e: e.scalar_tensor_tensor(out=s5c[:, 1, j:j + 1], in0=hr[:, TS - 1:TS], scalar=lbi[:, j:j + 1], in1=c1, op0=ALU.mult, op1=ALU.add), [ehr, e_sm2], [e_s5c])
                 pe(lambda e: e.matmul(psY[0][:, 0:TS], Cl[:, 0, j, :], hr, start=(jj == 0), stop=(jj == 3)), [ec, ehr], [epsY[0]], chain=(jj > 0))
                 pe(lambda e: e.matmul(psZ[0][:, 0:TS], Cl[:, 1, j, :], hi, start=(jj == 0), stop=(jj == 3)), [ec, ehi], [epsZ[0]], chain=(jj > 0))
             y, ey = gt(); y = y[:, 0:TS]
             act(lambda e: e.activation(out=y, in_=psZ[0][:, 0:TS], func=AF.Copy), [epsZ[0]], [ey])
             dve(lambda e: e.tensor_tensor(y, psY[0][:, 0:TS], y, ALU.subtract), [epsY[0], ey], [ey])
             dve(lambda e: e.scalar_tensor_tensor(out=y, in0=P.t[:, 7 + i, hs], scalar=pc(S5D + i), in1=y, op0=ALU.mult, op1=ALU.add), [P.e[7 + i], ec, ey], [ey])
             act(lambda e: e.activation(out=y, in_=y, func=AF.Gelu), [ey], [ey])
             S.dma(nextq(), yT[1, i * 128:(i + 1) * 128, blk * T + hf * TS:blk * T + (hf + 1) * TS], y, reads=[ey], writes=[eo])

    S.wait_all("sp", [eo]); S.wait_all("pool", [eo])
    cx.st.close()
    return nc


OFF = dict(z=0, xs=1024, B=2048, C=2304, dt=2560, u=2576, rq=3600, rk=4112, rv=4624, rg=5648, gq=6672, gk=7184, gv=7696, gr=8720, gc=9744, gate=9760)


def _mixer_consts(T=512):
    c = {}
    c["c_ident"] = np.eye(128, dtype=np.float32); c["c_ones"] = np.ones((128, 128), np.float32)
    w = np.zeros((128, 255), np.float32); w[:, 127] = 1.0; c["c_wide"] = w
    sel = np.zeros((128, 2, 128), np.float32)
    for i in range(2):
        for m in range(128):
            sel[2 * i + m // 64, i, m] = 1.0
    c["c_sel"] = sel
    rot = np.zeros((128, 128), np.float32)
    for m in range(128):
        if m % 64 < 32:
            rot[m + 32, m] = -1.0
        else:
            rot[m - 32, m] = 1.0
    c["c_rot"] = rot
    c["c_tau"] = np.tile(np.arange(T, dtype=np.float32)[None, :], (128, 1))
    return c


def _mixer_inputs(inp, l, j, T=512):
    g = j // 2
    w_in = inp["w_in"][l]
    Wm = np.zeros((D, NWT * 128), np.float32)
    def put(t, c0, n):
        Wm[:, t * 128:t * 128 + n] = w_in[:, c0:c0 + n]
    put(0, OFF["z"] + 256 * j, 256); put(2, OFF["xs"] + 256 * j, 256); put(4, OFF["B"] + 128 * g, 128); put(5, OFF["C"] + 128 * g, 128)
    put(6, OFF["dt"] + 4 * j, 4); put(7, OFF["u"] + 256 * j, 256); put(9, OFF["rq"] + 128 * j, 128); put(10, OFF["rk"] + 128 * j, 128)
    put(11, OFF["rv"] + 256 * j, 256); put(13, OFF["rg"] + 256 * j, 256); put(15, OFF["gq"] + 128 * j, 128); put(16, OFF["gk"] + 128 * j, 128)
    put(17, OFF["gv"] + 256 * j, 256); put(19, OFF["gr"] + 256 * j, 256); put(21, OFF["gc"], 16)
    par = np.zeros((128, 64), np.float32)
    p = np.arange(128)
    cw, cb = inp["ssd_conv_w"][l], inp["ssd_conv_b"][l]
    chans = [256 * j + p, 256 * j + 128 + p, 1024 + 128 * g + p, 1280 + 128 * g + p]
    for ci, ch in enumerate(chans):
        for k in range(4):
            par[:, 4 * ci + k] = cw[k, ch]
        par[:, 16 + ci] = cb[ch]
    par[0:4, 20] = inp["ssd_dt_bias"][l][4 * j:4 * j + 4]; par[0:4, 21] = inp["ssd_a_log"][l][4 * j:4 * j + 4]
    for i in range(2):
        par[:, 22 + i] = inp["ssd_d"][l][4 * j + 2 * i + p // 64]
        par[:, 24 + i] = inp["s5_d"][l][256 * j + 128 * i + p]
        par[:, 36 + i] = inp["ret_norm_w"][l][(2 * j + i) * 128 + p]
        par[:, 41 + i] = inp["gla_norm_w"][l][256 * j + 128 * i + p]
    par[:, 26] = inp["s5_lambda_re"][l][p % 64]; par[:, 27] = inp["s5_lambda_im"][l][p % 64]
    for jt in range(8):
        par[:, 28 + jt] = inp["s5_log_dt"][l][16 * j + 2 * jt + p // 64]
    inv_freq = (1.0 / (np.float32(10000.0) ** (np.arange(32, dtype=np.float32) / np.float32(32)))).astype(np.float32)
    par[:, 38] = inv_freq[(p % 64) % 32]
    lg = np.log1p(-np.exp2(-5.0 - np.arange(8, dtype=np.float32))).astype(np.float32)
    par[:, 39] = lg[2 * j + p // 64]
    par[:, 40] = inp["gla_b_alpha"][l][128 * j + p]
    wal = np.zeros((128, 128), np.float32); wal[0:16] = inp["gla_w_alpha"][l][:, 128 * j:128 * j + 128]
    s5B = np.zeros((2, 128, 8, 128), np.float32); s5C = np.zeros((2, 128, 8, 128), np.float32)
    for ri, (bn, cn) in enumerate((("s5_b_re", "s5_c_re"), ("s5_b_im", "s5_c_im"))):
        bb, cc = inp[bn][l], inp[cn][l]
        for jt in range(8):
            for g2 in range(2):
                gl = 2 * jt + g2
                G = 16 * j + gl
                k0 = (gl % 8) * 16
                s5B[ri, k0:k0 + 16, jt, g2 * 64:(g2 + 1) * 64] = bb[G].T
                s5C[ri, g2 * 64:(g2 + 1) * 64, jt, k0:k0 + 16] = cc[G].T
    d = dict(Wm=Wm, cpar=par, w_alpha=wal, s5B=s5B, s5C=s5C)
    d.update(_mixer_consts(T))
    return d


_PROGS = {}


def _prog(kind, n):
    key = (kind, n)
    if key not in _PROGS:
        _PROGS[key] = build_mixer(n) if kind == "m" else build_rest(n)
    return _PROGS[key]


def kernel(**inp):
    inp = {k: np.asarray(v) for k, v in inp.items()}
    x = inp["x"]; Bn, Sn, _ = x.shape
    NQ = 4; NT = Sn // NQ
    hT = [np.ascontiguousarray(x[b].T) for b in range(Bn)]
    rconst = dict(c_ones=np.ones((128, 128), np.float32), c_ident=np.eye(128, dtype=np.float32), c_sel=np.zeros((32, 32 * 128), np.float32))
    for l in range(2):
        mi = [_mixer_inputs(inp, l, j) for j in range(4)]
        ins = []
        for c in range(8):
            b, j = divmod(c, 4)
            d = dict(mi[j]); d["hT"] = hT[b]; d["pos"] = np.ascontiguousarray(inp["positions"][b].astype(np.int32))
            ins.append(d)
        res = run_bass_kernel_spmd(_prog("m", Sn), ins, core_ids=list(range(8)))
        ym = [r["yT"] for r in res.results]
        W = dict(w_gate=np.ascontiguousarray(inp["w_in"][l][:, OFF["gate"]:]), w_branch=inp["w_branch"][l], w_out=inp["w_out"][l],
                 ssd_norm_w=inp["ssd_norm_w"][l], s5_w_glu=inp["s5_w_glu"][l], s5_b_glu=inp["s5_b_glu"][l],
                 ln1_w=inp["ln1_w"][l], ln1_b=inp["ln1_b"][l], ln2_w=inp["ln2_w"][l], ln2_b=inp["ln2_b"][l], ln3_w=inp["ln3_w"][l], ln3_b=inp["ln3_b"][l],
                 router_w=inp["router_w"][l], router_b=inp["router_b"][l], moe_w_gate_up=inp["moe_w_gate_up"][l], moe_b_gate_up=inp["moe_b_gate_up"][l],
                 moe_w_down=inp["moe_w_down"][l], moe_b_down=inp["moe_b_down"][l], ple_w_gate=inp["ple_w_gate"][l], ple_w_proj=inp["ple_w_proj"][l])
        ins = []
        for c in range(8):
            b, q = divmod(c, 4)
            ts = slice(q * NT, (q + 1) * NT)
            ysT = np.concatenate([ym[b * 4 + j][:, :, ts] for j in range(4)], axis=1)
            d = dict(hT=np.ascontiguousarray(hT[b][:, ts]), ysT=np.ascontiguousarray(ysT), pT=np.ascontiguousarray(inp["p"][l, b, ts].T))
            d.update(W); d.update(rconst)
            ins.append(d)
        res = run_bass_kernel_spmd(_prog("r", NT), ins, core_ids=list(range(8)))
        hT = [np.concatenate([res.results[b * 4 + q]["oT"] for q in range(4)], axis=1) for b in range(Bn)]
    return np.ascontiguousarray(np.stack([h.T for h in hT], 0)).astype(np.float32)
```

```python
import numpy as np
import concourse.bass as bass
import concourse.mybir as mybir

F32 = mybir.dt.float32
F32R = mybir.dt.float32r
BF16 = mybir.dt.bfloat16
I32 = mybir.dt.int32
AF = mybir.ActivationFunctionType
ALU = mybir.AluOpType
AX = mybir.AxisListType

SEM_ROLL = 12000


class Ent:
    __slots__ = ("name", "lastw", "reads", "dsem", "dcount")

    def __init__(self, name):
        self.name = name
        self.lastw = None
        self.reads = {}
        self.dsem = None
        self.dcount = 0


class Sched:
    def __init__(self, nc, stack):
        self.nc = nc
        self.stack = stack
        self.eng = {"pe": nc.tensor, "dve": nc.vector, "act": nc.scalar, "pool": nc.gpsimd, "sp": nc.sync}
        self.sems = {}
        self.cur = {}
        self.epoch = {e: 0 for e in self.eng}
        self.seen = {e: {} for e in self.eng}
        self.nsem = 0
        self.ninst = 0
        for e in self.eng:
            self._newsem(e)

    def _alloc(self, name):
        self.nsem += 1
        return self.stack.enter_context(self.nc.semaphore(name))

    def _newsem(self, e):
        key = f"{e}{self.epoch[e]}"
        self.epoch[e] += 1
        self.sems[key] = self._alloc("s_" + key)
        self.cur[e] = [key, 0]

    def ent(self, name):
        return Ent(name)

    def _deps(self, e, reads, writes):
        deps = {}
        def add(ev):
            if ev is None:
                return
            k, v = ev
            if deps.get(k, 0) < v:
                deps[k] = v
        for r in reads:
            add(r.lastw)
        for w in writes:
            add(w.lastw)
            for k, v in w.reads.items():
                add((k, v))
        return deps

    def _wait(self, e, deps, skip_self_pe=False):
        seen = self.seen[e]
        engine = self.eng[e]
        for k, v in deps.items():
            if skip_self_pe and k.startswith("pe") and e == "pe":
                continue
            if seen.get(k, 0) >= v:
                continue
            engine.wait_ge(self.sems[k], v)
            seen[k] = v

    def _mark(self, ev, reads, writes):
        k, v = ev
        for r in reads:
            if r.reads.get(k, 0) < v:
                r.reads[k] = v
        for w in writes:
            w.lastw = ev
            w.reads = {}

    def op(self, e, fn, reads=(), writes=(), pe_chain=False):
        deps = self._deps(e, reads, writes)
        self._wait(e, deps, skip_self_pe=pe_chain)
        inst = fn(self.eng[e])
        cur = self.cur[e]
        cur[1] += 1
        inst.then_inc(self.sems[cur[0]], 1)
        ev = (cur[0], cur[1])
        self._mark(ev, reads, writes)
        self.ninst += 1
        if cur[1] >= SEM_ROLL:
            self._newsem(e)
        return inst

    def dma(self, q, out, in_, reads=(), writes=(), sem_ent=None, **kw):
        deps = self._deps(q, reads, writes)
        self._wait(q, deps)
        se = sem_ent if sem_ent is not None else writes[0]
        if se.dsem is None:
            se.dsem = f"d{self.nsem}_{se.name}"
            self.sems[se.dsem] = self._alloc(se.dsem)
        se.dcount += 16
        self.eng[q].dma_start(out=out, in_=in_, **kw).then_inc(self.sems[se.dsem], 16)
        ev = (se.dsem, se.dcount)
        self._mark(ev, reads, writes)
        self.ninst += 1

    def wait_all(self, e, ents):
        deps = self._deps(e, ents, ents)
        self._wait(e, deps)


import contextlib
from concourse.bass_utils import run_bass_kernel_spmd

D = 2048
ALPHA = 4.0 ** 0.25
LN_EPS = 1e-5
RMS_EPS = 1e-6


class Ctx:
    def __init__(self):
        self.nc = bass.Bass("TRN2", target_bir_lowering=False)
        self.st = contextlib.ExitStack()
        self.S = Sched(self.nc, self.st)
        self.n = 0

    def din(self, name, shape, dt=F32):
        return self.nc.dram_tensor(name, list(shape), dt, kind="ExternalInput").ap()

    def dout(self, name, shape, dt=F32):
        return self.nc.dram_tensor(name, list(shape), dt, kind="ExternalOutput").ap()

    def sb(self, name, shape, dt=F32):
        t = self.st.enter_context(self.nc.sbuf_tensor(name, list(shape), dt))
        return t

    def ps(self, name, shape=(128, 512), dt=F32):
        return self.st.enter_context(self.nc.psum_tensor(name, list(shape), dt))

    def ent(self, name):
        return self.S.ent(name)


class Tiles:
    def __init__(self, cx, name, n, w, dt=F32):
        self.t = cx.sb(name, [128, n, w], dt)
        self.e = [cx.ent(f"{name}{i}") for i in range(n)]
        self.n = n

    def __getitem__(self, i):
        return self.t[:, i, :]


def build_rest(NT, T=512, NE=32):
    cx = Ctx(); nc = cx.nc; S = cx.S
    NB = NT // T
    hT = cx.din("hT", [D, NT]); ysT = cx.din("ysT", [4, 1024, NT]); pT = cx.din("pT", [256, NT])
    w_gate = cx.din("w_gate", [D, 4 * D]); w_branch = cx.din("w_branch", [4, 1024, D]); w_out = cx.din("w_out", [D, D])
    ssd_norm_w = cx.din("ssd_norm_w", [1024]); w_glu = cx.din("s5_w_glu", [1024, 1024]); b_glu = cx.din("s5_b_glu", [1024])
    lnw = [cx.din(f"ln{i}_w", [D]) for i in (1, 2, 3)]; lnb = [cx.din(f"ln{i}_b", [D]) for i in (1, 2, 3)]
    router_w = cx.din("router_w", [D, NE]); router_b = cx.din("router_b", [NE])
    w_gu = cx.din("moe_w_gate_up", [NE, D, D]); b_gu = cx.din("moe_b_gate_up", [NE, D])
    w_dn = cx.din("moe_w_down", [NE, 1024, D]); b_dn = cx.din("moe_b_down", [NE, D])
    ple_wg = cx.din("ple_w_gate", [D, D]); ple_wp = cx.din("ple_w_proj", [256, D])
    c_ones = cx.din("c_ones", [128, 128]); c_ident = cx.din("c_ident", [128, 128]); c_sel = cx.din("c_sel", [32, 32 * 128])
    oT = cx.dout("oT", [D, NT])
    eo = cx.ent("oT")

    ec = cx.ent("consts")
    ones = cx.sb("ones", [128, 128]); ident = cx.sb("ident", [128, 128])
    lnw_t = cx.sb("lnw_t", [128, 3, 16]); lnb_t = cx.sb("lnb_t", [128, 3, 16])
    snw_t = cx.sb("snw_t", [128, 8]); bglu_t = cx.sb("bglu_t", [128, 8])
    bgu_t = cx.sb("bgu_t", [128, NE, 16]); bdn_t = cx.sb("bdn_t", [NE, D])
    rw_t = cx.sb("rw_t", [128, 16, NE]); rb_t = cx.sb("rb_t", [128, NE])
    wp_t = [cx.sb(f"wp_t{i}", [128, 2, 128]) for i in range(2)]; ewp = [cx.ent(f"wp{i}") for i in range(2)]
    S.dma("sp", ones[:], c_ones, writes=[ec]); S.dma("sp", ident[:], c_ident, writes=[ec])
    for i in range(3):
        S.dma("sp", lnw_t[:, i, :], lnw[i].rearrange("(t p) -> p t", p=128), writes=[ec], allow_slow_non_contiguous=True)
        S.dma("sp", lnb_t[:, i, :], lnb[i].rearrange("(t p) -> p t", p=128), writes=[ec], allow_slow_non_contiguous=True)
    S.dma("sp", snw_t[:], ssd_norm_w.rearrange("(t p) -> p t", p=128), writes=[ec], allow_slow_non_contiguous=True)
    S.dma("sp", bglu_t[:], b_glu.rearrange("(t p) -> p t", p=128), writes=[ec], allow_slow_non_contiguous=True)
    for e8 in range(NE // 8):
        S.dma("sp", bgu_t[:, e8 * 8:(e8 + 1) * 8, :], b_gu[e8 * 8:(e8 + 1) * 8].rearrange("e (t p) -> p e t", p=128), writes=[ec], allow_slow_non_contiguous=True)
    S.dma("sp", bdn_t[:], b_dn, writes=[ec])
    S.dma("sp", rw_t[:], router_w.rearrange("(k p) e -> p k e", p=128), writes=[ec])
    S.dma("sp", rb_t[:], router_b.partition_broadcast(128), writes=[ec])

    h = Tiles(cx, "h", 16, T); u = Tiles(cx, "u", 16, T); mg = Tiles(cx, "mg", 16, T)
    hb = Tiles(cx, "hb", 16, T, BF16); mgb = Tiles(cx, "mgb", 16, T, BF16)
    ysA = Tiles(cx, "ysA", 8, T, BF16)
    ppt = Tiles(cx, "ppt", 2, T)
    tmp = Tiles(cx, "tmp", 4, T)
    mean = cx.sb("mean", [128, T]); e_mean = cx.ent("mean")
    rstd = cx.sb("rstd", [128, T]); e_rstd = cx.ent("rstd")
    gT = cx.sb("gT", [NE, T]); e_gT = cx.ent("gT")
    gbc = mean; e_gbc = e_mean
    rs = cx.sb("rs", [128, 160]); e_rs = cx.ent("rs")
    NWA = 2
    wa = [cx.sb(f"wa{i}", [128, 16, 128]) for i in range(NWA)]; ewa = [cx.ent(f"wa{i}") for i in range(NWA)]
    wb = [cx.sb(f"wb{i}", [128, 8, 128]) for i in range(NWA)]; ewb = [cx.ent(f"wb{i}") for i in range(NWA)]
    wab = [cx.sb(f"wab{i}", [128, 16, 128], BF16) for i in range(NWA)]; ewab = [cx.ent(f"wab{i}") for i in range(NWA)]
    wbb = [cx.sb(f"wbb{i}", [128, 8, 128], BF16) for i in range(NWA)]; ewbb = [cx.ent(f"wbb{i}") for i in range(NWA)]
    cnt_c = [0]
    def cast(dst, edst, src, esrc):
        cnt_c[0] += 1
        if cnt_c[0] % 2:
            S.op("pool", lambda e: e.tensor_copy(dst, src), reads=[esrc], writes=[edst])
        else:
            S.op("act", lambda e: e.activation(out=dst, in_=src, func=AF.Copy), reads=[esrc], writes=[edst])
    cnt = {"wa": 0, "wb": 0, "tmp": 0, "ps": 0, "q": 0}
    psA = [cx.ps(f"psA{i}") for i in range(3)]; epsA = [cx.ent(f"psA{i}") for i in range(3)]
    psB = [cx.ps(f"psB{i}") for i in range(3)]; epsB = [cx.ent(f"psB{i}") for i in range(3)]
    psS = cx.ps("psS"); epsS = cx.ent("psS")
    psQ = cx.ps("psQ"); epsQ = cx.ent("psQ")
    pc = {"A": 0, "B": 0}

    def nextq():
        return "sp"

    def load_wa(src2d, c0):
        i = cnt["wa"] % NWA; cnt["wa"] += 1
        S.dma(nextq(), wa[i][:], src2d[:, c0:c0 + 128].rearrange("(k p) c -> p k c", p=128), writes=[ewa[i]])
        cast(wab[i][:], ewab[i], wa[i][:], ewa[i])
        return wab[i], ewab[i]

    def load_wb(src2d, c0):
        i = cnt["wb"] % NWA; cnt["wb"] += 1
        S.dma(nextq(), wb[i][:], src2d[:, c0:c0 + 128].rearrange("(k p) c -> p k c", p=128), writes=[ewb[i]])
        cast(wbb[i][:], ewbb[i], wb[i][:], ewb[i])
        return wbb[i], ewbb[i]

    def getps(kind):
        lst, el = (psA, epsA) if kind == "A" else (psB, epsB)
        i = pc[kind] % 3; pc[kind] += 1
        return lst[i], el[i]

    def gettmp():
        i = cnt["tmp"] % 4; cnt["tmp"] += 1
        return tmp[i], tmp.e[i]

    def mm_acc(ps, eps, w, ew, acts, nk):
        for k in range(nk):
            S.op("pe", lambda e, k=k: e.matmul(ps[:], w[:, k, :], acts[k], start=(k == 0), stop=(k == nk - 1)),
                 reads=[ew, acts.e[k]], writes=[eps], pe_chain=(k > 0))

    def stats(src, idxs, nfeat, eps_val):
        n = len(idxs)
        for j, k in enumerate(idxs):
            S.op("pe", lambda e, k=k, j=j: e.matmul(psS[:], ones[:], src[k], start=(j == 0), stop=(j == n - 1)),
                 reads=[ec, src.e[k]], writes=[epsS], pe_chain=(j > 0))
        for j, k in enumerate(idxs):
            t, et = gettmp()
            S.op("act", lambda e, k=k, t=t: e.activation(out=t, in_=src[k], func=AF.Square), reads=[src.e[k]], writes=[et])
            S.op("pe", lambda e, t=t, j=j: e.matmul(psQ[:], ones[:], t, start=(j == 0), stop=(j == n - 1)),
                 reads=[ec, et], writes=[epsQ], pe_chain=(j > 0))
        return n

    def layernorm(src, dst, li, dstb=None):
        stats(src, list(range(16)), D, LN_EPS)
        S.op("act", lambda e: e.activation(out=mean[:], in_=psS[:], func=AF.Copy, scale=1.0 / D), reads=[epsS], writes=[e_mean])
        t, et = gettmp()
        S.op("dve", lambda e: e.tensor_tensor(t, mean[:], mean[:], ALU.mult), reads=[e_mean], writes=[et])
        S.op("dve", lambda e: e.scalar_tensor_tensor(out=rstd[:], in0=psQ[:], scalar=1.0 / D, in1=t, op0=ALU.mult, op1=ALU.subtract),
             reads=[epsQ, et], writes=[e_rstd])
        S.op("dve", lambda e: e.tensor_scalar(rstd[:], rstd[:], LN_EPS, None, ALU.add), reads=[e_rstd], writes=[e_rstd])
        S.op("act", lambda e: e.activation(out=rstd[:], in_=rstd[:], func=AF.Sqrt), reads=[e_rstd], writes=[e_rstd])
        S.op("dve", lambda e: e.reciprocal(rstd[:], rstd[:]), reads=[e_rstd], writes=[e_rstd])
        for k in range(16):
            t, et = gettmp()
            S.op("dve", lambda e, k=k, t=t: e.tensor_tensor(t, src[k], mean[:], ALU.subtract), reads=[src.e[k], e_mean], writes=[et])
            S.op("pool", lambda e, t=t: e.tensor_tensor(t, t, rstd[:], ALU.mult), reads=[et, e_rstd], writes=[et])
            S.op("act", lambda e, k=k, t=t: e.activation(out=dst[k], in_=t, func=AF.Identity, scale=lnw_t[:, li, k:k + 1], bias=lnb_t[:, li, k:k + 1]),
                 reads=[et, ec], writes=[dst.e[k]])
            if dstb is not None:
                S.op("pool", lambda e, k=k: e.tensor_copy(dstb[k], dst[k]), reads=[dst.e[k]], writes=[dstb.e[k]])

    for blk in range(NB):
        tk = slice(blk * T, (blk + 1) * T)
        for k in range(16):
            S.dma(nextq(), h[k], hT[k * 128:(k + 1) * 128, tk], writes=[h.e[k]])
            cast(hb[k], hb.e[k], h[k], h.e[k])
        for k in range(2):
            S.dma(nextq(), ppt[k], pT[k * 128:(k + 1) * 128, tk], writes=[ppt.e[k]])
        for n in range(4):
            for k in range(8):
                S.dma(nextq(), u[k], ysT[n, k * 128:(k + 1) * 128, tk], writes=[u.e[k]])
            if n == 0:
                for g in range(2):
                    stats(u, [4 * g + i for i in range(4)], 512, RMS_EPS)
                    S.op("dve", lambda e: e.tensor_scalar(rstd[:], psQ[:], 1.0 / 512, RMS_EPS, ALU.mult, ALU.add), reads=[epsQ], writes=[e_rstd])
                    S.op("act", lambda e: e.activation(out=rstd[:], in_=rstd[:], func=AF.Sqrt), reads=[e_rstd], writes=[e_rstd])
                    S.op("dve", lambda e: e.reciprocal(rstd[:], rstd[:]), reads=[e_rstd], writes=[e_rstd])
                    for i in range(4):
                        k = 4 * g + i
                        S.op("dve", lambda e, k=k: e.scalar_tensor_tensor(out=ysA[k], in0=u[k], scalar=snw_t[:, k:k + 1], in1=rstd[:], op0=ALU.mult, op1=ALU.mult),
                             reads=[u.e[k], e_rstd, ec], writes=[ysA.e[k]])
            elif n == 1:
                for k in range(8):
                    cast(mgb[k], mgb.e[k], u[k], u.e[k])
                for m in range(8):
                    w, ew = load_wb(w_glu, m * 128)
                    ps, eps = getps("A")
                    mm_acc(ps, eps, w, ew, mgb, 8)
                    t, et = gettmp()
                    S.op("act", lambda e, t=t, ps=ps, m=m: e.activation(out=t, in_=ps[:], func=AF.Sigmoid, bias=bglu_t[:, m:m + 1]), reads=[eps, ec], writes=[et])
                    S.op("dve", lambda e, t=t, m=m: e.tensor_tensor(ysA[m], u[m], t, ALU.mult), reads=[u.e[m], et], writes=[ysA.e[m]])
            else:
                for k in range(8):
                    cast(ysA[k], ysA.e[k], u[k], u.e[k])
            ysrc = ysA
            for m in range(16):
                w, ew = load_wa(w_gate, n * D + m * 128)
                psg, epsg = getps("A")
                mm_acc(psg, epsg, w, ew, hb, 16)
                w2, ew2 = load_wb(w_branch[n], m * 128)
                psb, epsb = getps("B")
                mm_acc(psb, epsb, w2, ew2, ysrc, 8)
                t, et = gettmp()
                S.op("act", lambda e, t=t, psg=psg: e.activation(out=t, in_=psg[:], func=AF.Sigmoid), reads=[epsg], writes=[et])
                if n == 0:
                    S.op("dve", lambda e, t=t, psb=psb, m=m: e.tensor_tensor(mg[m], t, psb[:], ALU.mult), reads=[et, epsb], writes=[mg.e[m]])
                else:
                    S.op("dve", lambda e, t=t, psb=psb: e.tensor_tensor(t, t, psb[:], ALU.mult), reads=[et, epsb], writes=[et])
                    S.op("pool", lambda e, t=t, m=m: e.tensor_tensor(mg[m], mg[m], t, ALU.add), reads=[et, mg.e[m]], writes=[mg.e[m]])
        for m in range(16):
            cast(mgb[m], mgb.e[m], mg[m], mg.e[m])
        for m in range(16):
            w, ew = load_wa(w_out, m * 128)
            ps, eps = getps("A")
            mm_acc(ps, eps, w, ew, mgb, 16)
            S.op("dve", lambda e, ps=ps, m=m: e.scalar_tensor_tensor(out=u[m], in0=h[m], scalar=ALPHA, in1=ps[:], op0=ALU.mult, op1=ALU.add),
                 reads=[h.e[m], eps], writes=[u.e[m]])
        layernorm(u, h, 0, hb)
        for tt in range(T // 128):
            ts_ = slice(tt * 128, (tt + 1) * 128)
            ps, eps = getps("B")
            for k in range(16):
                S.op("pe", lambda e, k=k, ps=ps: e.matmul(ps[:, 0:NE], h.t[:, k, ts_], rw_t[:, k, :], start=(k == 0), stop=(k == 15)),
                     reads=[h.e[k], ec], writes=[eps], pe_chain=(k > 0))
            lg = rs[:, 0:NE]; m8 = rs[:, 32:40]; msk = rs[:, 40:40 + NE]; ex = rs[:, 72:72 + NE]; sm = rs[:, 104:105]; nmx = rs[:, 105:106]; gw = rs[:, 112:112 + NE]
            S.op("dve", lambda e, ps=ps: e.tensor_tensor(lg, ps[:, 0:NE], rb_t[:], ALU.add), reads=[eps, ec], writes=[e_rs])
            S.op("dve", lambda e: e.max(m8, lg), reads=[e_rs], writes=[e_rs])
            S.op("dve", lambda e: e.tensor_scalar(msk, lg, rs[:, 35:36], None, ALU.is_ge), reads=[e_rs], writes=[e_rs])
            S.op("dve", lambda e: e.tensor_scalar(nmx, rs[:, 32:33], -1.0, None, ALU.mult), reads=[e_rs], writes=[e_rs])
            S.op("act", lambda e: e.activation(out=ex, in_=lg, func=AF.Exp, bias=nmx), reads=[e_rs], writes=[e_rs])
            S.op("dve", lambda e: e.tensor_tensor(ex, ex, msk, ALU.mult), reads=[e_rs], writes=[e_rs])
            S.op("dve", lambda e: e.reduce_sum(sm, ex, AX.X), reads=[e_rs], writes=[e_rs])
            S.op("dve", lambda e: e.reciprocal(sm, sm), reads=[e_rs], writes=[e_rs])
            S.op("dve", lambda e: e.tensor_scalar(gw, ex, sm, None, ALU.mult), reads=[e_rs], writes=[e_rs])
            ps2, eps2 = getps("B")
            S.op("pe", lambda e, ps2=ps2: e.transpose(ps2[0:NE, 0:128], gw, ident[:]), reads=[e_rs, ec], writes=[eps2])
            S.op("act", lambda e, ps2=ps2: e.activation(out=gT[:, ts_], in_=ps2[0:NE, 0:128], func=AF.Copy), reads=[eps2], writes=[e_gT])
        for m in range(16):
            ps, eps = getps("A")
            S.op("pe", lambda e, ps=ps, m=m: e.matmul(ps[:], bdn_t[:, m * 128:(m + 1) * 128], gT[:], start=True, stop=True), reads=[ec, e_gT], writes=[eps])
            S.op("act", lambda e, ps=ps, m=m: e.activation(out=mg[m], in_=ps[:], func=AF.Copy), reads=[eps], writes=[mg.e[m]])
        for ex_i in range(NE):
            ps, eps = getps("B")
            S.op("pe", lambda e, ps=ps: e.matmul(ps[:], ident[0:NE, ex_i:ex_i + 1].to_broadcast([NE, 128]), gT[:], start=True, stop=True), reads=[ec, e_gT], writes=[eps])
            S.op("act", lambda e, ps=ps: e.activation(out=gbc[:], in_=ps[:], func=AF.Copy), reads=[eps], writes=[e_gbc])
            for f in range(8):
                w, ew = load_wa(w_gu[ex_i], f * 128)
                psg, epsg = getps("A")
                mm_acc(psg, epsg, w, ew, hb, 16)
                w2, ew2 = load_wa(w_gu[ex_i], 1024 + f * 128)
                psu, epsu = getps("B")
                mm_acc(psu, epsu, w2, ew2, hb, 16)
                gc, egc = gettmp(); sg, esg = gettmp(); uc, euc = gettmp()
                S.op("dve", lambda e: e.tensor_scalar(gc, psg[:], bgu_t[:, ex_i, f:f + 1], 7.0, ALU.add, ALU.min), reads=[epsg, ec], writes=[egc])
                S.op("act", lambda e: e.activation(out=sg, in_=gc, func=AF.Sigmoid, scale=1.702), reads=[egc], writes=[esg])
                S.op("dve", lambda e: e.tensor_scalar(uc, psu[:], bgu_t[:, ex_i, 8 + f:9 + f], 7.0, ALU.add, ALU.min), reads=[epsu, ec], writes=[euc])
                S.op("dve", lambda e: e.tensor_scalar(uc, uc, -7.0, 1.0, ALU.max, ALU.add), reads=[euc], writes=[euc])
                S.op("pool", lambda e: e.tensor_tensor(gc, gc, sg, ALU.mult), reads=[egc, esg], writes=[egc])
                S.op("pool", lambda e: e.tensor_tensor(uc, uc, gc, ALU.mult), reads=[egc, euc], writes=[euc])
                S.op("pool", lambda e: e.tensor_tensor(ysA[f], uc, gbc[:], ALU.mult), reads=[euc, e_gbc], writes=[ysA.e[f]])
            for m in range(16):
                w, ew = load_wb(w_dn[ex_i], m * 128)
                ps, eps = getps("A")
                mm_acc(ps, eps, w, ew, ysA, 8)
                S.op("dve", lambda e, ps=ps, m=m: e.tensor_tensor(mg[m], mg[m], ps[:], ALU.add), reads=[mg.e[m], eps], writes=[mg.e[m]])
        for m in range(16):
            S.op("dve", lambda e, m=m: e.scalar_tensor_tensor(out=u[m], in0=h[m], scalar=ALPHA, in1=mg[m], op0=ALU.mult, op1=ALU.add),
                 reads=[h.e[m], mg.e[m]], writes=[u.e[m]])
        layernorm(u, h, 1, hb)
        for m in range(16):
            w, ew = load_wa(ple_wg, m * 128)
            psg, epsg = getps("A")
            mm_acc(psg, epsg, w, ew, hb, 16)
            psb, epsb = getps("B")
            wpt, ewpt = wp_t[m % 2], ewp[m % 2]
            S.dma(nextq(), wpt[:], ple_wp[:, m * 128:(m + 1) * 128].rearrange("(k p) d -> p k d", p=128), writes=[ewpt])
            for k in range(2):
                S.op("pe", lambda e, k=k, psb=psb, wpt=wpt: e.matmul(psb[:], wpt[:, k, :], ppt[k], start=(k == 0), stop=(k == 1)),
                     reads=[ewpt, ppt.e[k]], writes=[epsb], pe_chain=(k > 0))
            t, et = gettmp()
            S.op("act", lambda e, t=t, psg=psg: e.activation(out=t, in_=psg[:], func=AF.Sigmoid), reads=[epsg], writes=[et])
            S.op("dve", lambda e, t=t, psb=psb: e.tensor_tensor(t, t, psb[:], ALU.mult), reads=[et, epsb], writes=[et])
            S.op("dve", lambda e, t=t, m=m: e.scalar_tensor_tensor(out=u[m], in0=h[m], scalar=ALPHA, in1=t, op0=ALU.mult, op1=ALU.add),
                 reads=[h.e[m], et], writes=[u.e[m]])
        layernorm(u, mg, 2)
        for k in range(16):
            S.dma(nextq(), oT[k * 128:(k + 1) * 128, tk], mg[k], reads=[mg.e[k]], writes=[eo])
    S.wait_all("sp", [eo])
    cx.st.close()
    return nc


NWT = 22
TWO_PI = 2.0 * np.pi


def build_mixer(S_len, T=512):
    cx = Ctx(); nc = cx.nc; S = cx.S
    NB = S_len // T
    hT = cx.din("hT", [D, S_len]); Wm = cx.din("Wm", [D, NWT * 128]); pos = cx.din("pos", [S_len], I32)
    cpar = cx.din("cpar", [128, 64])
    c_ident = cx.din("c_ident", [128, 128]); c_ones = cx.din("c_ones", [128, 128]); c_wide = cx.din("c_wide", [128, 255])
    c_sel = cx.din("c_sel", [128, 2, 128]); c_rot = cx.din("c_rot", [128, 128]); c_tau = cx.din("c_tau", [128, T])
    w_alpha = cx.din("w_alpha", [128, 128])
    s5B = cx.din("s5B", [2, 128, 8, 128]); s5C = cx.din("s5C", [2, 128, 8, 128])
    yT = cx.dout("yT", [4, 256, S_len]); eo = cx.ent("yT")
    ec = cx.ent("consts")
    par = cx.sb("par", [128, 64]); ident = cx.sb("ident", [128, 128]); ones = cx.sb("ones", [128, 128]); wide = cx.sb("wide", [128, 255])
    sel = cx.sb("sel", [128, 2, 128]); rot = cx.sb("rot", [128, 128]); tau = cx.sb("tau", [128, T]); wal = cx.sb("wal", [128, 128])
    Bl = cx.sb("Bl", [128, 2, 8, 128]); Cl = cx.sb("Cl", [128, 2, 8, 128])
    for dst, src in ((par, cpar), (ident, c_ident), (ones, c_ones), (wide, c_wide), (sel, c_sel), (rot, c_rot), (tau, c_tau), (wal, w_alpha)):
        S.dma("sp", dst[:], src, writes=[ec])
    for ri in range(2):
        S.dma("sp", Bl[:, ri], s5B[ri], writes=[ec]); S.dma("sp", Cl[:, ri], s5C[ri], writes=[ec])
    CW = 0
    CB = 16
    DTB, ALOG, DSK = 20, 21, 22
    S5D = 24
    LRE, LIM = 26, 27
    LDT = 28
    RNW = 36
    INVF = 38
    LGAM = 39
    BAL = 40
    GNW = 41
    def pc(c):
        return par[:, c:c + 1]

    P = Tiles(cx, "P", NWT, T)
    xe = cx.sb("xe", [128, 4, T + 3]); e_xe = [cx.ent(f"xe{i}") for i in range(4)]
    hb = cx.sb("hb", [128, 16, T]); e_hb = cx.ent("hb")
    wa = [cx.sb(f"mwa{i}", [128, 16, 128]) for i in range(2)]; ewa = [cx.ent(f"mwa{i}") for i in range(2)]
    tmp = Tiles(cx, "mt", 12, T)
    Lg = Tiles(cx, "lg", 7, T)
    cnt = {"t": 0, "q": 0, "pa": 0, "pb": 0}
    psA = [cx.ps(f"mpA{i}") for i in range(2)]; epsA = [cx.ent(f"mpA{i}") for i in range(2)]
    psB = [cx.ps(f"mpB{i}") for i in range(2)]; epsB = [cx.ent(f"mpB{i}") for i in range(2)]
    psY = [cx.ps(f"mpY{i}") for i in range(2)]; epsY = [cx.ent(f"mpY{i}") for i in range(2)]
    psZ = [cx.ps(f"mpZ{i}") for i in range(2)]; epsZ = [cx.ent(f"mpZ{i}") for i in range(2)]
    carry = cx.sb("carry", [128, 3, 256]); e_carry = [[cx.ent(f"carry{i}_{c}") for c in range(256)] for i in range(3)]
    s5c = cx.sb("s5c", [128, 2, 8]); e_s5c = cx.ent("s5c")
    posf = cx.sb("posf", [128, T]); posi = cx.sb("posi", [128, T], I32); e_pos = cx.ent("pos")
    kint = cx.sb("kint", [128, T], I32); e_kint = cx.ent("kint")
    TS = T // 2
    Tp = cx.sb("Tp", [128, 2, 8, TS]); Tn = cx.sb("Tn", [128, 2, 8, TS]); e_tab = cx.ent("tab")
    sm = cx.sb("sm", [128, 64]); e_sm = cx.ent("sm")

    def gt():
        i = cnt["t"] % 12; cnt["t"] += 1
        return tmp[i], tmp.e[i]

    def gp(kind):
        lst, el = (psA, epsA) if kind == "A" else (psB, epsB)
        key = "pa" if kind == "A" else "pb"
        i = cnt[key] % 2; cnt[key] += 1
        return lst[i], el[i]

    def nextq():
        cnt["q"] += 1
        return "sp" if cnt["q"] % 2 else "pool"

    def dve(fn, r, w): S.op("dve", fn, reads=r, writes=w)
    def act(fn, r, w): S.op("act", fn, reads=r, writes=w)
    def pool(fn, r, w): S.op("pool", fn, reads=r, writes=w)
    def pe(fn, r, w, chain=False): S.op("pe", fn, reads=r, writes=w, pe_chain=chain)

    def sincos(ang, eang, out_sin, eout, shift):
        t, et = gt()
        dve(lambda e: e.tensor_scalar(t, ang, shift, 1.0 / TWO_PI, ALU.add, ALU.mult), [eang], [et])
        dve(lambda e: e.tensor_copy(kint[:], t), [et], [e_kint])
        dve(lambda e: e.tensor_copy(t, kint[:]), [e_kint], [et])
        t2, et2 = gt()
        dve(lambda e: e.tensor_scalar(t2, ang, shift, None, ALU.add), [eang], [et2])
        dve(lambda e: e.scalar_tensor_tensor(out=t2, in0=t, scalar=-6.28125, in1=t2, op0=ALU.mult, op1=ALU.add), [et2, et], [et2])
        dve(lambda e: e.scalar_tensor_tensor(out=t2, in0=t, scalar=-1.9353071795864769e-3, in1=t2, op0=ALU.mult, op1=ALU.add), [et2, et], [et2])
        dve(lambda e: e.tensor_scalar(t, t2, float(np.pi), -TWO_PI, ALU.is_gt, ALU.mult), [et2], [et])
        t3, et3 = gt()
        dve(lambda e: e.tensor_scalar(t3, t2, -float(np.pi), TWO_PI, ALU.is_lt, ALU.mult), [et2], [et3])
        dve(lambda e: e.tensor_tensor(t2, t2, t, ALU.add), [et2, et], [et2])
        dve(lambda e: e.tensor_tensor(t2, t2, t3, ALU.add), [et2, et3], [et2])
        dve(lambda e: e.tensor_scalar(t2, t2, float(np.pi), -float(np.pi), ALU.min, ALU.max), [et2], [et2])
        act(lambda e: e.activation(out=out_sin, in_=t2, func=AF.Sin), [et2], [eout])

    S.wait_all("dve", [ec]); S.wait_all("act", [ec])
    lr = sm[:, 0:1]; dtc = sm[:, 8:16]; acol = sm[:, 16:24]; thcol = sm[:, 24:32]
    dve(lambda e: e.tensor_scalar(lr, pc(LRE), -1e-4, None, ALU.min), [ec], [e_sm])
    act(lambda e: e.activation(out=dtc, in_=par[:, LDT:LDT + 8], func=AF.Exp), [ec], [e_sm])
    dve(lambda e: e.tensor_scalar(acol, dtc, lr, None, ALU.mult), [e_sm], [e_sm])
    dve(lambda e: e.tensor_scalar(thcol, dtc, pc(LIM), None, ALU.mult), [e_sm, ec], [e_sm])
    nacol = sm[:, 32:40]
    dve(lambda e: e.tensor_scalar(nacol, acol, -1.0, None, ALU.mult), [e_sm], [e_sm])
    for j in range(8):
        ang, eang = gt()
        dve(lambda e: e.tensor_scalar(ang, tau[:], thcol[:, j:j + 1], None, ALU.mult), [ec, e_sm], [eang])
        sn, esn = gt(); cs, ecs = gt(); mg_, emg = gt(); mn, emn = gt()
        sincos(ang, eang, sn, esn, 0.0)
        sincos(ang, eang, cs, ecs, float(np.pi / 2))
        act(lambda e: e.activation(out=mg_, in_=tau[:], func=AF.Exp, scale=acol[:, j:j + 1]), [ec, e_sm], [emg])
        act(lambda e: e.activation(out=mn, in_=tau[:], func=AF.Exp, scale=nacol[:, j:j + 1]), [ec, e_sm], [emn])
        dve(lambda e: e.tensor_tensor(Tp[:, 0, j, :], mg_[:, 0:TS], cs[:, 0:TS], ALU.mult), [emg, ecs], [e_tab])
        dve(lambda e: e.tensor_tensor(Tp[:, 1, j, :], mg_[:, 0:TS], sn[:, 0:TS], ALU.mult), [emg, esn], [e_tab])
        dve(lambda e: e.tensor_tensor(Tn[:, 0, j, :], mn[:, 0:TS], cs[:, 0:TS], ALU.mult), [emn, ecs], [e_tab])
        dve(lambda e: e.scalar_tensor_tensor(out=Tn[:, 1, j, :], in0=mn[:, 0:TS], scalar=-1.0, in1=sn[:, 0:TS], op0=ALU.mult, op1=ALU.mult), [emn, esn], [e_tab])
    abr = sm[:, 40:48]; abi = sm[:, 48:56]; fre = sm[:, 56:64]
    sm2 = cx.sb("sm2", [128, 64]); e_sm2 = cx.ent("sm2")
    fim = sm2[:, 0:8]; den = sm2[:, 8:9]; t8 = sm2[:, 16:24]; lbr = sm2[:, 24:32]; lbi = sm2[:, 32:40]
    dve(lambda e: e.tensor_copy(lbr, Tp[:, 0, :, 1]), [e_tab], [e_sm2])
    dve(lambda e: e.tensor_copy(lbi, Tp[:, 1, :, 1]), [e_tab], [e_sm2])
    dve(lambda e: e.tensor_scalar(abr, lbr, -1.0, None, ALU.add), [e_sm2], [e_sm])
    dve(lambda e: e.tensor_tensor(den, lr, lr, ALU.mult), [e_sm], [e_sm2])
    dve(lambda e: e.scalar_tensor_tensor(out=den, in0=pc(LIM), scalar=pc(LIM), in1=den, op0=ALU.mult, op1=ALU.add), [ec, e_sm2], [e_sm2])
    dve(lambda e: e.reciprocal(den, den), [e_sm2], [e_sm2])
    dve(lambda e: e.tensor_scalar(t8, lbi, pc(LIM), None, ALU.mult), [e_sm2, ec], [e_sm2])
    dve(lambda e: e.scalar_tensor_tensor(out=fre, in0=abr, scalar=lr, in1=t8, op0=ALU.mult, op1=ALU.add), [e_sm, e_sm2], [e_sm])
    dve(lambda e: e.tensor_scalar(fre, fre, den, None, ALU.mult), [e_sm, e_sm2], [e_sm])
    dve(lambda e: e.tensor_scalar(t8, abr, pc(LIM), None, ALU.mult), [e_sm, ec], [e_sm2])
    dve(lambda e: e.scalar_tensor_tensor(out=fim, in0=lbi, scalar=lr, in1=t8, op0=ALU.mult, op1=ALU.subtract), [e_sm, e_sm2], [e_sm2])
    dve(lambda e: e.tensor_scalar(fim, fim, den, None, ALU.mult), [e_sm2], [e_sm2])
    for j in range(8):
        a_, ea = gt(); b_, eb = gt(); a_ = a_[:, 0:TS]; b_ = b_[:, 0:TS]
        dve(lambda e: e.tensor_scalar(a_, Tn[:, 0, j, :], fre[:, j:j + 1], None, ALU.mult), [e_tab, e_sm], [ea])
        dve(lambda e: e.tensor_scalar(b_, Tn[:, 1, j, :], fre[:, j:j + 1], None, ALU.mult), [e_tab, e_sm], [eb])
        dve(lambda e: e.scalar_tensor_tensor(out=b_, in0=Tn[:, 0, j, :], scalar=fim[:, j:j + 1], in1=b_, op0=ALU.mult, op1=ALU.add), [e_tab, e_sm2, eb], [eb])
        dve(lambda e: e.scalar_tensor_tensor(out=t8[:, 0:1].to_broadcast([128, 1]) if False else a_, in0=Tn[:, 1, j, :], scalar=fim[:, j:j + 1], in1=a_, op0=ALU.mult, op1=ALU.subtract), [e_tab, e_sm2, ea], [ea])
        dve(lambda e: e.tensor_scalar(Tn[:, 0, j, :], a_, -1.0, None, ALU.mult), [ea], [e_tab])
        dve(lambda e: e.tensor_copy(Tn[:, 1, j, :], b_), [eb], [e_tab])
    for i in range(3):
        dve(lambda e, i=i: e.memset(carry[:, i, :], 0.0), [], e_carry[i])
    dve(lambda e: e.memset(s5c[:], 0.0), [], [e_s5c])
    for i in range(4):
        dve(lambda e, i=i: e.memset(xe[:, i, 0:3], 0.0), [], [e_xe[i]])
    nbal = sm2[:, 40:41]; nalog = sm2[:, 41:42]
    dve(lambda e: e.tensor_scalar(nbal, pc(BAL), -1.0, None, ALU.mult), [ec], [e_sm2])
    act(lambda e: e.activation(out=nalog, in_=pc(ALOG), func=AF.Exp), [ec], [e_sm2])
    dve(lambda e: e.tensor_scalar(nalog, nalog, -1.0, None, ALU.mult), [e_sm2], [e_sm2])
    gam = sm2[:, 42:43]
    act(lambda e: e.activation(out=gam, in_=pc(LGAM), func=AF.Exp), [ec], [e_sm2])

    def chan_scan(mix, nch, k_ap, ek, q_ap, eq, a_of, v_of, out_of):
        for e_ in range(nch):
            vt, evt, row, halves = v_of(e_)
            pb, epb = gp("B")
            if halves is None:
                pe(lambda e: e.matmul(pb[:], ident[:, row:row + 1].to_broadcast([128, 128]), vt, start=True, stop=True), [ec, evt], [epb])
            else:
                (v0, ev0), (v1, ev1) = halves
                pe(lambda e: e.matmul(pb[0:64, :], ident[:, row:row + 1].to_broadcast([128, 64]), v0, start=True, stop=True), [ec, ev0], [epb])
                pe(lambda e: e.matmul(pb[64:128, :], ident[:, row:row + 1].to_broadcast([128, 64]), v1, start=True, stop=True), [ec, ev1], [epb])
            d1, ed1 = gt()
            dve(lambda e: e.tensor_tensor(d1, k_ap, pb[:], ALU.mult), [ek, epb], [ed1])
            a_ap, ea = a_of(e_)
            st, est = gt()
            dve(lambda e: e.tensor_tensor_scan(st, a_ap, d1, carry[:, mix, e_:e_ + 1], ALU.mult, ALU.add), [ea, ed1, e_carry[mix][e_]], [est])
            act(lambda e: e.activation(out=carry[:, mix, e_:e_ + 1], in_=st[:, T - 1:T], func=AF.Copy), [est], [e_carry[mix][e_]])
            pool(lambda e: e.tensor_tensor(st, st, q_ap, ALU.mult), [est, eq], [est])
            out_of(e_, st, est)

    for blk in range(NB):
        tk = slice(blk * T, (blk + 1) * T)
        S.dma("sp", hb[:], hT.rearrange("(k p) t -> p k t", p=128)[:, :, tk], writes=[e_hb])
        S.dma("pool", posi[:], pos[tk].partition_broadcast(128), writes=[e_pos])
        for wt in range(NWT):
            i = wt % 2
            S.dma(nextq(), wa[i][:], Wm[:, wt * 128:(wt + 1) * 128].rearrange("(k p) c -> p k c", p=128), writes=[ewa[i]])
            ps, eps = gp("A")
            for k in range(16):
                pe(lambda e, k=k: e.matmul(ps[:], wa[i][:, k, :], hb[:, k, :], start=(k == 0), stop=(k == 15)), [ewa[i], e_hb], [eps], chain=(k > 0))
            if 2 <= wt <= 5:
                ci = wt - 2
                act(lambda e: e.activation(out=xe[:, ci, 3:T + 3], in_=ps[:], func=AF.Copy), [eps], [e_xe[ci]])
            else:
                act(lambda e: e.activation(out=P[wt], in_=ps[:], func=AF.Copy), [eps], [P.e[wt]])
        for ci in range(4):
            wt = ci + 2
            dve(lambda e: e.tensor_scalar(P[wt], xe[:, ci, 3:T + 3], pc(CW + 4 * ci + 3), None, ALU.mult), [e_xe[ci], ec], [P.e[wt]])
            for k in range(3):
                dve(lambda e, k=k: e.scalar_tensor_tensor(out=P[wt], in0=xe[:, ci, k:T + k], scalar=pc(CW + 4 * ci + k), in1=P[wt], op0=ALU.mult, op1=ALU.add),
                    [e_xe[ci], ec, P.e[wt]], [P.e[wt]])
            dve(lambda e: e.tensor_copy(xe[:, ci, 0:3], xe[:, ci, T:T + 3]), [e_xe[ci]], [e_xe[ci]])
            act(lambda e: e.activation(out=P[wt], in_=P[wt], func=AF.Silu, bias=pc(CB + ci)), [P.e[wt], ec], [P.e[wt]])
        act(lambda e: e.activation(out=P[6], in_=P[6], func=AF.Exp, bias=pc(DTB)), [P.e[6], ec], [P.e[6]])
        act(lambda e: e.activation(out=P[6], in_=P[6], func=AF.Ln, bias=1.0), [P.e[6]], [P.e[6]])
        arow, earow = Lg[6], Lg.e[6]
        act(lambda e: e.activation(out=arow, in_=P[6], func=AF.Exp, scale=nalog), [P.e[6], e_sm2], [earow])
        abc = []
        for r in range(4):
            pb, epb = gp("B")
            pe(lambda e: e.matmul(pb[:], ident[:, r:r + 1].to_broadcast([128, 128]), arow, start=True, stop=True), [ec, earow], [epb])
            t, et = Lg[r], Lg.e[r]
            act(lambda e: e.activation(out=t, in_=pb[:], func=AF.Copy), [epb], [et])
            abc.append((t, et))
        xdt = []
        for i in range(2):
            pb, epb = gp("B")
            pe(lambda e: e.matmul(pb[:], sel[:, i, :], P[6], start=True, stop=True), [ec, P.e[6]], [epb])
            t, et = Lg[4 + i], Lg.e[4 + i]
            dve(lambda e: e.tensor_tensor(t, P[2 + i], pb[:], ALU.mult), [P.e[2 + i], epb], [et])
            xdt.append((t, et))
        def ssd_out(e_, st, est):
            i, el = divmod(e_, 128)
            pe(lambda e: e.matmul(psY[i][:], wide[:, 127 - el:255 - el], st, start=(el == 0), stop=(el == 127)), [ec, est], [epsY[i]], chain=(el > 0))
        chan_scan(0, 256, P[4], P.e[4], P[5], P.e[5], lambda e_: abc[e_ // 64], lambda e_: (xdt[e_ // 128][0], xdt[e_ // 128][1], e_ % 128, None), ssd_out)
        for i in range(2):
            t, et = gt()
            dve(lambda e: e.scalar_tensor_tensor(out=t, in0=P[2 + i], scalar=pc(DSK + i), in1=psY[i][:], op0=ALU.mult, op1=ALU.add), [P.e[2 + i], ec, epsY[i]], [et])
            act(lambda e: e.activation(out=P[i], in_=P[i], func=AF.Silu), [P.e[i]], [P.e[i]])
            dve(lambda e: e.tensor_tensor(t, t, P[i], ALU.mult), [et, P.e[i]], [et])
            S.dma(nextq(), yT[0, i * 128:(i + 1) * 128, tk], t, reads=[et], writes=[eo])
        dve(lambda e: e.tensor_copy(posf[:], posi[:]), [e_pos], [e_pos])
        ang, eang = gt()
        dve(lambda e: e.tensor_scalar(ang, posf[:], pc(INVF), None, ALU.mult), [e_pos, ec], [eang])
        sn, esn = gt(); cs, ecs = gt()
        sincos(ang, eang, sn, esn, 0.0); sincos(ang, eang, cs, ecs, float(np.pi / 2))
        for wt, scale in ((9, 1.0), (10, 0.125)):
            pb, epb = gp("B")
            pe(lambda e: e.matmul(pb[:], rot[:], P[wt], start=True, stop=True), [ec, P.e[wt]], [epb])
            t, et = gt()
            dve(lambda e: e.tensor_tensor(t, pb[:], sn, ALU.mult), [epb, esn], [et])
            dve(lambda e: e.tensor_tensor(P[wt], P[wt], cs, ALU.mult), [P.e[wt], ecs], [P.e[wt]])
            dve(lambda e: e.tensor_tensor(P[wt], P[wt], t, ALU.add), [P.e[wt], et], [P.e[wt]])
            if scale != 1.0:
                dve(lambda e: e.tensor_scalar(P[wt], P[wt], scale, None, ALU.mult), [P.e[wt]], [P.e[wt]])
        gam_t, egam = Lg[0], Lg.e[0]
        dve(lambda e: e.tensor_scalar(gam_t, tau[:], 0.0, gam, ALU.mult, ALU.add), [ec, e_sm2], [egam])
        def ret_out(e_, st, est):
            for hh in range(2):
                pe(lambda e, hh=hh: e.matmul(psY[hh][:], wide[hh * 64:(hh + 1) * 64, 127 - e_:255 - e_], st[hh * 64:(hh + 1) * 64, :], start=(e_ == 0), stop=(e_ == 127)),
                   [ec, est], [epsY[hh]], chain=(e_ > 0))
        chan_scan(1, 128, P[10], P.e[10], P[9], P.e[9], lambda e_: (gam_t, egam),
                  lambda e_: (None, None, e_, ((P[11], P.e[11]), (P[12], P.e[12]))), ret_out)
        for hh in range(2):
            y, ey = gt()
            act(lambda e: e.activation(out=y, in_=psY[hh][:], func=AF.Copy), [epsY[hh]], [ey])
            sq, esq = gt()
            act(lambda e: e.activation(out=sq, in_=y, func=AF.Square), [ey], [esq])
            pa, epa = gp("A"); pb, epb = gp("B")
            pe(lambda e: e.matmul(pa[:], ones[:], y, start=True, stop=True), [ec, ey], [epa])
            pe(lambda e: e.matmul(pb[:], ones[:], sq, start=True, stop=True), [ec, esq], [epb])
            mu, emu = gt(); rs_, ers = gt()
            act(lambda e: e.activation(out=mu, in_=pa[:], func=AF.Copy, scale=1.0 / 128), [epa], [emu])
            dve(lambda e: e.tensor_tensor(rs_, mu, mu, ALU.mult), [emu], [ers])
            dve(lambda e: e.scalar_tensor_tensor(out=rs_, in0=pb[:], scalar=1.0 / 128, in1=rs_, op0=ALU.mult, op1=ALU.subtract), [epb, ers], [ers])
            dve(lambda e: e.tensor_scalar(rs_, rs_, LN_EPS, None, ALU.add), [ers], [ers])
            act(lambda e: e.activation(out=rs_, in_=rs_, func=AF.Sqrt), [ers], [ers])
            dve(lambda e: e.reciprocal(rs_, rs_), [ers], [ers])
            dve(lambda e: e.tensor_tensor(y, y, mu, ALU.subtract), [ey, emu], [ey])
            dve(lambda e: e.scalar_tensor_tensor(out=y, in0=y, scalar=pc(RNW + hh), in1=rs_, op0=ALU.mult, op1=ALU.mult), [ey, ers, ec], [ey])
            act(lambda e: e.activation(out=P[13 + hh], in_=P[13 + hh], func=AF.Silu), [P.e[13 + hh]], [P.e[13 + hh]])
            dve(lambda e: e.tensor_tensor(y, y, P[13 + hh], ALU.mult), [ey, P.e[13 + hh]], [ey])
            S.dma(nextq(), yT[2, hh * 128:(hh + 1) * 128, tk], y, reads=[ey], writes=[eo])
        pb, epb = gp("B")
        pe(lambda e: e.matmul(pb[:], wal[:], P[21], start=True, stop=True), [ec, P.e[21]], [epb])
        al, eal = Lg[1], Lg.e[1]
        act(lambda e: e.activation(out=al, in_=pb[:], func=AF.Exp, scale=-1.0, bias=nbal), [epb, e_sm2], [eal])
        act(lambda e: e.activation(out=al, in_=al, func=AF.Ln, bias=1.0), [eal], [eal])
        act(lambda e: e.activation(out=al, in_=al, func=AF.Exp, scale=-1.0 / 16.0), [eal], [eal])
        dve(lambda e: e.tensor_scalar(P[15], P[15], float(128 ** -0.5), None, ALU.mult), [P.e[15]], [P.e[15]])
        def gla_out(e_, st, est):
            i, el = divmod(e_, 128)
            pe(lambda e: e.matmul(psY[i][:], wide[:, 127 - el:255 - el], st, start=(el == 0), stop=(el == 127)), [ec, est], [epsY[i]], chain=(el > 0))
        chan_scan(2, 256, P[16], P.e[16], P[15], P.e[15], lambda e_: (al, eal), lambda e_: (P[17 + e_ // 128], P.e[17 + e_ // 128], e_ % 128, None), gla_out)
        ys_ = []
        pa, epa = gp("A")
        for i in range(2):
            y, ey = gt(); sq, esq = gt()
            act(lambda e: e.activation(out=y, in_=psY[i][:], func=AF.Copy), [epsY[i]], [ey])
            act(lambda e: e.activation(out=sq, in_=y, func=AF.Square), [ey], [esq])
            pe(lambda e, i=i: e.matmul(pa[:], ones[:], sq, start=(i == 0), stop=(i == 1)), [ec, esq], [epa], chain=(i > 0))
            ys_.append((y, ey))
        rs_, ers = gt()
        dve(lambda e: e.tensor_scalar(rs_, pa[:], 1.0 / 256, RMS_EPS, ALU.mult, ALU.add), [epa], [ers])
        act(lambda e: e.activation(out=rs_, in_=rs_, func=AF.Sqrt), [ers], [ers])
        dve(lambda e: e.reciprocal(rs_, rs_), [ers], [ers])
        for i in range(2):
            y, ey = ys_[i]
            dve(lambda e: e.scalar_tensor_tensor(out=y, in0=y, scalar=pc(GNW + i), in1=rs_, op0=ALU.mult, op1=ALU.mult), [ey, ers, ec], [ey])
            act(lambda e: e.activation(out=P[19 + i], in_=P[19 + i], func=AF.Silu), [P.e[19 + i]], [P.e[19 + i]])
            dve(lambda e: e.tensor_tensor(y, y, P[19 + i], ALU.mult), [ey, P.e[19 + i]], [ey])
            S.dma(nextq(), yT[3, i * 128:(i + 1) * 128, tk], y, reads=[ey], writes=[eo])
        for hf in range(2):
         hs = slice(hf * TS, (hf + 1) * TS)
         for i in range(2):
             for jj in range(4):
                 j = 4 * i + jj
                 xr, exr = gt(); xi, exi = gt(); xr = xr[:, 0:TS]; xi = xi[:, 0:TS]
                 for ri, (xx, exx) in enumerate(((xr, exr), (xi, exi))):
                     pb, epb = gp("B")
                     pe(lambda e: e.matmul(pb[:, 0:TS], Bl[:, ri, j, :], P.t[:, 7 + i, hs], start=True, stop=True), [ec, P.e[7 + i]], [epb])
                     act(lambda e: e.activation(out=xx, in_=pb[:, 0:TS], func=AF.Copy), [epb], [exx])
                 wr, ewr = gt(); wi, ewi = gt(); t1, et1 = gt(); wr = wr[:, 0:TS]; wi = wi[:, 0:TS]; t1 = t1[:, 0:TS]
                 dve(lambda e: e.tensor_tensor(wr, xr, Tn[:, 0, j, :], ALU.mult), [exr, e_tab], [ewr])
                 pool(lambda e: e.tensor_tensor(t1, xi, Tn[:, 1, j, :], ALU.mult), [exi, e_tab], [et1])
                 dve(lambda e: e.tensor_tensor(wr, wr, t1, ALU.subtract), [ewr, et1], [ewr])
                 pool(lambda e: e.tensor_tensor(wi, xr, Tn[:, 1, j, :], ALU.mult), [exr, e_tab], [ewi])
                 dve(lambda e: e.tensor_tensor(t1, xi, Tn[:, 0, j, :], ALU.mult), [exi, e_tab, ewr], [et1])
                 dve(lambda e: e.tensor_tensor(wi, wi, t1, ALU.add), [ewi, et1], [ewi])
                 dve(lambda e: e.tensor_tensor_scan(wr, ones[:, 0:1].to_broadcast([128, TS]), wr, s5c[:, 0, j:j + 1], ALU.mult, ALU.add), [ec, ewr, e_s5c], [ewr])
                 dve(lambda e: e.tensor_tensor_scan(wi, ones[:, 0:1].to_broadcast([128, TS]), wi, s5c[:, 1, j:j + 1], ALU.mult, ALU.add), [ec, ewi, e_s5c], [ewi])
                 hr, ehr = xr, exr; hi, ehi = xi, exi
                 dve(lambda e: e.tensor_tensor(hr, wr, Tp[:, 0, j, :], ALU.mult), [ewr, e_tab], [ehr])
                 pool(lambda e: e.tensor_tensor(t1, wi, Tp[:, 1, j, :], ALU.mult), [ewi, e_tab], [et1])
                 dve(lambda e: e.tensor_tensor(hr, hr, t1, ALU.subtract), [ehr, et1], [ehr])
                 pool(lambda e: e.tensor_tensor(hi, wr, Tp[:, 1, j, :], ALU.mult), [ewr, e_tab], [ehi])
                 dve(lambda e: e.tensor_tensor(t1, wi, Tp[:, 0, j, :], ALU.mult), [ewi, e_tab, ehr], [et1])
                 dve(lambda e: e.tensor_tensor(hi, hi, t1, ALU.add), [ehi, et1], [ehi])
                 c1 = sm2[:, 48:49]
                 dve(lambda e: e.tensor_scalar(c1, hi[:, TS - 1:TS], lbi[:, j:j + 1], None, ALU.mult), [ehi, e_sm2], [e_sm2])
                 dve(lambda e: e.scalar_tensor_tensor(out=s5c[:, 0, j:j + 1], in0=hr[:, TS - 1:TS], scalar=lbr[:, j:j + 1], in1=c1, op0=ALU.mult, op1=ALU.subtract), [ehr, e_sm2], [e_s5c])
                 dve(lambda e: e.tensor_scalar(c1, hi[:, TS - 1:TS], lbr[:, j:j + 1], None, ALU.mult), [ehi, e_sm2], [e_sm2])
                 dve(lambda e: e.scalar_tensor_tensor(out=s5c[:, 1, j:j + 1], in0=hr[:, TS - 1:TS], scalar=lbi[:, j:j + 1], in1=c1, op0=ALU.mult, op1=ALU.add), [ehr, e_sm2], [e_s5c])
                 pe(lambda e: e.matmul(psY[0][:, 0:TS], Cl[:, 0, j, :], hr, start=(jj == 0), stop=(jj == 3)), [ec, ehr], [epsY[0]], chain=(jj > 0))
                 pe(lambda e: e.matmul(psZ[0][:, 0:TS], Cl[:, 1, j, :], hi, start=(jj == 0), stop=(jj == 3)), [ec, ehi], [epsZ[0]], chain=(jj > 0))
             y, ey = gt(); y = y[:, 0:TS]
             act(lambda e: e.activation(out=y, in_=psZ[0][:, 0:TS], func=AF.Copy), [epsZ[0]], [ey])
             dve(lambda e: e.tensor_tensor(y, psY[0][:, 0:TS], y, ALU.subtract), [epsY[0], ey], [ey])
             dve(lambda e: e.scalar_tensor_tensor(out=y, in0=P.t[:, 7 + i, hs], scalar=pc(S5D + i), in1=y, op0=ALU.mult, op1=ALU.add), [P.e[7 + i], ec, ey], [ey])
             act(lambda e: e.activation(out=y, in_=y, func=AF.Gelu), [ey], [ey])
             S.dma(nextq(), yT[1, i * 128:(i + 1) * 128, blk * T + hf * TS:blk * T + (hf + 1) * TS], y, reads=[ey], writes=[eo])

    S.wait_all("sp", [eo]); S.wait_all("pool", [eo])
    cx.st.close()
    return nc


OFF = dict(z=0, xs=1024, B=2048, C=2304, dt=2560, u=2576, rq=3600, rk=4112, rv=4624, rg=5648, gq=6672, gk=7184, gv=7696, gr=8720, gc=9744, gate=9760)


def _mixer_consts(T=512):
    c = {}
    c["c_ident"] = np.eye(128, dtype=np.float32); c["c_ones"] = np.ones((128, 128), np.float32)
    w = np.zeros((128, 255), np.float32); w[:, 127] = 1.0; c["c_wide"] = w
    sel = np.zeros((128, 2, 128), np.float32)
    for i in range(2):
        for m in range(128):
            sel[2 * i + m // 64, i, m] = 1.0
    c["c_sel"] = sel
    rot = np.zeros((128, 128), np.float32)
    for m in range(128):
        if m % 64 < 32:
            rot[m + 32, m] = -1.0
        else:
            rot[m - 32, m] = 1.0
    c["c_rot"] = rot
    c["c_tau"] = np.tile(np.arange(T, dtype=np.float32)[None, :], (128, 1))
    return c


def _mixer_inputs(inp, l, j, T=512):
    g = j // 2
    w_in = inp["w_in"][l]
    Wm = np.zeros((D, NWT * 128), np.float32)
    def put(t, c0, n):
        Wm[:, t * 128:t * 128 + n] = w_in[:, c0:c0 + n]
    put(0, OFF["z"] + 256 * j, 256); put(2, OFF["xs"] + 256 * j, 256); put(4, OFF["B"] + 128 * g, 128); put(5, OFF["C"] + 128 * g, 128)
    put(6, OFF["dt"] + 4 * j, 4); put(7, OFF["u"] + 256 * j, 256); put(9, OFF["rq"] + 128 * j, 128); put(10, OFF["rk"] + 128 * j, 128)
    put(11, OFF["rv"] + 256 * j, 256); put(13, OFF["rg"] + 256 * j, 256); put(15, OFF["gq"] + 128 * j, 128); put(16, OFF["gk"] + 128 * j, 128)
    put(17, OFF["gv"] + 256 * j, 256); put(19, OFF["gr"] + 256 * j, 256); put(21, OFF["gc"], 16)
    par = np.zeros((128, 64), np.float32)
    p = np.arange(128)
    cw, cb = inp["ssd_conv_w"][l], inp["ssd_conv_b"][l]
    chans = [256 * j + p, 256 * j + 128 + p, 1024 + 128 * g + p, 1280 + 128 * g + p]
    for ci, ch in enumerate(chans):
        for k in range(4):
            par[:, 4 * ci + k] = cw[k, ch]
        par[:, 16 + ci] = cb[ch]
    par[0:4, 20] = inp["ssd_dt_bias"][l][4 * j:4 * j + 4]; par[0:4, 21] = inp["ssd_a_log"][l][4 * j:4 * j + 4]
    for i in range(2):
        par[:, 22 + i] = inp["ssd_d"][l][4 * j + 2 * i + p // 64]
        par[:, 24 + i] = inp["s5_d"][l][256 * j + 128 * i + p]
        par[:, 36 + i] = inp["ret_norm_w"][l][(2 * j + i) * 128 + p]
        par[:, 41 + i] = inp["gla_norm_w"][l][256 * j + 128 * i + p]
    par[:, 26] = inp["s5_lambda_re"][l][p % 64]; par[:, 27] = inp["s5_lambda_im"][l][p % 64]
    for jt in range(8):
        par[:, 28 + jt] = inp["s5_log_dt"][l][16 * j + 2 * jt + p // 64]
    inv_freq = (1.0 / (np.float32(10000.0) ** (np.arange(32, dtype=np.float32) / np.float32(32)))).astype(np.float32)
    par[:, 38] = inv_freq[(p % 64) % 32]
    lg = np.log1p(-np.exp2(-5.0 - np.arange(8, dtype=np.float32))).astype(np.float32)
    par[:, 39] = lg[2 * j + p // 64]
    par[:, 40] = inp["gla_b_alpha"][l][128 * j + p]
    wal = np.zeros((128, 128), np.float32); wal[0:16] = inp["gla_w_alpha"][l][:, 128 * j:128 * j + 128]
    s5B = np.zeros((2, 128, 8, 128), np.float32); s5C = np.zeros((2, 128, 8, 128), np.float32)
    for ri, (bn, cn) in enumerate((("s5_b_re", "s5_c_re"), ("s5_b_im", "s5_c_im"))):
        bb, cc = inp[bn][l], inp[cn][l]
        for jt in range(8):
            for g2 in range(2):
                gl = 2 * jt + g2
                G = 16 * j + gl
                k0 = (gl % 8) * 16
                s5B[ri, k0:k0 + 16, jt, g2 * 64:(g2 + 1) * 64] = bb[G].T
                s5C[ri, g2 * 64:(g2 + 1) * 64, jt, k0:k0 + 16] = cc[G].T
    d = dict(Wm=Wm, cpar=par, w_alpha=wal, s5B=s5B, s5C=s5C)
    d.update(_mixer_consts(T))
    return d


_PROGS = {}


def _prog(kind, n):
    key = (kind, n)
    if key not in _PROGS:
        _PROGS[key] = build_mixer(n) if kind == "m" else build_rest(n)
    return _PROGS[key]


def kernel(**inp):
    inp = {k: np.asarray(v) for k, v in inp.items()}
    x = inp["x"]; Bn, Sn, _ = x.shape
    NQ = 4; NT = Sn // NQ
    hT = [np.ascontiguousarray(x[b].T) for b in range(Bn)]
    rconst = dict(c_ones=np.ones((128, 128), np.float32), c_ident=np.eye(128, dtype=np.float32), c_sel=np.zeros((32, 32 * 128), np.float32))
    for l in range(2):
        mi = [_mixer_inputs(inp, l, j) for j in range(4)]
        ins = []
        for c in range(8):
            b, j = divmod(c, 4)
            d = dict(mi[j]); d["hT"] = hT[b]; d["pos"] = np.ascontiguousarray(inp["positions"][b].astype(np.int32))
            ins.append(d)
        res = run_bass_kernel_spmd(_prog("m", Sn), ins, core_ids=list(range(8)))
        ym = [r["yT"] for r in res.results]
        W = dict(w_gate=np.ascontiguousarray(inp["w_in"][l][:, OFF["gate"]:]), w_branch=inp["w_branch"][l], w_out=inp["w_out"][l],
                 ssd_norm_w=inp["ssd_norm_w"][l], s5_w_glu=inp["s5_w_glu"][l], s5_b_glu=inp["s5_b_glu"][l],
                 ln1_w=inp["ln1_w"][l], ln1_b=inp["ln1_b"][l], ln2_w=inp["ln2_w"][l], ln2_b=inp["ln2_b"][l], ln3_w=inp["ln3_w"][l], ln3_b=inp["ln3_b"][l],
                 router_w=inp["router_w"][l], router_b=inp["router_b"][l], moe_w_gate_up=inp["moe_w_gate_up"][l], moe_b_gate_up=inp["moe_b_gate_up"][l],
                 moe_w_down=inp["moe_w_down"][l], moe_b_down=inp["moe_b_down"][l], ple_w_gate=inp["ple_w_gate"][l], ple_w_proj=inp["ple_w_proj"][l])
        ins = []
        for c in range(8):
            b, q = divmod(c, 4)
            ts = slice(q * NT, (q + 1) * NT)
            ysT = np.concatenate([ym[b * 4 + j][:, :, ts] for j in range(4)], axis=1)
            d = dict(hT=np.ascontiguousarray(hT[b][:, ts]), ysT=np.ascontiguousarray(ysT), pT=np.ascontiguousarray(inp["p"][l, b, ts].T))
            d.update(W); d.update(rconst)
            ins.append(d)
        res = run_bass_kernel_spmd(_prog("r", NT), ins, core_ids=list(range(8)))
        hT = [np.concatenate([res.results[b * 4 + q]["oT"] for q in range(4)], axis=1) for b in range(Bn)]
    return np.ascontiguousarray(np.stack([h.T for h in hT], 0)).astype(np.float32)
```

```python
import numpy as np
import concourse.bass as bass
import concourse.mybir as mybir

F32 = mybir.dt.float32
F32R = mybir.dt.float32r
BF16 = mybir.dt.bfloat16
I32 = mybir.dt.int32
AF = mybir.ActivationFunctionType
ALU = mybir.AluOpType
AX = mybir.AxisListType

SEM_ROLL = 12000


class Ent:
    __slots__ = ("name", "lastw", "reads", "dsem", "dcount")

    def __init__(self, name):
        self.name = name
        self.lastw = None
        self.reads = {}
        self.dsem = None
        self.dcount = 0


class Sched:
    def __init__(self, nc, stack):
        self.nc = nc
        self.stack = stack
        self.eng = {"pe": nc.tensor, "dve": nc.vector, "act": nc.scalar, "pool": nc.gpsimd, "sp": nc.sync}
        self.sems = {}
        self.cur = {}
        self.epoch = {e: 0 for e in self.eng}
        self.seen = {e: {} for e in self.eng}
        self.nsem = 0
        self.ninst = 0
        for e in self.eng:
            self._newsem(e)

    def _alloc(self, name):
        self.nsem += 1
        return self.stack.enter_context(self.nc.semaphore(name))

    def _newsem(self, e):
        key = f"{e}{self.epoch[e]}"
        self.epoch[e] += 1
        self.sems[key] = self._alloc("s_" + key)
        self.cur[e] = [key, 0]

    def ent(self, name):
        return Ent(name)

    def _deps(self, e, reads, writes):
        deps = {}
        def add(ev):
            if ev is None:
                return
            k, v = ev
            if deps.get(k, 0) < v:
                deps[k] = v
        for r in reads:
            add(r.lastw)
        for w in writes:
            add(w.lastw)
            for k, v in w.reads.items():
                add((k, v))
        return deps

    def _wait(self, e, deps, skip_self_pe=False):
        seen = self.seen[e]
        engine = self.eng[e]
        for k, v in deps.items():
            if skip_self_pe and k.startswith("pe") and e == "pe":
                continue
            if seen.get(k, 0) >= v:
                continue
            engine.wait_ge(self.sems[k], v)
            seen[k] = v

    def _mark(self, ev, reads, writes):
        k, v = ev
        for r in reads:
            if r.reads.get(k, 0) < v:
                r.reads[k] = v
        for w in writes:
            w.lastw = ev
            w.reads = {}

    def op(self, e, fn, reads=(), writes=(), pe_chain=False):
        deps = self._deps(e, reads, writes)
        self._wait(e, deps, skip_self_pe=pe_chain)
        inst = fn(self.eng[e])
        cur = self.cur[e]
        cur[1] += 1
        inst.then_inc(self.sems[cur[0]], 1)
        ev = (cur[0], cur[1])
        self._mark(ev, reads, writes)
        self.ninst += 1
        if cur[1] >= SEM_ROLL:
            self._newsem(e)
        return inst

    def dma(self, q, out, in_, reads=(), writes=(), sem_ent=None, **kw):
        deps = self._deps(q, reads, writes)
        self._wait(q, deps)
        se = sem_ent if sem_ent is not None else writes[0]
        if se.dsem is None:
            se.dsem = f"d{self.nsem}_{se.name}"
            self.sems[se.dsem] = self._alloc(se.dsem)
        se.dcount += 16
        self.eng[q].dma_start(out=out, in_=in_, **kw).then_inc(self.sems[se.dsem], 16)
        ev = (se.dsem, se.dcount)
        self._mark(ev, reads, writes)
        self.ninst += 1

    def wait_all(self, e, ents):
        deps = self._deps(e, ents, ents)
        self._wait(e, deps)


import contextlib
from concourse.bass_utils import run_bass_kernel_spmd

D = 2048
ALPHA = 4.0 ** 0.25
LN_EPS = 1e-5
RMS_EPS = 1e-6


class Ctx:
    def __init__(self):
        self.nc = bass.Bass("TRN2", target_bir_lowering=False)
        self.st = contextlib.ExitStack()
        self.S = Sched(self.nc, self.st)
        self.n = 0

    def din(self, name, shape, dt=F32):
        return self.nc.dram_tensor(name, list(shape), dt, kind="ExternalInput").ap()

    def dout(self, name, shape, dt=F32):
        return self.nc.dram_tensor(name, list(shape), dt, kind="ExternalOutput").ap()

    def sb(self, name, shape, dt=F32):
        t = self.st.enter_context(self.nc.sbuf_tensor(name, list(shape), dt))
        return t

    def ps(self, name, shape=(128, 512), dt=F32):
        return self.st.enter_context(self.nc.psum_tensor(name, list(shape), dt))

    def ent(self, name):
        return self.S.ent(name)


class Tiles:
    def __init__(self, cx, name, n, w, dt=F32):
        self.t = cx.sb(name, [128, n, w], dt)
        self.e = [cx.ent(f"{name}{i}") for i in range(n)]
        self.n = n

    def __getitem__(self, i):
        return self.t[:, i, :]


def build_rest(NT, T=512, NE=32):
    cx = Ctx(); nc = cx.nc; S = cx.S
    NB = NT // T
    hT = cx.din("hT", [D, NT]); ysT = cx.din("ysT", [4, 1024, NT]); pT = cx.din("pT", [256, NT])
    w_gate = cx.din("w_gate", [D, 4 * D]); w_branch = cx.din("w_branch", [4, 1024, D]); w_out = cx.din("w_out", [D, D])
    ssd_norm_w = cx.din("ssd_norm_w", [1024]); w_glu = cx.din("s5_w_glu", [1024, 1024]); b_glu = cx.din("s5_b_glu", [1024])
    lnw = [cx.din(f"ln{i}_w", [D]) for i in (1, 2, 3)]; lnb = [cx.din(f"ln{i}_b", [D]) for i in (1, 2, 3)]
    router_w = cx.din("router_w", [D, NE]); router_b = cx.din("router_b", [NE])
    w_gu = cx.din("moe_w_gate_up", [NE, D, D]); b_gu = cx.din("moe_b_gate_up", [NE, D])
    w_dn = cx.din("moe_w_down", [NE, 1024, D]); b_dn = cx.din("moe_b_down", [NE, D])
    ple_wg = cx.din("ple_w_gate", [D, D]); ple_wp = cx.din("ple_w_proj", [256, D])
    c_ones = cx.din("c_ones", [128, 128]); c_ident = cx.din("c_ident", [128, 128]); c_sel = cx.din("c_sel", [32, 32 * 128])
    oT = cx.dout("oT", [D, NT])
    eo = cx.ent("oT")

    ec = cx.ent("consts")
    ones = cx.sb("ones", [128, 128]); ident = cx.sb("ident", [128, 128])
    lnw_t = cx.sb("lnw_t", [128, 3, 16]); lnb_t = cx.sb("lnb_t", [128, 3, 16])
    snw_t = cx.sb("snw_t", [128, 8]); bglu_t = cx.sb("bglu_t", [128, 8])
    bgu_t = cx.sb("bgu_t", [128, NE, 16]); bdn_t = cx.sb("bdn_t", [NE, D])
    rw_t = cx.sb("rw_t", [128, 16, NE]); rb_t = cx.sb("rb_t", [128, NE])
    wp_t = [cx.sb(f"wp_t{i}", [128, 2, 128]) for i in range(2)]; ewp = [cx.ent(f"wp{i}") for i in range(2)]
    S.dma("sp", ones[:], c_ones, writes=[ec]); S.dma("sp", ident[:], c_ident, writes=[ec])
    for i in range(3):
        S.dma("sp", lnw_t[:, i, :], lnw[i].rearrange("(t p) -> p t", p=128), writes=[ec], allow_slow_non_contiguous=True)
        S.dma("sp", lnb_t[:, i, :], lnb[i].rearrange("(t p) -> p t", p=128), writes=[ec], allow_slow_non_contiguous=True)
    S.dma("sp", snw_t[:], ssd_norm_w.rearrange("(t p) -> p t", p=128), writes=[ec], allow_slow_non_contiguous=True)
    S.dma("sp", bglu_t[:], b_glu.rearrange("(t p) -> p t", p=128), writes=[ec], allow_slow_non_contiguous=True)
    for e8 in range(NE // 8):
        S.dma("sp", bgu_t[:, e8 * 8:(e8 + 1) * 8, :], b_gu[e8 * 8:(e8 + 1) * 8].rearrange("e (t p) -> p e t", p=128), writes=[ec], allow_slow_non_contiguous=True)
    S.dma("sp", bdn_t[:], b_dn, writes=[ec])
    S.dma("sp", rw_t[:], router_w.rearrange("(k p) e -> p k e", p=128), writes=[ec])
    S.dma("sp", rb_t[:], router_b.partition_broadcast(128), writes=[ec])

    h = Tiles(cx, "h", 16, T); u = Tiles(cx, "u", 16, T); mg = Tiles(cx, "mg", 16, T)
    hb = Tiles(cx, "hb", 16, T, BF16); mgb = Tiles(cx, "mgb", 16, T, BF16)
    ysA = Tiles(cx, "ysA", 8, T, BF16)
    ppt = Tiles(cx, "ppt", 2, T)
    tmp = Tiles(cx, "tmp", 4, T)
    mean = cx.sb("mean", [128, T]); e_mean = cx.ent("mean")
    rstd = cx.sb("rstd", [128, T]); e_rstd = cx.ent("rstd")
    gT = cx.sb("gT", [NE, T]); e_gT = cx.ent("gT")
    gbc = mean; e_gbc = e_mean
    rs = cx.sb("rs", [128, 160]); e_rs = cx.ent("rs")
    NWA = 2
    wa = [cx.sb(f"wa{i}", [128, 16, 128]) for i in range(NWA)]; ewa = [cx.ent(f"wa{i}") for i in range(NWA)]
    wb = [cx.sb(f"wb{i}", [128, 8, 128]) for i in range(NWA)]; ewb = [cx.ent(f"wb{i}") for i in range(NWA)]
    wab = [cx.sb(f"wab{i}", [128, 16, 128], BF16) for i in range(NWA)]; ewab = [cx.ent(f"wab{i}") for i in range(NWA)]
    wbb = [cx.sb(f"wbb{i}", [128, 8, 128], BF16) for i in range(NWA)]; ewbb = [cx.ent(f"wbb{i}") for i in range(NWA)]
    cnt_c = [0]
    def cast(dst, edst, src, esrc):
        cnt_c[0] += 1
        if cnt_c[0] % 2:
            S.op("pool", lambda e: e.tensor_copy(dst, src), reads=[esrc], writes=[edst])
        else:
            S.op("act", lambda e: e.activation(out=dst, in_=src, func=AF.Copy), reads=[esrc], writes=[edst])
    cnt = {"wa": 0, "wb": 0, "tmp": 0, "ps": 0, "q": 0}
    psA = [cx.ps(f"psA{i}") for i in range(3)]; epsA = [cx.ent(f"psA{i}") for i in range(3)]
    psB = [cx.ps(f"psB{i}") for i in range(3)]; epsB = [cx.ent(f"psB{i}") for i in range(3)]
    psS = cx.ps("psS"); epsS = cx.ent("psS")
    psQ = cx.ps("psQ"); epsQ = cx.ent("psQ")
    pc = {"A": 0, "B": 0}

    def nextq():
        return "sp"

    def load_wa(src2d, c0):
        i = cnt["wa"] % NWA; cnt["wa"] += 1
        S.dma(nextq(), wa[i][:], src2d[:, c0:c0 + 128].rearrange("(k p) c -> p k c", p=128), writes=[ewa[i]])
        cast(wab[i][:], ewab[i], wa[i][:], ewa[i])
        return wab[i], ewab[i]

    def load_wb(src2d, c0):
        i = cnt["wb"] % NWA; cnt["wb"] += 1
        S.dma(nextq(), wb[i][:], src2d[:, c0:c0 + 128].rearrange("(k p) c -> p k c", p=128), writes=[ewb[i]])
        cast(wbb[i][:], ewbb[i], wb[i][:], ewb[i])
        return wbb[i], ewbb[i]

    def getps(kind):
        lst, el = (psA, epsA) if kind == "A" else (psB, epsB)
        i = pc[kind] % 3; pc[kind] += 1
        return lst[i], el[i]

    def gettmp():
        i = cnt["tmp"] % 4; cnt["tmp"] += 1
        return tmp[i], tmp.e[i]

    def mm_acc(ps, eps, w, ew, acts, nk):
        for k in range(nk):
            S.op("pe", lambda e, k=k: e.matmul(ps[:], w[:, k, :], acts[k], start=(k == 0), stop=(k == nk - 1)),
                 reads=[ew, acts.e[k]], writes=[eps], pe_chain=(k > 0))

    def stats(src, idxs, nfeat, eps_val):
        n = len(idxs)
        for j, k in enumerate(idxs):
            S.op("pe", lambda e, k=k, j=j: e.matmul(psS[:], ones[:], src[k], start=(j == 0), stop=(j == n - 1)),
                 reads=[ec, src.e[k]], writes=[epsS], pe_chain=(j > 0))
        for j, k in enumerate(idxs):
            t, et = gettmp()
            S.op("act", lambda e, k=k, t=t: e.activation(out=t, in_=src[k], func=AF.Square), reads=[src.e[k]], writes=[et])
            S.op("pe", lambda e, t=t, j=j: e.matmul(psQ[:], ones[:], t, start=(j == 0), stop=(j == n - 1)),
                 reads=[ec, et], writes=[epsQ], pe_chain=(j > 0))
        return n

    def layernorm(src, dst, li, dstb=None):
        stats(src, list(range(16)), D, LN_EPS)
        S.op("act", lambda e: e.activation(out=mean[:], in_=psS[:], func=AF.Copy, scale=1.0 / D), reads=[epsS], writes=[e_mean])
        t, et = gettmp()
        S.op("dve", lambda e: e.tensor_tensor(t, mean[:], mean[:], ALU.mult), reads=[e_mean], writes=[et])
        S.op("dve", lambda e: e.scalar_tensor_tensor(out=rstd[:], in0=psQ[:], scalar=1.0 / D, in1=t, op0=ALU.mult, op1=ALU.subtract),
             reads=[epsQ, et], writes=[e_rstd])
        S.op("dve", lambda e: e.tensor_scalar(rstd[:], rstd[:], LN_EPS, None, ALU.add), reads=[e_rstd], writes=[e_rstd])
        S.op("act", lambda e: e.activation(out=rstd[:], in_=rstd[:], func=AF.Sqrt), reads=[e_rstd], writes=[e_rstd])
        S.op("dve", lambda e: e.reciprocal(rstd[:], rstd[:]), reads=[e_rstd], writes=[e_rstd])
        for k in range(16):
            t, et = gettmp()
            S.op("dve", lambda e, k=k, t=t: e.tensor_tensor(t, src[k], mean[:], ALU.subtract), reads=[src.e[k], e_mean], writes=[et])
            S.op("pool", lambda e, t=t: e.tensor_tensor(t, t, rstd[:], ALU.mult), reads=[et, e_rstd], writes=[et])
            S.op("act", lambda e, k=k, t=t: e.activation(out=dst[k], in_=t, func=AF.Identity, scale=lnw_t[:, li, k:k + 1], bias=lnb_t[:, li, k:k + 1]),
                 reads=[et, ec], writes=[dst.e[k]])
            if dstb is not None:
                S.op("pool", lambda e, k=k: e.tensor_copy(dstb[k], dst[k]), reads=[dst.e[k]], writes=[dstb.e[k]])

    for blk in range(NB):
        tk = slice(blk * T, (blk + 1) * T)
        for k in range(16):
            S.dma(nextq(), h[k], hT[k * 128:(k + 1) * 128, tk], writes=[h.e[k]])
            cast(hb[k], hb.e[k], h[k], h.e[k])
        for k in range(2):
            S.dma(nextq(), ppt[k], pT[k * 128:(k + 1) * 128, tk], writes=[ppt.e[k]])
        for n in range(4):
            for k in range(8):
                S.dma(nextq(), u[k], ysT[n, k * 128:(k + 1) * 128, tk], writes=[u.e[k]])
            if n == 0:
                for g in range(2):
                    stats(u, [4 * g + i for i in range(4)], 512, RMS_EPS)
                    S.op("dve", lambda e: e.tensor_scalar(rstd[:], psQ[:], 1.0 / 512, RMS_EPS, ALU.mult, ALU.add), reads=[epsQ], writes=[e_rstd])
                    S.op("act", lambda e: e.activation(out=rstd[:], in_=rstd[:], func=AF.Sqrt), reads=[e_rstd], writes=[e_rstd])
                    S.op("dve", lambda e: e.reciprocal(rstd[:], rstd[:]), reads=[e_rstd], writes=[e_rstd])
                    for i in range(4):
                        k = 4 * g + i
                        S.op("dve", lambda e, k=k: e.scalar_tensor_tensor(out=ysA[k], in0=u[k], scalar=snw_t[:, k:k + 1], in1=rstd[:], op0=ALU.mult, op1=ALU.mult),
                             reads=[u.e[k], e_rstd, ec], writes=[ysA.e[k]])
            elif n == 1:
                for k in range(8):
                    cast(mgb[k], mgb.e[k], u[k], u.e[k])
                for m in range(8):
                    w, ew = load_wb(w_glu, m * 128)
                    ps, eps = getps("A")
                    mm_acc(ps, eps, w, ew, mgb, 8)
                    t, et = gettmp()
                    S.op("act", lambda e, t=t, ps=ps, m=m: e.activation(out=t, in_=ps[:], func=AF.Sigmoid, bias=bglu_t[:, m:m + 1]), reads=[eps, ec], writes=[et])
                    S.op("dve", lambda e, t=t, m=m: e.tensor_tensor(ysA[m], u[m], t, ALU.mult), reads=[u.e[m], et], writes=[ysA.e[m]])
            else:
                for k in range(8):
                    cast(ysA[k], ysA.e[k], u[k], u.e[k])
            ysrc = ysA
            for m in range(16):
                w, ew = load_wa(w_gate, n * D + m * 128)
                psg, epsg = getps("A")
                mm_acc(psg, epsg, w, ew, hb, 16)
                w2, ew2 = load_wb(w_branch[n], m * 128)
                psb, epsb = getps("B")
                mm_acc(psb, epsb, w2, ew2, ysrc, 8)
                t, et = gettmp()
                S.op("act", lambda e, t=t, psg=psg: e.activation(out=t, in_=psg[:], func=AF.Sigmoid), reads=[epsg], writes=[et])
                if n == 0:
                    S.op("dve", lambda e, t=t, psb=psb, m=m: e.tensor_tensor(mg[m], t, psb[:], ALU.mult), reads=[et, epsb], writes=[mg.e[m]])
                else:
                    S.op("dve", lambda e, t=t, psb=psb: e.tensor_tensor(t, t, psb[:], ALU.mult), reads=[et, epsb], writes=[et])
                    S.op("pool", lambda e, t=t, m=m: e.tensor_tensor(mg[m], mg[m], t, ALU.add), reads=[et, mg.e[m]], writes=[mg.e[m]])
        for m in range(16):
            cast(mgb[m], mgb.e[m], mg[m], mg.e[m])
        for m in range(16):
            w, ew = load_wa(w_out, m * 128)
            ps, eps = getps("A")
            mm_acc(ps, eps, w, ew, mgb, 16)
            S.op("dve", lambda e, ps=ps, m=m: e.scalar_tensor_tensor(out=u[m], in0=h[m], scalar=ALPHA, in1=ps[:], op0=ALU.mult, op1=ALU.add),
                 reads=[h.e[m], eps], writes=[u.e[m]])
        layernorm(u, h, 0, hb)
        for tt in range(T // 128):
            ts_ = slice(tt * 128, (tt + 1) * 128)
            ps, eps = getps("B")
            for k in range(16):
                S.op("pe", lambda e, k=k, ps=ps: e.matmul(ps[:, 0:NE], h.t[:, k, ts_], rw_t[:, k, :], start=(k == 0), stop=(k == 15)),
                     reads=[h.e[k], ec], writes=[eps], pe_chain=(k > 0))
            lg = rs[:, 0:NE]; m8 = rs[:, 32:40]; msk = rs[:, 40:40 + NE]; ex = rs[:, 72:72 + NE]; sm = rs[:, 104:105]; nmx = rs[:, 105:106]; gw = rs[:, 112:112 + NE]
            S.op("dve", lambda e, ps=ps: e.tensor_tensor(lg, ps[:, 0:NE], rb_t[:], ALU.add), reads=[eps, ec], writes=[e_rs])
            S.op("dve", lambda e: e.max(m8, lg), reads=[e_rs], writes=[e_rs])
            S.op("dve", lambda e: e.tensor_scalar(msk, lg, rs[:, 35:36], None, ALU.is_ge), reads=[e_rs], writes=[e_rs])
            S.op("dve", lambda e: e.tensor_scalar(nmx, rs[:, 32:33], -1.0, None, ALU.mult), reads=[e_rs], writes=[e_rs])
            S.op("act", lambda e: e.activation(out=ex, in_=lg, func=AF.Exp, bias=nmx), reads=[e_rs], writes=[e_rs])
            S.op("dve", lambda e: e.tensor_tensor(ex, ex, msk, ALU.mult), reads=[e_rs], writes=[e_rs])
            S.op("dve", lambda e: e.reduce_sum(sm, ex, AX.X), reads=[e_rs], writes=[e_rs])
            S.op("dve", lambda e: e.reciprocal(sm, sm), reads=[e_rs], writes=[e_rs])
            S.op("dve", lambda e: e.tensor_scalar(gw, ex, sm, None, ALU.mult), reads=[e_rs], writes=[e_rs])
            ps2, eps2 = getps("B")
            S.op("pe", lambda e, ps2=ps2: e.transpose(ps2[0:NE, 0:128], gw, ident[:]), reads=[e_rs, ec], writes=[eps2])
            S.op("act", lambda e, ps2=ps2: e.activation(out=gT[:, ts_], in_=ps2[0:NE, 0:128], func=AF.Copy), reads=[eps2], writes=[e_gT])
        for m in range(16):
            ps, eps = getps("A")
            S.op("pe", lambda e, ps=ps, m=m: e.matmul(ps[:], bdn_t[:, m * 128:(m + 1) * 128], gT[:], start=True, stop=True), reads=[ec, e_gT], writes=[eps])
            S.op("act", lambda e, ps=ps, m=m: e.activation(out=mg[m], in_=ps[:], func=AF.Copy), reads=[eps], writes=[mg.e[m]])
        for ex_i in range(NE):
            ps, eps = getps("B")
            S.op("pe", lambda e, ps=ps: e.matmul(ps[:], ident[0:NE, ex_i:ex_i + 1].to_broadcast([NE, 128]), gT[:], start=True, stop=True), reads=[ec, e_gT], writes=[eps])
            S.op("act", lambda e, ps=ps: e.activation(out=gbc[:], in_=ps[:], func=AF.Copy), reads=[eps], writes=[e_gbc])
            for f in range(8):
                w, ew = load_wa(w_gu[ex_i], f * 128)
                psg, epsg = getps("A")
                mm_acc(psg, epsg, w, ew, hb, 16)
                w2, ew2 = load_wa(w_gu[ex_i], 1024 + f * 128)
                psu, epsu = getps("B")
                mm_acc(psu, epsu, w2, ew2, hb, 16)
                gc, egc = gettmp(); sg, esg = gettmp(); uc, euc = gettmp()
                S.op("dve", lambda e: e.tensor_scalar(gc, psg[:], bgu_t[:, ex_i, f:f + 1], 7.0, ALU.add, ALU.min), reads=[epsg, ec], writes=[egc])
                S.op("act", lambda e: e.activation(out=sg, in_=gc, func=AF.Sigmoid, scale=1.702), reads=[egc], writes=[esg])
                S.op("dve", lambda e: e.tensor_scalar(uc, psu[:], bgu_t[:, ex_i, 8 + f:9 + f], 7.0, ALU.add, ALU.min), reads=[epsu, ec], writes=[euc])
                S.op("dve", lambda e: e.tensor_scalar(uc, uc, -7.0, 1.0, ALU.max, ALU.add), reads=[euc], writes=[euc])
                S.op("pool", lambda e: e.tensor_tensor(gc, gc, sg, ALU.mult), reads=[egc, esg], writes=[egc])
                S.op("pool", lambda e: e.tensor_tensor(uc, uc, gc, ALU.mult), reads=[egc, euc], writes=[euc])
                S.op("pool", lambda e: e.tensor_tensor(ysA[f], uc, gbc[:], ALU.mult), reads=[euc, e_gbc], writes=[ysA.e[f]])
            for m in range(16):
                w, ew = load_wb(w_dn[ex_i], m * 128)
                ps, eps = getps("A")
                mm_acc(ps, eps, w, ew, ysA, 8)
                S.op("dve", lambda e, ps=ps, m=m: e.tensor_tensor(mg[m], mg[m], ps[:], ALU.add), reads=[mg.e[m], eps], writes=[mg.e[m]])
        for m in range(16):
            S.op("dve", lambda e, m=m: e.scalar_tensor_tensor(out=u[m], in0=h[m], scalar=ALPHA, in1=mg[m], op0=ALU.mult, op1=ALU.add),
                 reads=[h.e[m], mg.e[m]], writes=[u.e[m]])
        layernorm(u, h, 1, hb)
        for m in range(16):
            w, ew = load_wa(ple_wg, m * 128)
            psg, epsg = getps("A")
            mm_acc(psg, epsg, w, ew, hb, 16)
            psb, epsb = getps("B")
            wpt, ewpt = wp_t[m % 2], ewp[m % 2]
            S.dma(nextq(), wpt[:], ple_wp[:, m * 128:(m + 1) * 128].rearrange("(k p) d -> p k d", p=128), writes=[ewpt])
            for k in range(2):
                S.op("pe", lambda e, k=k, psb=psb, wpt=wpt: e.matmul(psb[:], wpt[:, k, :], ppt[k], start=(k == 0), stop=(k == 1)),
                     reads=[ewpt, ppt.e[k]], writes=[epsb], pe_chain=(k > 0))
            t, et = gettmp()
            S.op("act", lambda e, t=t, psg=psg: e.activation(out=t, in_=psg[:], func=AF.Sigmoid), reads=[epsg], writes=[et])
            S.op("dve", lambda e, t=t, psb=psb: e.tensor_tensor(t, t, psb[:], ALU.mult), reads=[et, epsb], writes=[et])
            S.op("dve", lambda e, t=t, m=m: e.scalar_tensor_tensor(out=u[m], in0=h[m], scalar=ALPHA, in1=t, op0=ALU.mult, op1=ALU.add),
                 reads=[h.e[m], et], writes=[u.e[m]])
        layernorm(u, mg, 2)
        for k in range(16):
            S.dma(nextq(), oT[k * 128:(k + 1) * 128, tk], mg[k], reads=[mg.e[k]], writes=[eo])
    S.wait_all("sp", [eo])
    cx.st.close()
    return nc


NWT = 22
TWO_PI = 2.0 * np.pi


def build_mixer(S_len, T=512):
    cx = Ctx(); nc = cx.nc; S = cx.S
    NB = S_len // T
    hT = cx.din("hT", [D, S_len]); Wm = cx.din("Wm", [D, NWT * 128]); pos = cx.din("pos", [S_len], I32)
    cpar = cx.din("cpar", [128, 64])
    c_ident = cx.din("c_ident", [128, 128]); c_ones = cx.din("c_ones", [128, 128]); c_wide = cx.din("c_wide", [128, 255])
    c_sel = cx.din("c_sel", [128, 2, 128]); c_rot = cx.din("c_rot", [128, 128]); c_tau = cx.din("c_tau", [128, T])
    w_alpha = cx.din("w_alpha", [128, 128])
    s5B = cx.din("s5B", [2, 128, 8, 128]); s5C = cx.din("s5C", [2, 128, 8, 128])
    yT = cx.dout("yT", [4, 256, S_len]); eo = cx.ent("yT")
    ec = cx.ent("consts")
    par = cx.sb("par", [128, 64]); ident = cx.sb("ident", [128, 128]); ones = cx.sb("ones", [128, 128]); wide = cx.sb("wide", [128, 255])
    sel = cx.sb("sel", [128, 2, 128]); rot = cx.sb("rot", [128, 128]); tau = cx.sb("tau", [128, T]); wal = cx.sb("wal", [128, 128])
    Bl = cx.sb("Bl", [128, 2, 8, 128]); Cl = cx.sb("Cl", [128, 2, 8, 128])
    for dst, src in ((par, cpar), (ident, c_ident), (ones, c_ones), (wide, c_wide), (sel, c_sel), (rot, c_rot), (tau, c_tau), (wal, w_alpha)):
        S.dma("sp", dst[:], src, writes=[ec])
    for ri in range(2):
        S.dma("sp", Bl[:, ri], s5B[ri], writes=[ec]); S.dma("sp", Cl[:, ri], s5C[ri], writes=[ec])
    CW = 0
    CB = 16
    DTB, ALOG, DSK = 20, 21, 22
    S5D = 24
    LRE, LIM = 26, 27
    LDT = 28
    RNW = 36
    INVF = 38
    LGAM = 39
    BAL = 40
    GNW = 41
    def pc(c):
        return par[:, c:c + 1]

    P = Tiles(cx, "P", NWT, T)
    xe = cx.sb("xe", [128, 4, T + 3]); e_xe = [cx.ent(f"xe{i}") for i in range(4)]
    hb = cx.sb("hb", [128, 16, T]); e_hb = cx.ent("hb")
    wa = [cx.sb(f"mwa{i}", [128, 16, 128]) for i in range(2)]; ewa = [cx.ent(f"mwa{i}") for i in range(2)]
    tmp = Tiles(cx, "mt", 12, T)
    Lg = Tiles(cx, "lg", 5, T)
    Vb = Tiles(cx, "vb", 2, T, BF16); prb = Tiles(cx, "prb", 4, T, BF16); cpr = [0]
    ident_b = cx.sb("ident_b", [128, 128], BF16); wide_b = cx.sb("wide_b", [128, 255], BF16)
    cnt = {"t": 0, "q": 0, "pa": 0, "pb": 0}
    psA = [cx.ps(f"mpA{i}") for i in range(2)]; epsA = [cx.ent(f"mpA{i}") for i in range(2)]
    psB = [cx.ps(f"mpB{i}") for i in range(2)]; epsB = [cx.ent(f"mpB{i}") for i in range(2)]
    psY = [cx.ps(f"mpY{i}") for i in range(2)]; epsY = [cx.ent(f"mpY{i}") for i in range(2)]
    psZ = [cx.ps(f"mpZ{i}") for i in range(2)]; epsZ = [cx.ent(f"mpZ{i}") for i in range(2)]
    carry = cx.sb("carry", [128, 3, 256]); e_carry = [[cx.ent(f"carry{i}_{c}") for c in range(256)] for i in range(3)]
    s5c = cx.sb("s5c", [128, 2, 8]); e_s5c = cx.ent("s5c")
    posf = cx.sb("posf", [128, T]); posi = cx.sb("posi", [128, T], I32); e_pos = cx.ent("pos")
    kint = cx.sb("kint", [128, T], I32); e_kint = cx.ent("kint")
    TS = T // 2
    Tp = cx.sb("Tp", [128, 2, 8, TS]); Tn = cx.sb("Tn", [128, 2, 8, TS]); e_tab = cx.ent("tab")
    sm = cx.sb("sm", [128, 64]); e_sm = cx.ent("sm")

    def gt():
        i = cnt["t"] % 12; cnt["t"] += 1
        return tmp[i], tmp.e[i]

    def gp(kind):
        lst, el = (psA, epsA) if kind == "A" else (psB, epsB)
        key = "pa" if kind == "A" else "pb"
        i = cnt[key] % 2; cnt[key] += 1
        return lst[i], el[i]

    def nextq():
        cnt["q"] += 1
        return "sp" if cnt["q"] % 2 else "pool"

    def dve(fn, r, w): S.op("dve", fn, reads=r, writes=w)
    def act(fn, r, w): S.op("act", fn, reads=r, writes=w)
    def pool(fn, r, w): S.op("pool", fn, reads=r, writes=w)
    def pe(fn, r, w, chain=False): S.op("pe", fn, reads=r, writes=w, pe_chain=chain)

    def sincos(ang, eang, out_sin, eout, shift):
        t, et = gt()
        dve(lambda e: e.tensor_scalar(t, ang, shift, 1.0 / TWO_PI, ALU.add, ALU.mult), [eang], [et])
        dve(lambda e: e.tensor_copy(kint[:], t), [et], [e_kint])
        dve(lambda e: e.tensor_copy(t, kint[:]), [e_kint], [et])
        t2, et2 = gt()
        dve(lambda e: e.tensor_scalar(t2, ang, shift, None, ALU.add), [eang], [et2])
        dve(lambda e: e.scalar_tensor_tensor(out=t2, in0=t, scalar=-6.28125, in1=t2, op0=ALU.mult, op1=ALU.add), [et2, et], [et2])
        dve(lambda e: e.scalar_tensor_tensor(out=t2, in0=t, scalar=-1.9353071795864769e-3, in1=t2, op0=ALU.mult, op1=ALU.add), [et2, et], [et2])
        dve(lambda e: e.tensor_scalar(t, t2, float(np.pi), -TWO_PI, ALU.is_gt, ALU.mult), [et2], [et])
        t3, et3 = gt()
        dve(lambda e: e.tensor_scalar(t3, t2, -float(np.pi), TWO_PI, ALU.is_lt, ALU.mult), [et2], [et3])
        dve(lambda e: e.tensor_tensor(t2, t2, t, ALU.add), [et2, et], [et2])
        dve(lambda e: e.tensor_tensor(t2, t2, t3, ALU.add), [et2, et3], [et2])
        dve(lambda e: e.tensor_scalar(t2, t2, float(np.pi), -float(np.pi), ALU.min, ALU.max), [et2], [et2])
        act(lambda e: e.activation(out=out_sin, in_=t2, func=AF.Sin), [et2], [eout])

    S.wait_all("dve", [ec]); S.wait_all("act", [ec])
    S.op("dve", lambda e: e.tensor_copy(ident_b[:], ident[:]), reads=[ec], writes=[ec])
    S.op("dve", lambda e: e.tensor_copy(wide_b[:], wide[:]), reads=[ec], writes=[ec])
    lr = sm[:, 0:1]; dtc = sm[:, 8:16]; acol = sm[:, 16:24]; thcol = sm[:, 24:32]
    dve(lambda e: e.tensor_scalar(lr, pc(LRE), -1e-4, None, ALU.min), [ec], [e_sm])
    act(lambda e: e.activation(out=dtc, in_=par[:, LDT:LDT + 8], func=AF.Exp), [ec], [e_sm])
    dve(lambda e: e.tensor_scalar(acol, dtc, lr, None, ALU.mult), [e_sm], [e_sm])
    dve(lambda e: e.tensor_scalar(thcol, dtc, pc(LIM), None, ALU.mult), [e_sm, ec], [e_sm])
    nacol = sm[:, 32:40]
    dve(lambda e: e.tensor_scalar(nacol, acol, -1.0, None, ALU.mult), [e_sm], [e_sm])
    for j in range(8):
        ang, eang = gt()
        dve(lambda e: e.tensor_scalar(ang, tau[:], thcol[:, j:j + 1], None, ALU.mult), [ec, e_sm], [eang])
        sn, esn = gt(); cs, ecs = gt(); mg_, emg = gt(); mn, emn = gt()
        sincos(ang, eang, sn, esn, 0.0)
        sincos(ang, eang, cs, ecs, float(np.pi / 2))
        act(lambda e: e.activation(out=mg_, in_=tau[:], func=AF.Exp, scale=acol[:, j:j + 1]), [ec, e_sm], [emg])
        act(lambda e: e.activation(out=mn, in_=tau[:], func=AF.Exp, scale=nacol[:, j:j + 1]), [ec, e_sm], [emn])
        dve(lambda e: e.tensor_tensor(Tp[:, 0, j, :], mg_[:, 0:TS], cs[:, 0:TS], ALU.mult), [emg, ecs], [e_tab])
        dve(lambda e: e.tensor_tensor(Tp[:, 1, j, :], mg_[:, 0:TS], sn[:, 0:TS], ALU.mult), [emg, esn], [e_tab])
        dve(lambda e: e.tensor_tensor(Tn[:, 0, j, :], mn[:, 0:TS], cs[:, 0:TS], ALU.mult), [emn, ecs], [e_tab])
        dve(lambda e: e.scalar_tensor_tensor(out=Tn[:, 1, j, :], in0=mn[:, 0:TS], scalar=-1.0, in1=sn[:, 0:TS], op0=ALU.mult, op1=ALU.mult), [emn, esn], [e_tab])
    abr = sm[:, 40:48]; abi = sm[:, 48:56]; fre = sm[:, 56:64]
    sm2 = cx.sb("sm2", [128, 64]); e_sm2 = cx.ent("sm2")
    fim = sm2[:, 0:8]; den = sm2[:, 8:9]; t8 = sm2[:, 16:24]; lbr = sm2[:, 24:32]; lbi = sm2[:, 32:40]
    dve(lambda e: e.tensor_copy(lbr, Tp[:, 0, :, 1]), [e_tab], [e_sm2])
    dve(lambda e: e.tensor_copy(lbi, Tp[:, 1, :, 1]), [e_tab], [e_sm2])
    dve(lambda e: e.tensor_scalar(abr, lbr, -1.0, None, ALU.add), [e_sm2], [e_sm])
    dve(lambda e: e.tensor_tensor(den, lr, lr, ALU.mult), [e_sm], [e_sm2])
    dve(lambda e: e.scalar_tensor_tensor(out=den, in0=pc(LIM), scalar=pc(LIM), in1=den, op0=ALU.mult, op1=ALU.add), [ec, e_sm2], [e_sm2])
    dve(lambda e: e.reciprocal(den, den), [e_sm2], [e_sm2])
    dve(lambda e: e.tensor_scalar(t8, lbi, pc(LIM), None, ALU.mult), [e_sm2, ec], [e_sm2])
    dve(lambda e: e.scalar_tensor_tensor(out=fre, in0=abr, scalar=lr, in1=t8, op0=ALU.mult, op1=ALU.add), [e_sm, e_sm2], [e_sm])
    dve(lambda e: e.tensor_scalar(fre, fre, den, None, ALU.mult), [e_sm, e_sm2], [e_sm])
    dve(lambda e: e.tensor_scalar(t8, abr, pc(LIM), None, ALU.mult), [e_sm, ec], [e_sm2])
    dve(lambda e: e.scalar_tensor_tensor(out=fim, in0=lbi, scalar=lr, in1=t8, op0=ALU.mult, op1=ALU.subtract), [e_sm, e_sm2], [e_sm2])
    dve(lambda e: e.tensor_scalar(fim, fim, den, None, ALU.mult), [e_sm2], [e_sm2])
    for j in range(8):
        a_, ea = gt(); b_, eb = gt(); a_ = a_[:, 0:TS]; b_ = b_[:, 0:TS]
        dve(lambda e: e.tensor_scalar(a_, Tn[:, 0, j, :], fre[:, j:j + 1], None, ALU.mult), [e_tab, e_sm], [ea])
        dve(lambda e: e.tensor_scalar(b_, Tn[:, 1, j, :], fre[:, j:j + 1], None, ALU.mult), [e_tab, e_sm], [eb])
        dve(lambda e: e.scalar_tensor_tensor(out=b_, in0=Tn[:, 0, j, :], scalar=fim[:, j:j + 1], in1=b_, op0=ALU.mult, op1=ALU.add), [e_tab, e_sm2, eb], [eb])
        dve(lambda e: e.scalar_tensor_tensor(out=t8[:, 0:1].to_broadcast([128, 1]) if False else a_, in0=Tn[:, 1, j, :], scalar=fim[:, j:j + 1], in1=a_, op0=ALU.mult, op1=ALU.subtract), [e_tab, e_sm2, ea], [ea])
        dve(lambda e: e.tensor_scalar(Tn[:, 0, j, :], a_, -1.0, None, ALU.mult), [ea], [e_tab])
        dve(lambda e: e.tensor_copy(Tn[:, 1, j, :], b_), [eb], [e_tab])
    for i in range(3):
        dve(lambda e, i=i: e.memset(carry[:, i, :], 0.0), [], e_carry[i])
    dve(lambda e: e.memset(s5c[:], 0.0), [], [e_s5c])
    for i in range(4):
        dve(lambda e, i=i: e.memset(xe[:, i, 0:3], 0.0), [], [e_xe[i]])
    nbal = sm2[:, 40:41]; nalog = sm2[:, 41:42]
    dve(lambda e: e.tensor_scalar(nbal, pc(BAL), -1.0, None, ALU.mult), [ec], [e_sm2])
    act(lambda e: e.activation(out=nalog, in_=pc(ALOG), func=AF.Exp), [ec], [e_sm2])
    dve(lambda e: e.tensor_scalar(nalog, nalog, -1.0, None, ALU.mult), [e_sm2], [e_sm2])
    gam = sm2[:, 42:43]
    act(lambda e: e.activation(out=gam, in_=pc(LGAM), func=AF.Exp), [ec], [e_sm2])

    ps4 = [(psA[0], epsA[0]), (psA[1], epsA[1]), (psB[0], epsB[0]), (psB[1], epsB[1])]
    c4 = [0]

    def chan_scan(mix, nch, k_ap, ek, q_ap, eq, a_of, v_of, out_of):
        LAG = 3
        state = {}

        def s1(e_):
            vt, evt, row, halves = v_of(e_)
            pb, epb = ps4[c4[0] % 4]; c4[0] += 1
            if halves is None:
                pe(lambda e: e.matmul(pb[:], ident_b[:, row:row + 1].to_broadcast([128, 128]), vt, start=True, stop=True), [ec, evt], [epb])
            else:
                (v0, ev0), (v1, ev1) = halves
                pe(lambda e: e.matmul(pb[0:64, :], ident_b[:, row:row + 1].to_broadcast([128, 64]), v0, start=True, stop=True), [ec, ev0], [epb])
                pe(lambda e: e.matmul(pb[64:128, :], ident_b[:, row:row + 1].to_broadcast([128, 64]), v1, start=True, stop=True), [ec, ev1], [epb])
            state[e_] = (pb, epb)

        def s2a(e_):
            pb, epb = state[e_]
            d1, ed1 = gt()
            dve(lambda e: e.tensor_tensor(d1, k_ap, pb[:], ALU.mult), [ek, epb], [ed1])
            state[e_] = (d1, ed1)

        def s2b(e_):
            d1, ed1 = state[e_]
            a_ap, ea = a_of(e_)
            st, est = gt()
            dve(lambda e: e.tensor_tensor_scan(st, a_ap, d1, carry[:, mix, e_:e_ + 1], ALU.mult, ALU.add), [ea, ed1, e_carry[mix][e_]], [est])
            act(lambda e: e.activation(out=carry[:, mix, e_:e_ + 1], in_=st[:, T - 1:T], func=AF.Copy), [est], [e_carry[mix][e_]])
            ip = cpr[0] % 4; cpr[0] += 1
            pr_, epr = prb[ip], prb.e[ip]
            pool(lambda e: e.tensor_tensor(pr_, st, q_ap, ALU.mult), [est, eq], [epr])
            state[e_] = (pr_, epr)

        for step in range(nch + LAG):
            if step < nch:
                s1(step)
            if 0 <= step - 2 < nch:
                s2a(step - 2)
            if step >= LAG:
                e_ = step - LAG
                s2b(e_)
                st, est = state.pop(e_)
                out_of(e_, st, est)

    for blk in range(NB):
        tk = slice(blk * T, (blk + 1) * T)
        S.dma("sp", hb[:], hT.rearrange("(k p) t -> p k t", p=128)[:, :, tk], writes=[e_hb])
        S.dma("pool", posi[:], pos[tk].partition_broadcast(128), writes=[e_pos])
        for wt in range(NWT):
            i = wt % 2
            S.dma(nextq(), wa[i][:], Wm[:, wt * 128:(wt + 1) * 128].rearrange("(k p) c -> p k c", p=128), writes=[ewa[i]])
            ps, eps = gp("A")
            for k in range(16):
                pe(lambda e, k=k: e.matmul(ps[:], wa[i][:, k, :], hb[:, k, :], start=(k == 0), stop=(k == 15)), [ewa[i], e_hb], [eps], chain=(k > 0))
            if 2 <= wt <= 5:
                ci = wt - 2
                act(lambda e: e.activation(out=xe[:, ci, 3:T + 3], in_=ps[:], func=AF.Copy), [eps], [e_xe[ci]])
            else:
                act(lambda e: e.activation(out=P[wt], in_=ps[:], func=AF.Copy), [eps], [P.e[wt]])
        for ci in range(4):
            wt = ci + 2
            dve(lambda e: e.tensor_scalar(P[wt], xe[:, ci, 3:T + 3], pc(CW + 4 * ci + 3), None, ALU.mult), [e_xe[ci], ec], [P.e[wt]])
            for k in range(3):
                dve(lambda e, k=k: e.scalar_tensor_tensor(out=P[wt], in0=xe[:, ci, k:T + k], scalar=pc(CW + 4 * ci + k), in1=P[wt], op0=ALU.mult, op1=ALU.add),
                    [e_xe[ci], ec, P.e[wt]], [P.e[wt]])
            dve(lambda e: e.tensor_copy(xe[:, ci, 0:3], xe[:, ci, T:T + 3]), [e_xe[ci]], [e_xe[ci]])
            act(lambda e: e.activation(out=P[wt], in_=P[wt], func=AF.Silu, bias=pc(CB + ci)), [P.e[wt], ec], [P.e[wt]])
        act(lambda e: e.activation(out=P[6], in_=P[6], func=AF.Exp, bias=pc(DTB)), [P.e[6], ec], [P.e[6]])
        act(lambda e: e.activation(out=P[6], in_=P[6], func=AF.Ln, bias=1.0), [P.e[6]], [P.e[6]])
        arow, earow = Lg[4], Lg.e[4]
        act(lambda e: e.activation(out=arow, in_=P[6], func=AF.Exp, scale=nalog), [P.e[6], e_sm2], [earow])
        abc = []
        for r in range(4):
            pb, epb = gp("B")
            pe(lambda e: e.matmul(pb[:], ident[:, r:r + 1].to_broadcast([128, 128]), arow, start=True, stop=True), [ec, earow], [epb])
            t, et = Lg[r], Lg.e[r]
            act(lambda e: e.activation(out=t, in_=pb[:], func=AF.Copy), [epb], [et])
            abc.append((t, et))
        xdt = []
        for i in range(2):
            pb, epb = gp("B")
            pe(lambda e: e.matmul(pb[:], sel[:, i, :], P[6], start=True, stop=True), [ec, P.e[6]], [epb])
            t, et = Vb[i], Vb.e[i]
            dve(lambda e: e.tensor_tensor(t, P[2 + i], pb[:], ALU.mult), [P.e[2 + i], epb], [et])
            xdt.append((t, et))
        def ssd_out(e_, st, est):
            i, el = divmod(e_, 128)
            pe(lambda e: e.matmul(psY[i][:], wide_b[:, 127 - el:255 - el], st, start=(el == 0), stop=(el == 127)), [ec, est], [epsY[i]], chain=(el > 0))
        chan_scan(0, 256, P[4], P.e[4], P[5], P.e[5], lambda e_: abc[e_ // 64], lambda e_: (xdt[e_ // 128][0], xdt[e_ // 128][1], e_ % 128, None), ssd_out)
        for i in range(2):
            t, et = gt()
            dve(lambda e: e.scalar_tensor_tensor(out=t, in0=P[2 + i], scalar=pc(DSK + i), in1=psY[i][:], op0=ALU.mult, op1=ALU.add), [P.e[2 + i], ec, epsY[i]], [et])
            act(lambda e: e.activation(out=P[i], in_=P[i], func=AF.Silu), [P.e[i]], [P.e[i]])
            dve(lambda e: e.tensor_tensor(t, t, P[i], ALU.mult), [et, P.e[i]], [et])
            S.dma(nextq(), yT[0, i * 128:(i + 1) * 128, tk], t, reads=[et], writes=[eo])
        dve(lambda e: e.tensor_copy(posf[:], posi[:]), [e_pos], [e_pos])
        ang, eang = gt()
        dve(lambda e: e.tensor_scalar(ang, posf[:], pc(INVF), None, ALU.mult), [e_pos, ec], [eang])
        sn, esn = gt(); cs, ecs = gt()
        sincos(ang, eang, sn, esn, 0.0); sincos(ang, eang, cs, ecs, float(np.pi / 2))
        for wt, scale in ((9, 1.0), (10, 0.125)):
            pb, epb = gp("B")
            pe(lambda e: e.matmul(pb[:], rot[:], P[wt], start=True, stop=True), [ec, P.e[wt]], [epb])
            t, et = gt()
            dve(lambda e: e.tensor_tensor(t, pb[:], sn, ALU.mult), [epb, esn], [et])
            dve(lambda e: e.tensor_tensor(P[wt], P[wt], cs, ALU.mult), [P.e[wt], ecs], [P.e[wt]])
            dve(lambda e: e.tensor_tensor(P[wt], P[wt], t, ALU.add), [P.e[wt], et], [P.e[wt]])
            if scale != 1.0:
                dve(lambda e: e.tensor_scalar(P[wt], P[wt], scale, None, ALU.mult), [P.e[wt]], [P.e[wt]])
        gam_t, egam = Lg[0], Lg.e[0]
        dve(lambda e: e.tensor_scalar(gam_t, tau[:], 0.0, gam, ALU.mult, ALU.add), [ec, e_sm2], [egam])
        def ret_out(e_, st, est):
            for hh in range(2):
                pe(lambda e, hh=hh: e.matmul(psY[hh][:], wide_b[hh * 64:(hh + 1) * 64, 127 - e_:255 - e_], st[hh * 64:(hh + 1) * 64, :], start=(e_ == 0), stop=(e_ == 127)),
                   [ec, est], [epsY[hh]], chain=(e_ > 0))
        for i in range(2):
            act(lambda e, i=i: e.activation(out=Vb[i], in_=P[11 + i], func=AF.Copy), [P.e[11 + i]], [Vb.e[i]])
        chan_scan(1, 128, P[10], P.e[10], P[9], P.e[9], lambda e_: (gam_t, egam),
                  lambda e_: (None, None, e_, ((Vb[0], Vb.e[0]), (Vb[1], Vb.e[1]))), ret_out)
        for hh in range(2):
            y, ey = gt()
            act(lambda e: e.activation(out=y, in_=psY[hh][:], func=AF.Copy), [epsY[hh]], [ey])
            sq, esq = gt()
            act(lambda e: e.activation(out=sq, in_=y, func=AF.Square), [ey], [esq])
            pa, epa = gp("A"); pb, epb = gp("B")
            pe(lambda e: e.matmul(pa[:], ones[:], y, start=True, stop=True), [ec, ey], [epa])
            pe(lambda e: e.matmul(pb[:], ones[:], sq, start=True, stop=True), [ec, esq], [epb])
            mu, emu = gt(); rs_, ers = gt()
            act(lambda e: e.activation(out=mu, in_=pa[:], func=AF.Copy, scale=1.0 / 128), [epa], [emu])
            dve(lambda e: e.tensor_tensor(rs_, mu, mu, ALU.mult), [emu], [ers])
            dve(lambda e: e.scalar_tensor_tensor(out=rs_, in0=pb[:], scalar=1.0 / 128, in1=rs_, op0=ALU.mult, op1=ALU.subtract), [epb, ers], [ers])
            dve(lambda e: e.tensor_scalar(rs_, rs_, LN_EPS, None, ALU.add), [ers], [ers])
            act(lambda e: e.activation(out=rs_, in_=rs_, func=AF.Sqrt), [ers], [ers])
            dve(lambda e: e.reciprocal(rs_, rs_), [ers], [ers])
            dve(lambda e: e.tensor_tensor(y, y, mu, ALU.subtract), [ey, emu], [ey])
            dve(lambda e: e.scalar_tensor_tensor(out=y, in0=y, scalar=pc(RNW + hh), in1=rs_, op0=ALU.mult, op1=ALU.mult), [ey, ers, ec], [ey])
            act(lambda e: e.activation(out=P[13 + hh], in_=P[13 + hh], func=AF.Silu), [P.e[13 + hh]], [P.e[13 + hh]])
            dve(lambda e: e.tensor_tensor(y, y, P[13 + hh], ALU.mult), [ey, P.e[13 + hh]], [ey])
            S.dma(nextq(), yT[2, hh * 128:(hh + 1) * 128, tk], y, reads=[ey], writes=[eo])
        pb, epb = gp("B")
        pe(lambda e: e.matmul(pb[:], wal[:], P[21], start=True, stop=True), [ec, P.e[21]], [epb])
        al, eal = Lg[1], Lg.e[1]
        act(lambda e: e.activation(out=al, in_=pb[:], func=AF.Exp, scale=-1.0, bias=nbal), [epb, e_sm2], [eal])
        act(lambda e: e.activation(out=al, in_=al, func=AF.Ln, bias=1.0), [eal], [eal])
        act(lambda e: e.activation(out=al, in_=al, func=AF.Exp, scale=-1.0 / 16.0), [eal], [eal])
        dve(lambda e: e.tensor_scalar(P[15], P[15], float(128 ** -0.5), None, ALU.mult), [P.e[15]], [P.e[15]])
        def gla_out(e_, st, est):
            i, el = divmod(e_, 128)
            pe(lambda e: e.matmul(psY[i][:], wide_b[:, 127 - el:255 - el], st, start=(el == 0), stop=(el == 127)), [ec, est], [epsY[i]], chain=(el > 0))
        for i in range(2):
            act(lambda e, i=i: e.activation(out=Vb[i], in_=P[17 + i], func=AF.Copy), [P.e[17 + i]], [Vb.e[i]])
        chan_scan(2, 256, P[16], P.e[16], P[15], P.e[15], lambda e_: (al, eal), lambda e_: (Vb[e_ // 128], Vb.e[e_ // 128], e_ % 128, None), gla_out)
        ys_ = []
        pa, epa = gp("A")
        for i in range(2):
            y, ey = gt(); sq, esq = gt()
            act(lambda e: e.activation(out=y, in_=psY[i][:], func=AF.Copy), [epsY[i]], [ey])
            act(lambda e: e.activation(out=sq, in_=y, func=AF.Square), [ey], [esq])
            pe(lambda e, i=i: e.matmul(pa[:], ones[:], sq, start=(i == 0), stop=(i == 1)), [ec, esq], [epa], chain=(i > 0))
            ys_.append((y, ey))
        rs_, ers = gt()
        dve(lambda e: e.tensor_scalar(rs_, pa[:], 1.0 / 256, RMS_EPS, ALU.mult, ALU.add), [epa], [ers])
        act(lambda e: e.activation(out=rs_, in_=rs_, func=AF.Sqrt), [ers], [ers])
        dve(lambda e: e.reciprocal(rs_, rs_), [ers], [ers])
        for i in range(2):
            y, ey = ys_[i]
            dve(lambda e: e.scalar_tensor_tensor(out=y, in0=y, scalar=pc(GNW + i), in1=rs_, op0=ALU.mult, op1=ALU.mult), [ey, ers, ec], [ey])
            act(lambda e: e.activation(out=P[19 + i], in_=P[19 + i], func=AF.Silu), [P.e[19 + i]], [P.e[19 + i]])
            dve(lambda e: e.tensor_tensor(y, y, P[19 + i], ALU.mult), [ey, P.e[19 + i]], [ey])
            S.dma(nextq(), yT[3, i * 128:(i + 1) * 128, tk], y, reads=[ey], writes=[eo])
        for hf in range(2):
         hs = slice(hf * TS, (hf + 1) * TS)
         for i in range(2):
             for jj in range(4):
                 j = 4 * i + jj
                 xr, exr = gt(); xi, exi = gt(); xr = xr[:, 0:TS]; xi = xi[:, 0:TS]
                 for ri, (xx, exx) in enumerate(((xr, exr), (xi, exi))):
                     pb, epb = gp("B")
                     pe(lambda e: e.matmul(pb[:, 0:TS], Bl[:, ri, j, :], P.t[:, 7 + i, hs], start=True, stop=True), [ec, P.e[7 + i]], [epb])
                     act(lambda e: e.activation(out=xx, in_=pb[:, 0:TS], func=AF.Copy), [epb], [exx])
                 wr, ewr = gt(); wi, ewi = gt(); t1, et1 = gt(); wr = wr[:, 0:TS]; wi = wi[:, 0:TS]; t1 = t1[:, 0:TS]
                 dve(lambda e: e.tensor_tensor(wr, xr, Tn[:, 0, j, :], ALU.mult), [exr, e_tab], [ewr])
                 pool(lambda e: e.tensor_tensor(t1, xi, Tn[:, 1, j, :], ALU.mult), [exi, e_tab], [et1])
                 dve(lambda e: e.tensor_tensor(wr, wr, t1, ALU.subtract), [ewr, et1], [ewr])
                 pool(lambda e: e.tensor_tensor(wi, xr, Tn[:, 1, j, :], ALU.mult), [exr, e_tab], [ewi])
                 dve(lambda e: e.tensor_tensor(t1, xi, Tn[:, 0, j, :], ALU.mult), [exi, e_tab, ewr], [et1])
                 dve(lambda e: e.tensor_tensor(wi, wi, t1, ALU.add), [ewi, et1], [ewi])
                 dve(lambda e: e.tensor_tensor_scan(wr, ones[:, 0:1].to_broadcast([128, TS]), wr, s5c[:, 0, j:j + 1], ALU.mult, ALU.add), [ec, ewr, e_s5c], [ewr])
                 dve(lambda e: e.tensor_tensor_scan(wi, ones[:, 0:1].to_broadcast([128, TS]), wi, s5c[:, 1, j:j + 1], ALU.mult, ALU.add), [ec, ewi, e_s5c], [ewi])
                 hr, ehr = xr, exr; hi, ehi = xi, exi
                 dve(lambda e: e.tensor_tensor(hr, wr, Tp[:, 0, j, :], ALU.mult), [ewr, e_tab], [ehr])
                 pool(lambda e: e.tensor_tensor(t1, wi, Tp[:, 1, j, :], ALU.mult), [ewi, e_tab], [et1])
                 dve(lambda e: e.tensor_tensor(hr, hr, t1, ALU.subtract), [ehr, et1], [ehr])
                 pool(lambda e: e.tensor_tensor(hi, wr, Tp[:, 1, j, :], ALU.mult), [ewr, e_tab], [ehi])
                 dve(lambda e: e.tensor_tensor(t1, wi, Tp[:, 0, j, :], ALU.mult), [ewi, e_tab, ehr], [et1])
                 dve(lambda e: e.tensor_tensor(hi, hi, t1, ALU.add), [ehi, et1], [ehi])
                 c1 = sm2[:, 48:49]
                 dve(lambda e: e.tensor_scalar(c1, hi[:, TS - 1:TS], lbi[:, j:j + 1], None, ALU.mult), [ehi, e_sm2], [e_sm2])
                 dve(lambda e: e.scalar_tensor_tensor(out=s5c[:, 0, j:j + 1], in0=hr[:, TS - 1:TS], scalar=lbr[:, j:j + 1], in1=c1, op0=ALU.mult, op1=ALU.subtract), [ehr, e_sm2], [e_s5c])
                 dve(lambda e: e.tensor_scalar(c1, hi[:, TS - 1:TS], lbr[:, j:j + 1], None, ALU.mult), [ehi, e_sm2], [e_sm2])
                 dve(lambda e: e.scalar_tensor_tensor(out=s5c[:, 1, j:j + 1], in0=hr[:, TS - 1:TS], scalar=lbi[:, j:j + 1], in1=c1, op0=ALU.mult, op1=ALU.add), [ehr, e_sm2], [e_s5c])
                 pe(lambda e: e.matmul(psY[0][:, 0:TS], Cl[:, 0, j, :], hr, start=(jj == 0), stop=(jj == 3)), [ec, ehr], [epsY[0]], chain=(jj > 0))
                 pe(lambda e: e.matmul(psZ[0][:, 0:TS], Cl[:, 1, j, :], hi, start=(jj == 0), stop=(jj == 3)), [ec, ehi], [epsZ[0]], chain=(jj > 0))
             y, ey = gt(); y = y[:, 0:TS]
             act(lambda e: e.activation(out=y, in_=psZ[0][:, 0:TS], func=AF.Copy), [epsZ[0]], [ey])
             dve(lambda e: e.tensor_tensor(y, psY[0][:, 0:TS], y, ALU.subtract), [epsY[0], ey], [ey])
             dve(lambda e: e.scalar_tensor_tensor(out=y, in0=P.t[:, 7 + i, hs], scalar=pc(S5D + i), in1=y, op0=ALU.mult, op1=ALU.add), [P.e[7 + i], ec, ey], [ey])
             act(lambda e: e.activation(out=y, in_=y, func=AF.Gelu), [ey], [ey])
             S.dma(nextq(), yT[1, i * 128:(i + 1) * 128, blk * T + hf * TS:blk * T + (hf + 1) * TS], y, reads=[ey], writes=[eo])

    S.wait_all("sp", [eo]); S.wait_all("pool", [eo])
    cx.st.close()
    return nc


OFF = dict(z=0, xs=1024, B=2048, C=2304, dt=2560, u=2576, rq=3600, rk=4112, rv=4624, rg=5648, gq=6672, gk=7184, gv=7696, gr=8720, gc=9744, gate=9760)


def _mixer_consts(T=512):
    c = {}
    c["c_ident"] = np.eye(128, dtype=np.float32); c["c_ones"] = np.ones((128, 128), np.float32)
    w = np.zeros((128, 255), np.float32); w[:, 127] = 1.0; c["c_wide"] = w
    sel = np.zeros((128, 2, 128), np.float32)
    for i in range(2):
        for m in range(128):
            sel[2 * i + m // 64, i, m] = 1.0
    c["c_sel"] = sel
    rot = np.zeros((128, 128), np.float32)
    for m in range(128):
        if m % 64 < 32:
            rot[m + 32, m] = -1.0
        else:
            rot[m - 32, m] = 1.0
    c["c_rot"] = rot
    c["c_tau"] = np.tile(np.arange(T, dtype=np.float32)[None, :], (128, 1))
    return c


def _mixer_inputs(inp, l, j, T=512):
    g = j // 2
    w_in = inp["w_in"][l]
    Wm = np.zeros((D, NWT * 128), np.float32)
    def put(t, c0, n):
        Wm[:, t * 128:t * 128 + n] = w_in[:, c0:c0 + n]
    put(0, OFF["z"] + 256 * j, 256); put(2, OFF["xs"] + 256 * j, 256); put(4, OFF["B"] + 128 * g, 128); put(5, OFF["C"] + 128 * g, 128)
    put(6, OFF["dt"] + 4 * j, 4); put(7, OFF["u"] + 256 * j, 256); put(9, OFF["rq"] + 128 * j, 128); put(10, OFF["rk"] + 128 * j, 128)
    put(11, OFF["rv"] + 256 * j, 256); put(13, OFF["rg"] + 256 * j, 256); put(15, OFF["gq"] + 128 * j, 128); put(16, OFF["gk"] + 128 * j, 128)
    put(17, OFF["gv"] + 256 * j, 256); put(19, OFF["gr"] + 256 * j, 256); put(21, OFF["gc"], 16)
    par = np.zeros((128, 64), np.float32)
    p = np.arange(128)
    cw, cb = inp["ssd_conv_w"][l], inp["ssd_conv_b"][l]
    chans = [256 * j + p, 256 * j + 128 + p, 1024 + 128 * g + p, 1280 + 128 * g + p]
    for ci, ch in enumerate(chans):
        for k in range(4):
            par[:, 4 * ci + k] = cw[k, ch]
        par[:, 16 + ci] = cb[ch]
    par[0:4, 20] = inp["ssd_dt_bias"][l][4 * j:4 * j + 4]; par[0:4, 21] = inp["ssd_a_log"][l][4 * j:4 * j + 4]
    for i in range(2):
        par[:, 22 + i] = inp["ssd_d"][l][4 * j + 2 * i + p // 64]
        par[:, 24 + i] = inp["s5_d"][l][256 * j + 128 * i + p]
        par[:, 36 + i] = inp["ret_norm_w"][l][(2 * j + i) * 128 + p]
        par[:, 41 + i] = inp["gla_norm_w"][l][256 * j + 128 * i + p]
    par[:, 26] = inp["s5_lambda_re"][l][p % 64]; par[:, 27] = inp["s5_lambda_im"][l][p % 64]
    for jt in range(8):
        par[:, 28 + jt] = inp["s5_log_dt"][l][16 * j + 2 * jt + p // 64]
    inv_freq = (1.0 / (np.float32(10000.0) ** (np.arange(32, dtype=np.float32) / np.float32(32)))).astype(np.float32)
    par[:, 38] = inv_freq[(p % 64) % 32]
    lg = np.log1p(-np.exp2(-5.0 - np.arange(8, dtype=np.float32))).astype(np.float32)
    par[:, 39] = lg[2 * j + p // 64]
    par[:, 40] = inp["gla_b_alpha"][l][128 * j + p]
    wal = np.zeros((128, 128), np.float32); wal[0:16] = inp["gla_w_alpha"][l][:, 128 * j:128 * j + 128]
    s5B = np.zeros((2, 128, 8, 128), np.float32); s5C = np.zeros((2, 128, 8, 128), np.float32)
    for ri, (bn, cn) in enumerate((("s5_b_re", "s5_c_re"), ("s5_b_im", "s5_c_im"))):
        bb, cc = inp[bn][l], inp[cn][l]
        for jt in range(8):
            for g2 in range(2):
                gl = 2 * jt + g2
                G = 16 * j + gl
                k0 = (gl % 8) * 16
                s5B[ri, k0:k0 + 16, jt, g2 * 64:(g2 + 1) * 64] = bb[G].T
                s5C[ri, g2 * 64:(g2 + 1) * 64, jt, k0:k0 + 16] = cc[G].T
    d = dict(Wm=Wm, cpar=par, w_alpha=wal, s5B=s5B, s5C=s5C)
    d.update(_mixer_consts(T))
    return d


_PROGS = {}


def _prog(kind, n):
    key = (kind, n)
    if key not in _PROGS:
        _PROGS[key] = build_mixer(n) if kind == "m" else build_rest(n)
    return _PROGS[key]


def kernel(**inp):
    inp = {k: np.asarray(v) for k, v in inp.items()}
    x = inp["x"]; Bn, Sn, _ = x.shape
    NQ = 4; NT = Sn // NQ
    hT = [np.ascontiguousarray(x[b].T) for b in range(Bn)]
    rconst = dict(c_ones=np.ones((128, 128), np.float32), c_ident=np.eye(128, dtype=np.float32), c_sel=np.zeros((32, 32 * 128), np.float32))
    for l in range(2):
        mi = [_mixer_inputs(inp, l, j) for j in range(4)]
        ins = []
        for c in range(8):
            b, j = divmod(c, 4)
            d = dict(mi[j]); d["hT"] = hT[b]; d["pos"] = np.ascontiguousarray(inp["positions"][b].astype(np.int32))
            ins.append(d)
        res = run_bass_kernel_spmd(_prog("m", Sn), ins, core_ids=list(range(8)))
        ym = [r["yT"] for r in res.results]
        W = dict(w_gate=np.ascontiguousarray(inp["w_in"][l][:, OFF["gate"]:]), w_branch=inp["w_branch"][l], w_out=inp["w_out"][l],
                 ssd_norm_w=inp["ssd_norm_w"][l], s5_w_glu=inp["s5_w_glu"][l], s5_b_glu=inp["s5_b_glu"][l],
                 ln1_w=inp["ln1_w"][l], ln1_b=inp["ln1_b"][l], ln2_w=inp["ln2_w"][l], ln2_b=inp["ln2_b"][l], ln3_w=inp["ln3_w"][l], ln3_b=inp["ln3_b"][l],
                 router_w=inp["router_w"][l], router_b=inp["router_b"][l], moe_w_gate_up=inp["moe_w_gate_up"][l], moe_b_gate_up=inp["moe_b_gate_up"][l],
                 moe_w_down=inp["moe_w_down"][l], moe_b_down=inp["moe_b_down"][l], ple_w_gate=inp["ple_w_gate"][l], ple_w_proj=inp["ple_w_proj"][l])
        ins = []
        for c in range(8):
            b, q = divmod(c, 4)
            ts = slice(q * NT, (q + 1) * NT)
            ysT = np.concatenate([ym[b * 4 + j][:, :, ts] for j in range(4)], axis=1)
            d = dict(hT=np.ascontiguousarray(hT[b][:, ts]), ysT=np.ascontiguousarray(ysT), pT=np.ascontiguousarray(inp["p"][l, b, ts].T))
            d.update(W); d.update(rconst)
            ins.append(d)
        res = run_bass_kernel_spmd(_prog("r", NT), ins, core_ids=list(range(8)))
        hT = [np.concatenate([res.results[b * 4 + q]["oT"] for q in range(4)], axis=1) for b in range(Bn)]
    return np.ascontiguousarray(np.stack([h.T for h in hT], 0)).astype(np.float32)
```

```python
import numpy as np
import concourse.bass as bass
import concourse.mybir as mybir

F32 = mybir.dt.float32
F32R = mybir.dt.float32r
BF16 = mybir.dt.bfloat16
I32 = mybir.dt.int32
AF = mybir.ActivationFunctionType
ALU = mybir.AluOpType
AX = mybir.AxisListType

SEM_ROLL = 12000


class Ent:
    __slots__ = ("name", "lastw", "reads", "dsem", "dcount")

    def __init__(self, name):
        self.name = name
        self.lastw = None
        self.reads = {}
        self.dsem = None
        self.dcount = 0


class Sched:
    def __init__(self, nc, stack):
        self.nc = nc
        self.stack = stack
        self.eng = {"pe": nc.tensor, "dve": nc.vector, "act": nc.scalar, "pool": nc.gpsimd, "sp": nc.sync}
        self.sems = {}
        self.cur = {}
        self.epoch = {e: 0 for e in self.eng}
        self.seen = {e: {} for e in self.eng}
        self.nsem = 0
        self.ninst = 0
        for e in self.eng:
            self._newsem(e)

    def _alloc(self, name):
        self.nsem += 1
        return self.stack.enter_context(self.nc.semaphore(name))

    def _newsem(self, e):
        key = f"{e}{self.epoch[e]}"
        self.epoch[e] += 1
        self.sems[key] = self._alloc("s_" + key)
        self.cur[e] = [key, 0]

    def ent(self, name):
        return Ent(name)

    def _deps(self, e, reads, writes):
        deps = {}
        def add(ev):
            if ev is None:
                return
            k, v = ev
            if deps.get(k, 0) < v:
                deps[k] = v
        for r in reads:
            add(r.lastw)
        for w in writes:
            add(w.lastw)
            for k, v in w.reads.items():
                add((k, v))
        return deps

    def _wait(self, e, deps, skip_self_pe=False):
        seen = self.seen[e]
        engine = self.eng[e]
        for k, v in deps.items():
            if skip_self_pe and k.startswith("pe") and e == "pe":
                continue
            if seen.get(k, 0) >= v:
                continue
            engine.wait_ge(self.sems[k], v)
            seen[k] = v

    def _mark(self, ev, reads, writes):
        k, v = ev
        for r in reads:
            if r.reads.get(k, 0) < v:
                r.reads[k] = v
        for w in writes:
            w.lastw = ev
            w.reads = {}

    def op(self, e, fn, reads=(), writes=(), pe_chain=False):
        deps = self._deps(e, reads, writes)
        self._wait(e, deps, skip_self_pe=pe_chain)
        inst = fn(self.eng[e])
        cur = self.cur[e]
        cur[1] += 1
        inst.then_inc(self.sems[cur[0]], 1)
        ev = (cur[0], cur[1])
        self._mark(ev, reads, writes)
        self.ninst += 1
        if cur[1] >= SEM_ROLL:
            self._newsem(e)
        return inst

    def dma(self, q, out, in_, reads=(), writes=(), sem_ent=None, **kw):
        deps = self._deps(q, reads, writes)
        self._wait(q, deps)
        se = sem_ent if sem_ent is not None else writes[0]
        if se.dsem is None:
            se.dsem = f"d{self.nsem}_{se.name}"
            self.sems[se.dsem] = self._alloc(se.dsem)
        se.dcount += 16
        self.eng[q].dma_start(out=out, in_=in_, **kw).then_inc(self.sems[se.dsem], 16)
        ev = (se.dsem, se.dcount)
        self._mark(ev, reads, writes)
        self.ninst += 1

    def wait_all(self, e, ents):
        deps = self._deps(e, ents, ents)
        self._wait(e, deps)


import contextlib
from concourse.bass_utils import run_bass_kernel_spmd

D = 2048
ALPHA = 4.0 ** 0.25
LN_EPS = 1e-5
RMS_EPS = 1e-6


class Ctx:
    def __init__(self):
        self.nc = bass.Bass("TRN2", target_bir_lowering=False)
        self.st = contextlib.ExitStack()
        self.S = Sched(self.nc, self.st)
        self.n = 0

    def din(self, name, shape, dt=F32):
        return self.nc.dram_tensor(name, list(shape), dt, kind="ExternalInput").ap()

    def dout(self, name, shape, dt=F32):
        return self.nc.dram_tensor(name, list(shape), dt, kind="ExternalOutput").ap()

    def sb(self, name, shape, dt=F32):
        t = self.st.enter_context(self.nc.sbuf_tensor(name, list(shape), dt))
        return t

    def ps(self, name, shape=(128, 512), dt=F32):
        return self.st.enter_context(self.nc.psum_tensor(name, list(shape), dt))

    def ent(self, name):
        return self.S.ent(name)


class Tiles:
    def __init__(self, cx, name, n, w, dt=F32):
        self.t = cx.sb(name, [128, n, w], dt)
        self.e = [cx.ent(f"{name}{i}") for i in range(n)]
        self.n = n

    def __getitem__(self, i):
        return self.t[:, i, :]


def build_rest(NT, T=512, NE=32):
    cx = Ctx(); nc = cx.nc; S = cx.S
    NB = NT // T
    hT = cx.din("hT", [D, NT]); ysT = cx.din("ysT", [4, 1024, NT]); pT = cx.din("pT", [256, NT])
    w_gate = cx.din("w_gate", [D, 4 * D]); w_branch = cx.din("w_branch", [4, 1024, D]); w_out = cx.din("w_out", [D, D])
    ssd_norm_w = cx.din("ssd_norm_w", [1024]); w_glu = cx.din("s5_w_glu", [1024, 1024]); b_glu = cx.din("s5_b_glu", [1024])
    lnw = [cx.din(f"ln{i}_w", [D]) for i in (1, 2, 3)]; lnb = [cx.din(f"ln{i}_b", [D]) for i in (1, 2, 3)]
    router_w = cx.din("router_w", [D, NE]); router_b = cx.din("router_b", [NE])
    w_gu = cx.din("moe_w_gate_up", [NE, D, D]); b_gu = cx.din("moe_b_gate_up", [NE, D])
    w_dn = cx.din("moe_w_down", [NE, 1024, D]); b_dn = cx.din("moe_b_down", [NE, D])
    ple_wg = cx.din("ple_w_gate", [D, D]); ple_wp = cx.din("ple_w_proj", [256, D])
    c_ones = cx.din("c_ones", [128, 128]); c_ident = cx.din("c_ident", [128, 128]); c_sel = cx.din("c_sel", [32, 32 * 128])
    oT = cx.dout("oT", [D, NT])
    eo = cx.ent("oT")

    ec = cx.ent("consts")
    ones = cx.sb("ones", [128, 128]); ident = cx.sb("ident", [128, 128])
    lnw_t = cx.sb("lnw_t", [128, 3, 16]); lnb_t = cx.sb("lnb_t", [128, 3, 16])
    snw_t = cx.sb("snw_t", [128, 8]); bglu_t = cx.sb("bglu_t", [128, 8])
    bgu_t = cx.sb("bgu_t", [128, NE, 16]); bdn_t = cx.sb("bdn_t", [NE, D])
    rw_t = cx.sb("rw_t", [128, 16, NE]); rb_t = cx.sb("rb_t", [128, NE])
    wp_t = [cx.sb(f"wp_t{i}", [128, 2, 128]) for i in range(2)]; ewp = [cx.ent(f"wp{i}") for i in range(2)]
    S.dma("sp", ones[:], c_ones, writes=[ec]); S.dma("sp", ident[:], c_ident, writes=[ec])
    for i in range(3):
        S.dma("sp", lnw_t[:, i, :], lnw[i].rearrange("(t p) -> p t", p=128), writes=[ec], allow_slow_non_contiguous=True)
        S.dma("sp", lnb_t[:, i, :], lnb[i].rearrange("(t p) -> p t", p=128), writes=[ec], allow_slow_non_contiguous=True)
    S.dma("sp", snw_t[:], ssd_norm_w.rearrange("(t p) -> p t", p=128), writes=[ec], allow_slow_non_contiguous=True)
    S.dma("sp", bglu_t[:], b_glu.rearrange("(t p) -> p t", p=128), writes=[ec], allow_slow_non_contiguous=True)
    for e8 in range(NE // 8):
        S.dma("sp", bgu_t[:, e8 * 8:(e8 + 1) * 8, :], b_gu[e8 * 8:(e8 + 1) * 8].rearrange("e (t p) -> p e t", p=128), writes=[ec], allow_slow_non_contiguous=True)
    S.dma("sp", bdn_t[:], b_dn, writes=[ec])
    S.dma("sp", rw_t[:], router_w.rearrange("(k p) e -> p k e", p=128), writes=[ec])
    S.dma("sp", rb_t[:], router_b.partition_broadcast(128), writes=[ec])

    h = Tiles(cx, "h", 16, T); u = Tiles(cx, "u", 16, T); mg = Tiles(cx, "mg", 16, T)
    hb = Tiles(cx, "hb", 16, T, BF16); mgb = Tiles(cx, "mgb", 16, T, BF16)
    ysA = Tiles(cx, "ysA", 8, T, BF16)
    ppt = Tiles(cx, "ppt", 2, T)
    tmp = Tiles(cx, "tmp", 4, T)
    mean = cx.sb("mean", [128, T]); e_mean = cx.ent("mean")
    rstd = cx.sb("rstd", [128, T]); e_rstd = cx.ent("rstd")
    gT = cx.sb("gT", [NE, T]); e_gT = cx.ent("gT")
    gbc = mean; e_gbc = e_mean
    rs = cx.sb("rs", [128, 160]); e_rs = cx.ent("rs")
    NWA = 4
    wab = [cx.sb(f"wab{i}", [128, 16, 128], BF16) for i in range(NWA)]; ewab = [cx.ent(f"wab{i}") for i in range(NWA)]
    wbb = [cx.sb(f"wbb{i}", [128, 8, 128], BF16) for i in range(NWA)]; ewbb = [cx.ent(f"wbb{i}") for i in range(NWA)]
    cnt_c = [0]
    def cast(dst, edst, src, esrc):
        cnt_c[0] += 1
        if cnt_c[0] % 2:
            S.op("dve", lambda e: e.tensor_copy(dst, src), reads=[esrc], writes=[edst])
        else:
            S.op("act", lambda e: e.activation(out=dst, in_=src, func=AF.Copy), reads=[esrc], writes=[edst])
    cnt = {"wa": 0, "wb": 0, "tmp": 0, "ps": 0, "q": 0}
    psA = [cx.ps(f"psA{i}") for i in range(3)]; epsA = [cx.ent(f"psA{i}") for i in range(3)]
    psB = [cx.ps(f"psB{i}") for i in range(3)]; epsB = [cx.ent(f"psB{i}") for i in range(3)]
    psS = cx.ps("psS"); epsS = cx.ent("psS")
    psQ = cx.ps("psQ"); epsQ = cx.ent("psQ")
    pc = {"A": 0, "B": 0}

    def nextq():
        return "sp"

    def load_wa(src2d, c0):
        i = cnt["wa"] % NWA; cnt["wa"] += 1
        S.dma("pool", wab[i][:], src2d[:, c0:c0 + 128].rearrange("(k p) c -> p k c", p=128), writes=[ewab[i]])
        return wab[i], ewab[i]

    def load_wb(src2d, c0):
        i = cnt["wb"] % NWA; cnt["wb"] += 1
        S.dma("pool", wbb[i][:], src2d[:, c0:c0 + 128].rearrange("(k p) c -> p k c", p=128), writes=[ewbb[i]])
        return wbb[i], ewbb[i]

    def getps(kind):
        lst, el = (psA, epsA) if kind == "A" else (psB, epsB)
        i = pc[kind] % 3; pc[kind] += 1
        return lst[i], el[i]

    def gettmp():
        i = cnt["tmp"] % 4; cnt["tmp"] += 1
        return tmp[i], tmp.e[i]

    def mm_acc(ps, eps, w, ew, acts, nk):
        for k in range(nk):
            S.op("pe", lambda e, k=k: e.matmul(ps[:], w[:, k, :], acts[k], start=(k == 0), stop=(k == nk - 1)),
                 reads=[ew, acts.e[k]], writes=[eps], pe_chain=(k > 0))

    def stats(src, idxs, nfeat, eps_val):
        n = len(idxs)
        for j, k in enumerate(idxs):
            S.op("pe", lambda e, k=k, j=j: e.matmul(psS[:], ones[:], src[k], start=(j == 0), stop=(j == n - 1)),
                 reads=[ec, src.e[k]], writes=[epsS], pe_chain=(j > 0))
        for j, k in enumerate(idxs):
            t, et = gettmp()
            S.op("act", lambda e, k=k, t=t: e.activation(out=t, in_=src[k], func=AF.Square), reads=[src.e[k]], writes=[et])
            S.op("pe", lambda e, t=t, j=j: e.matmul(psQ[:], ones[:], t, start=(j == 0), stop=(j == n - 1)),
                 reads=[ec, et], writes=[epsQ], pe_chain=(j > 0))
        return n

    def layernorm(src, dst, li, dstb=None):
        stats(src, list(range(16)), D, LN_EPS)
        S.op("act", lambda e: e.activation(out=mean[:], in_=psS[:], func=AF.Copy, scale=1.0 / D), reads=[epsS], writes=[e_mean])
        t, et = gettmp()
        S.op("dve", lambda e: e.tensor_tensor(t, mean[:], mean[:], ALU.mult), reads=[e_mean], writes=[et])
        S.op("dve", lambda e: e.scalar_tensor_tensor(out=rstd[:], in0=psQ[:], scalar=1.0 / D, in1=t, op0=ALU.mult, op1=ALU.subtract),
             reads=[epsQ, et], writes=[e_rstd])
        S.op("dve", lambda e: e.tensor_scalar(rstd[:], rstd[:], LN_EPS, None, ALU.add), reads=[e_rstd], writes=[e_rstd])
        S.op("act", lambda e: e.activation(out=rstd[:], in_=rstd[:], func=AF.Sqrt), reads=[e_rstd], writes=[e_rstd])
        S.op("dve", lambda e: e.reciprocal(rstd[:], rstd[:]), reads=[e_rstd], writes=[e_rstd])
        for k in range(16):
            t, et = gettmp()
            S.op("dve", lambda e, k=k, t=t: e.tensor_tensor(t, src[k], mean[:], ALU.subtract), reads=[src.e[k], e_mean], writes=[et])
            S.op("dve", lambda e, t=t: e.tensor_tensor(t, t, rstd[:], ALU.mult), reads=[et, e_rstd], writes=[et])
            S.op("act", lambda e, k=k, t=t: e.activation(out=dst[k], in_=t, func=AF.Identity, scale=lnw_t[:, li, k:k + 1], bias=lnb_t[:, li, k:k + 1]),
                 reads=[et, ec], writes=[dst.e[k]])
            if dstb is not None:
                S.op("dve", lambda e, k=k: e.tensor_copy(dstb[k], dst[k]), reads=[dst.e[k]], writes=[dstb.e[k]])

    for blk in range(NB):
        tk = slice(blk * T, (blk + 1) * T)
        for k in range(16):
            S.dma(nextq(), h[k], hT[k * 128:(k + 1) * 128, tk], writes=[h.e[k]])
            cast(hb[k], hb.e[k], h[k], h.e[k])
        for k in range(2):
            S.dma(nextq(), ppt[k], pT[k * 128:(k + 1) * 128, tk], writes=[ppt.e[k]])
        for n in range(4):
            for k in range(8):
                S.dma(nextq(), u[k], ysT[n, k * 128:(k + 1) * 128, tk], writes=[u.e[k]])
            if n == 0:
                for g in range(2):
                    stats(u, [4 * g + i for i in range(4)], 512, RMS_EPS)
                    S.op("dve", lambda e: e.tensor_scalar(rstd[:], psQ[:], 1.0 / 512, RMS_EPS, ALU.mult, ALU.add), reads=[epsQ], writes=[e_rstd])
                    S.op("act", lambda e: e.activation(out=rstd[:], in_=rstd[:], func=AF.Sqrt), reads=[e_rstd], writes=[e_rstd])
                    S.op("dve", lambda e: e.reciprocal(rstd[:], rstd[:]), reads=[e_rstd], writes=[e_rstd])
                    for i in range(4):
                        k = 4 * g + i
                        S.op("dve", lambda e, k=k: e.scalar_tensor_tensor(out=ysA[k], in0=u[k], scalar=snw_t[:, k:k + 1], in1=rstd[:], op0=ALU.mult, op1=ALU.mult),
                             reads=[u.e[k], e_rstd, ec], writes=[ysA.e[k]])
            elif n == 1:
                for k in range(8):
                    cast(mgb[k], mgb.e[k], u[k], u.e[k])
                for m in range(8):
                    w, ew = load_wb(w_glu, m * 128)
                    ps, eps = getps("A")
                    mm_acc(ps, eps, w, ew, mgb, 8)
                    t, et = gettmp()
                    S.op("act", lambda e, t=t, ps=ps, m=m: e.activation(out=t, in_=ps[:], func=AF.Sigmoid, bias=bglu_t[:, m:m + 1]), reads=[eps, ec], writes=[et])
                    S.op("dve", lambda e, t=t, m=m: e.tensor_tensor(ysA[m], u[m], t, ALU.mult), reads=[u.e[m], et], writes=[ysA.e[m]])
            else:
                for k in range(8):
                    cast(ysA[k], ysA.e[k], u[k], u.e[k])
            ysrc = ysA
            for m in range(16):
                w, ew = load_wa(w_gate, n * D + m * 128)
                psg, epsg = getps("A")
                mm_acc(psg, epsg, w, ew, hb, 16)
                w2, ew2 = load_wb(w_branch[n], m * 128)
                psb, epsb = getps("B")
                mm_acc(psb, epsb, w2, ew2, ysrc, 8)
                t, et = gettmp()
                S.op("act", lambda e, t=t, psg=psg: e.activation(out=t, in_=psg[:], func=AF.Sigmoid), reads=[epsg], writes=[et])
                if n == 0:
                    S.op("dve", lambda e, t=t, psb=psb, m=m: e.tensor_tensor(mg[m], t, psb[:], ALU.mult), reads=[et, epsb], writes=[mg.e[m]])
                else:
                    S.op("dve", lambda e, t=t, psb=psb: e.tensor_tensor(t, t, psb[:], ALU.mult), reads=[et, epsb], writes=[et])
                    S.op("dve", lambda e, t=t, m=m: e.tensor_tensor(mg[m], mg[m], t, ALU.add), reads=[et, mg.e[m]], writes=[mg.e[m]])
        for m in range(16):
            cast(mgb[m], mgb.e[m], mg[m], mg.e[m])
        for m in range(16):
            w, ew = load_wa(w_out, m * 128)
            ps, eps = getps("A")
            mm_acc(ps, eps, w, ew, mgb, 16)
            S.op("dve", lambda e, ps=ps, m=m: e.scalar_tensor_tensor(out=u[m], in0=h[m], scalar=ALPHA, in1=ps[:], op0=ALU.mult, op1=ALU.add),
                 reads=[h.e[m], eps], writes=[u.e[m]])
        layernorm(u, h, 0, hb)
        for tt in range(T // 128):
            ts_ = slice(tt * 128, (tt + 1) * 128)
            ps, eps = getps("B")
            for k in range(16):
                S.op("pe", lambda e, k=k, ps=ps: e.matmul(ps[:, 0:NE], h.t[:, k, ts_], rw_t[:, k, :], start=(k == 0), stop=(k == 15)),
                     reads=[h.e[k], ec], writes=[eps], pe_chain=(k > 0))
            lg = rs[:, 0:NE]; m8 = rs[:, 32:40]; msk = rs[:, 40:40 + NE]; ex = rs[:, 72:72 + NE]; sm = rs[:, 104:105]; nmx = rs[:, 105:106]; gw = rs[:, 112:112 + NE]
            S.op("dve", lambda e, ps=ps: e.tensor_tensor(lg, ps[:, 0:NE], rb_t[:], ALU.add), reads=[eps, ec], writes=[e_rs])
            S.op("dve", lambda e: e.max(m8, lg), reads=[e_rs], writes=[e_rs])
            S.op("dve", lambda e: e.tensor_scalar(msk, lg, rs[:, 35:36], None, ALU.is_ge), reads=[e_rs], writes=[e_rs])
            S.op("dve", lambda e: e.tensor_scalar(nmx, rs[:, 32:33], -1.0, None, ALU.mult), reads=[e_rs], writes=[e_rs])
            S.op("act", lambda e: e.activation(out=ex, in_=lg, func=AF.Exp, bias=nmx), reads=[e_rs], writes=[e_rs])
            S.op("dve", lambda e: e.tensor_tensor(ex, ex, msk, ALU.mult), reads=[e_rs], writes=[e_rs])
            S.op("dve", lambda e: e.reduce_sum(sm, ex, AX.X), reads=[e_rs], writes=[e_rs])
            S.op("dve", lambda e: e.reciprocal(sm, sm), reads=[e_rs], writes=[e_rs])
            S.op("dve", lambda e: e.tensor_scalar(gw, ex, sm, None, ALU.mult), reads=[e_rs], writes=[e_rs])
            ps2, eps2 = getps("B")
            S.op("pe", lambda e, ps2=ps2: e.transpose(ps2[0:NE, 0:128], gw, ident[:]), reads=[e_rs, ec], writes=[eps2])
            S.op("act", lambda e, ps2=ps2: e.activation(out=gT[:, ts_], in_=ps2[0:NE, 0:128], func=AF.Copy), reads=[eps2], writes=[e_gT])
        for m in range(16):
            ps, eps = getps("A")
            S.op("pe", lambda e, ps=ps, m=m: e.matmul(ps[:], bdn_t[:, m * 128:(m + 1) * 128], gT[:], start=True, stop=True), reads=[ec, e_gT], writes=[eps])
            S.op("act", lambda e, ps=ps, m=m: e.activation(out=mg[m], in_=ps[:], func=AF.Copy), reads=[eps], writes=[mg.e[m]])
        for ex_i in range(NE):
            ps, eps = getps("B")
            S.op("pe", lambda e, ps=ps: e.matmul(ps[:], ident[0:NE, ex_i:ex_i + 1].to_broadcast([NE, 128]), gT[:], start=True, stop=True), reads=[ec, e_gT], writes=[eps])
            S.op("act", lambda e, ps=ps: e.activation(out=gbc[:], in_=ps[:], func=AF.Copy), reads=[eps], writes=[e_gbc])
            for f in range(8):
                w, ew = load_wa(w_gu[ex_i], f * 128)
                psg, epsg = getps("A")
                mm_acc(psg, epsg, w, ew, hb, 16)
                w2, ew2 = load_wa(w_gu[ex_i], 1024 + f * 128)
                psu, epsu = getps("B")
                mm_acc(psu, epsu, w2, ew2, hb, 16)
                gc, egc = gettmp(); sg, esg = gettmp(); uc, euc = gettmp()
                S.op("dve", lambda e: e.tensor_scalar(gc, psg[:], bgu_t[:, ex_i, f:f + 1], 7.0, ALU.add, ALU.min), reads=[epsg, ec], writes=[egc])
                S.op("act", lambda e: e.activation(out=sg, in_=gc, func=AF.Sigmoid, scale=1.702), reads=[egc], writes=[esg])
                S.op("dve", lambda e: e.tensor_scalar(uc, psu[:], bgu_t[:, ex_i, 8 + f:9 + f], 7.0, ALU.add, ALU.min), reads=[epsu, ec], writes=[euc])
                S.op("dve", lambda e: e.tensor_scalar(uc, uc, -7.0, 1.0, ALU.max, ALU.add), reads=[euc], writes=[euc])
                S.op("dve", lambda e: e.tensor_tensor(gc, gc, sg, ALU.mult), reads=[egc, esg], writes=[egc])
                S.op("dve", lambda e: e.tensor_tensor(uc, uc, gc, ALU.mult), reads=[egc, euc], writes=[euc])
                S.op("dve", lambda e: e.tensor_tensor(ysA[f], uc, gbc[:], ALU.mult), reads=[euc, e_gbc], writes=[ysA.e[f]])
            for m in range(16):
                w, ew = load_wb(w_dn[ex_i], m * 128)
                ps, eps = getps("A")
                mm_acc(ps, eps, w, ew, ysA, 8)
                S.op("dve", lambda e, ps=ps, m=m: e.tensor_tensor(mg[m], mg[m], ps[:], ALU.add), reads=[mg.e[m], eps], writes=[mg.e[m]])
        for m in range(16):
            S.op("dve", lambda e, m=m: e.scalar_tensor_tensor(out=u[m], in0=h[m], scalar=ALPHA, in1=mg[m], op0=ALU.mult, op1=ALU.add),
                 reads=[h.e[m], mg.e[m]], writes=[u.e[m]])
        layernorm(u, h, 1, hb)
        for m in range(16):
            w, ew = load_wa(ple_wg, m * 128)
            psg, epsg = getps("A")
            mm_acc(psg, epsg, w, ew, hb, 16)
            psb, epsb = getps("B")
            wpt, ewpt = wp_t[m % 2], ewp[m % 2]
            S.dma(nextq(), wpt[:], ple_wp[:, m * 128:(m + 1) * 128].rearrange("(k p) d -> p k d", p=128), writes=[ewpt])
            for k in range(2):
                S.op("pe", lambda e, k=k, psb=psb, wpt=wpt: e.matmul(psb[:], wpt[:, k, :], ppt[k], start=(k == 0), stop=(k == 1)),
                     reads=[ewpt, ppt.e[k]], writes=[epsb], pe_chain=(k > 0))
            t, et = gettmp()
            S.op("act", lambda e, t=t, psg=psg: e.activation(out=t, in_=psg[:], func=AF.Sigmoid), reads=[epsg], writes=[et])
            S.op("dve", lambda e, t=t, psb=psb: e.tensor_tensor(t, t, psb[:], ALU.mult), reads=[et, epsb], writes=[et])
            S.op("dve", lambda e, t=t, m=m: e.scalar_tensor_tensor(out=u[m], in0=h[m], scalar=ALPHA, in1=t, op0=ALU.mult, op1=ALU.add),
                 reads=[h.e[m], et], writes=[u.e[m]])
        layernorm(u, mg, 2)
        for k in range(16):
            S.dma(nextq(), oT[k * 128:(k + 1) * 128, tk], mg[k], reads=[mg.e[k]], writes=[eo])
    S.wait_all("sp", [eo])
    cx.st.close()
    return nc


NWT = 22
TWO_PI = 2.0 * np.pi


def build_mixer(S_len, T=512):
    cx = Ctx(); nc = cx.nc; S = cx.S
    NB = S_len // T
    hT = cx.din("hT", [D, S_len]); Wm = cx.din("Wm", [D, NWT * 128]); pos = cx.din("pos", [S_len], I32)
    cpar = cx.din("cpar", [128, 64])
    c_ident = cx.din("c_ident", [128, 128]); c_ones = cx.din("c_ones", [128, 128]); c_wide = cx.din("c_wide", [128, 255])
    c_sel = cx.din("c_sel", [128, 2, 128]); c_rot = cx.din("c_rot", [128, 128]); c_tau = cx.din("c_tau", [128, T])
    w_alpha = cx.din("w_alpha", [128, 128])
    s5B = cx.din("s5B", [2, 128, 8, 128]); s5C = cx.din("s5C", [2, 128, 8, 128])
    yT = cx.dout("yT", [4, 256, S_len]); eo = cx.ent("yT")
    ec = cx.ent("consts")
    par = cx.sb("par", [128, 64]); ident = cx.sb("ident", [128, 128]); ones = cx.sb("ones", [128, 128]); wide = cx.sb("wide", [128, 255])
    sel = cx.sb("sel", [128, 2, 128]); rot = cx.sb("rot", [128, 128]); tau = cx.sb("tau", [128, T]); wal = cx.sb("wal", [128, 128])
    Bl = cx.sb("Bl", [128, 2, 8, 128]); Cl = cx.sb("Cl", [128, 2, 8, 128])
    for dst, src in ((par, cpar), (ident, c_ident), (ones, c_ones), (wide, c_wide), (sel, c_sel), (rot, c_rot), (tau, c_tau), (wal, w_alpha)):
        S.dma("sp", dst[:], src, writes=[ec])
    for ri in range(2):
        S.dma("sp", Bl[:, ri], s5B[ri], writes=[ec]); S.dma("sp", Cl[:, ri], s5C[ri], writes=[ec])
    CW = 0
    CB = 16
    DTB, ALOG, DSK = 20, 21, 22
    S5D = 24
    LRE, LIM = 26, 27
    LDT = 28
    RNW = 36
    INVF = 38
    LGAM = 39
    BAL = 40
    GNW = 41
    def pc(c):
        return par[:, c:c + 1]

    P = Tiles(cx, "P", NWT, T)
    xe = cx.sb("xe", [128, 4, T + 3]); e_xe = [cx.ent(f"xe{i}") for i in range(4)]
    hb = cx.sb("hb", [128, 16, T]); e_hb = cx.ent("hb")
    wa = [cx.sb(f"mwa{i}", [128, 16, 128]) for i in range(2)]; ewa = [cx.ent(f"mwa{i}") for i in range(2)]
    tmp = Tiles(cx, "mt", 12, T)
    Lg = Tiles(cx, "lg", 5, T)
    Vb = Tiles(cx, "vb", 2, T, BF16); prb = Tiles(cx, "prb", 4, T, BF16); cpr = [0]
    ident_b = cx.sb("ident_b", [128, 128], BF16); wide_b = cx.sb("wide_b", [128, 255], BF16)
    cnt = {"t": 0, "q": 0, "pa": 0, "pb": 0}
    psA = [cx.ps(f"mpA{i}") for i in range(2)]; epsA = [cx.ent(f"mpA{i}") for i in range(2)]
    psB = [cx.ps(f"mpB{i}") for i in range(2)]; epsB = [cx.ent(f"mpB{i}") for i in range(2)]
    psY = [cx.ps(f"mpY{i}") for i in range(2)]; epsY = [cx.ent(f"mpY{i}") for i in range(2)]
    psZ = [cx.ps(f"mpZ{i}") for i in range(2)]; epsZ = [cx.ent(f"mpZ{i}") for i in range(2)]
    carry = cx.sb("carry", [128, 3, 256]); e_carry = [[cx.ent(f"carry{i}_{c}") for c in range(256)] for i in range(3)]
    s5c = cx.sb("s5c", [128, 2, 8]); e_s5c = cx.ent("s5c")
    posf = cx.sb("posf", [128, T]); posi = cx.sb("posi", [128, T], I32); e_pos = cx.ent("pos")
    kint = cx.sb("kint", [128, T], I32); e_kint = cx.ent("kint")
    TS = T // 2
    Tp = cx.sb("Tp", [128, 2, 8, TS]); Tn = cx.sb("Tn", [128, 2, 8, TS]); e_tab = cx.ent("tab")
    sm = cx.sb("sm", [128, 64]); e_sm = cx.ent("sm")

    def gt():
        i = cnt["t"] % 12; cnt["t"] += 1
        return tmp[i], tmp.e[i]

    def gp(kind):
        lst, el = (psA, epsA) if kind == "A" else (psB, epsB)
        key = "pa" if kind == "A" else "pb"
        i = cnt[key] % 2; cnt[key] += 1
        return lst[i], el[i]

    def nextq():
        cnt["q"] += 1
        return "sp" if cnt["q"] % 2 else "pool"

    def dve(fn, r, w): S.op("dve", fn, reads=r, writes=w)
    def act(fn, r, w): S.op("act", fn, reads=r, writes=w)
    def pool(fn, r, w): S.op("pool", fn, reads=r, writes=w)
    def pe(fn, r, w, chain=False): S.op("pe", fn, reads=r, writes=w, pe_chain=chain)

    def sincos(ang, eang, out_sin, eout, shift):
        t, et = gt()
        dve(lambda e: e.tensor_scalar(t, ang, shift, 1.0 / TWO_PI, ALU.add, ALU.mult), [eang], [et])
        dve(lambda e: e.tensor_copy(kint[:], t), [et], [e_kint])
        dve(lambda e: e.tensor_copy(t, kint[:]), [e_kint], [et])
        t2, et2 = gt()
        dve(lambda e: e.tensor_scalar(t2, ang, shift, None, ALU.add), [eang], [et2])
        dve(lambda e: e.scalar_tensor_tensor(out=t2, in0=t, scalar=-6.28125, in1=t2, op0=ALU.mult, op1=ALU.add), [et2, et], [et2])
        dve(lambda e: e.scalar_tensor_tensor(out=t2, in0=t, scalar=-1.9353071795864769e-3, in1=t2, op0=ALU.mult, op1=ALU.add), [et2, et], [et2])
        dve(lambda e: e.tensor_scalar(t, t2, float(np.pi), -TWO_PI, ALU.is_gt, ALU.mult), [et2], [et])
        t3, et3 = gt()
        dve(lambda e: e.tensor_scalar(t3, t2, -float(np.pi), TWO_PI, ALU.is_lt, ALU.mult), [et2], [et3])
        dve(lambda e: e.tensor_tensor(t2, t2, t, ALU.add), [et2, et], [et2])
        dve(lambda e: e.tensor_tensor(t2, t2, t3, ALU.add), [et2, et3], [et2])
        dve(lambda e: e.tensor_scalar(t2, t2, float(np.pi), -float(np.pi), ALU.min, ALU.max), [et2], [et2])
        act(lambda e: e.activation(out=out_sin, in_=t2, func=AF.Sin), [et2], [eout])

    S.wait_all("dve", [ec]); S.wait_all("act", [ec])
    S.op("dve", lambda e: e.tensor_copy(ident_b[:], ident[:]), reads=[ec], writes=[ec])
    S.op("dve", lambda e: e.tensor_copy(wide_b[:], wide[:]), reads=[ec], writes=[ec])
    lr = sm[:, 0:1]; dtc = sm[:, 8:16]; acol = sm[:, 16:24]; thcol = sm[:, 24:32]
    dve(lambda e: e.tensor_scalar(lr, pc(LRE), -1e-4, None, ALU.min), [ec], [e_sm])
    act(lambda e: e.activation(out=dtc, in_=par[:, LDT:LDT + 8], func=AF.Exp), [ec], [e_sm])
    dve(lambda e: e.tensor_scalar(acol, dtc, lr, None, ALU.mult), [e_sm], [e_sm])
    dve(lambda e: e.tensor_scalar(thcol, dtc, pc(LIM), None, ALU.mult), [e_sm, ec], [e_sm])
    nacol = sm[:, 32:40]
    dve(lambda e: e.tensor_scalar(nacol, acol, -1.0, None, ALU.mult), [e_sm], [e_sm])
    for j in range(8):
        ang, eang = gt()
        dve(lambda e: e.tensor_scalar(ang, tau[:], thcol[:, j:j + 1], None, ALU.mult), [ec, e_sm], [eang])
        sn, esn = gt(); cs, ecs = gt(); mg_, emg = gt(); mn, emn = gt()
        sincos(ang, eang, sn, esn, 0.0)
        sincos(ang, eang, cs, ecs, float(np.pi / 2))
        act(lambda e: e.activation(out=mg_, in_=tau[:], func=AF.Exp, scale=acol[:, j:j + 1]), [ec, e_sm], [emg])
        act(lambda e: e.activation(out=mn, in_=tau[:], func=AF.Exp, scale=nacol[:, j:j + 1]), [ec, e_sm], [emn])
        dve(lambda e: e.tensor_tensor(Tp[:, 0, j, :], mg_[:, 0:TS], cs[:, 0:TS], ALU.mult), [emg, ecs], [e_tab])
        dve(lambda e: e.tensor_tensor(Tp[:, 1, j, :], mg_[:, 0:TS], sn[:, 0:TS], ALU.mult), [emg, esn], [e_tab])
        dve(lambda e: e.tensor_tensor(Tn[:, 0, j, :], mn[:, 0:TS], cs[:, 0:TS], ALU.mult), [emn, ecs], [e_tab])
        dve(lambda e: e.scalar_tensor_tensor(out=Tn[:, 1, j, :], in0=mn[:, 0:TS], scalar=-1.0, in1=sn[:, 0:TS], op0=ALU.mult, op1=ALU.mult), [emn, esn], [e_tab])
    abr = sm[:, 40:48]; abi = sm[:, 48:56]; fre = sm[:, 56:64]
    sm2 = cx.sb("sm2", [128, 64]); e_sm2 = cx.ent("sm2")
    fim = sm2[:, 0:8]; den = sm2[:, 8:9]; t8 = sm2[:, 16:24]; lbr = sm2[:, 24:32]; lbi = sm2[:, 32:40]
    dve(lambda e: e.tensor_copy(lbr, Tp[:, 0, :, 1]), [e_tab], [e_sm2])
    dve(lambda e: e.tensor_copy(lbi, Tp[:, 1, :, 1]), [e_tab], [e_sm2])
    dve(lambda e: e.tensor_scalar(abr, lbr, -1.0, None, ALU.add), [e_sm2], [e_sm])
    dve(lambda e: e.tensor_tensor(den, lr, lr, ALU.mult), [e_sm], [e_sm2])
    dve(lambda e: e.scalar_tensor_tensor(out=den, in0=pc(LIM), scalar=pc(LIM), in1=den, op0=ALU.mult, op1=ALU.add), [ec, e_sm2], [e_sm2])
    dve(lambda e: e.reciprocal(den, den), [e_sm2], [e_sm2])
    dve(lambda e: e.tensor_scalar(t8, lbi, pc(LIM), None, ALU.mult), [e_sm2, ec], [e_sm2])
    dve(lambda e: e.scalar_tensor_tensor(out=fre, in0=abr, scalar=lr, in1=t8, op0=ALU.mult, op1=ALU.add), [e_sm, e_sm2], [e_sm])
    dve(lambda e: e.tensor_scalar(fre, fre, den, None, ALU.mult), [e_sm, e_sm2], [e_sm])
    dve(lambda e: e.tensor_scalar(t8, abr, pc(LIM), None, ALU.mult), [e_sm, ec], [e_sm2])
    dve(lambda e: e.scalar_tensor_tensor(out=fim, in0=lbi, scalar=lr, in1=t8, op0=ALU.mult, op1=ALU.subtract), [e_sm, e_sm2], [e_sm2])
    dve(lambda e: e.tensor_scalar(fim, fim, den, None, ALU.mult), [e_sm2], [e_sm2])
    for j in range(8):
        a_, ea = gt(); b_, eb = gt(); a_ = a_[:, 0:TS]; b_ = b_[:, 0:TS]
        dve(lambda e: e.tensor_scalar(a_, Tn[:, 0, j, :], fre[:, j:j + 1], None, ALU.mult), [e_tab, e_sm], [ea])
        dve(lambda e: e.tensor_scalar(b_, Tn[:, 1, j, :], fre[:, j:j + 1], None, ALU.mult), [e_tab, e_sm], [eb])
        dve(lambda e: e.scalar_tensor_tensor(out=b_, in0=Tn[:, 0, j, :], scalar=fim[:, j:j + 1], in1=b_, op0=ALU.mult, op1=ALU.add), [e_tab, e_sm2, eb], [eb])
        dve(lambda e: e.scalar_tensor_tensor(out=t8[:, 0:1].to_broadcast([128, 1]) if False else a_, in0=Tn[:, 1, j, :], scalar=fim[:, j:j + 1], in1=a_, op0=ALU.mult, op1=ALU.subtract), [e_tab, e_sm2, ea], [ea])
        dve(lambda e: e.tensor_scalar(Tn[:, 0, j, :], a_, -1.0, None, ALU.mult), [ea], [e_tab])
        dve(lambda e: e.tensor_copy(Tn[:, 1, j, :], b_), [eb], [e_tab])
    for i in range(3):
        dve(lambda e, i=i: e.memset(carry[:, i, :], 0.0), [], e_carry[i])
    dve(lambda e: e.memset(s5c[:], 0.0), [], [e_s5c])
    for i in range(4):
        dve(lambda e, i=i: e.memset(xe[:, i, 0:3], 0.0), [], [e_xe[i]])
    nbal = sm2[:, 40:41]; nalog = sm2[:, 41:42]
    dve(lambda e: e.tensor_scalar(nbal, pc(BAL), -1.0, None, ALU.mult), [ec], [e_sm2])
    act(lambda e: e.activation(out=nalog, in_=pc(ALOG), func=AF.Exp), [ec], [e_sm2])
    dve(lambda e: e.tensor_scalar(nalog, nalog, -1.0, None, ALU.mult), [e_sm2], [e_sm2])
    gam = sm2[:, 42:43]
    act(lambda e: e.activation(out=gam, in_=pc(LGAM), func=AF.Exp), [ec], [e_sm2])

    ps4 = [(psA[0], epsA[0]), (psA[1], epsA[1]), (psB[0], epsB[0]), (psB[1], epsB[1])]
    c4 = [0]

    def chan_scan(mix, nch, k_ap, ek, q_ap, eq, a_of, v_of, out_of):
        LAG = 3
        state = {}

        def s1(e_):
            vt, evt, row, halves = v_of(e_)
            pb, epb = ps4[c4[0] % 4]; c4[0] += 1
            if halves is None:
                pe(lambda e: e.matmul(pb[:], ident_b[:, row:row + 1].to_broadcast([128, 128]), vt, start=True, stop=True), [ec, evt], [epb])
            else:
                (v0, ev0), (v1, ev1) = halves
                pe(lambda e: e.matmul(pb[0:64, :], ident_b[:, row:row + 1].to_broadcast([128, 64]), v0, start=True, stop=True), [ec, ev0], [epb])
                pe(lambda e: e.matmul(pb[64:128, :], ident_b[:, row:row + 1].to_broadcast([128, 64]), v1, start=True, stop=True), [ec, ev1], [epb])
            state[e_] = (pb, epb)

        def s2a(e_):
            pb, epb = state[e_]
            d1, ed1 = gt()
            dve(lambda e: e.tensor_tensor(d1, k_ap, pb[:], ALU.mult), [ek, epb], [ed1])
            state[e_] = (d1, ed1)

        def s2b(e_):
            d1, ed1 = state[e_]
            a_ap, ea = a_of(e_)
            st, est = gt()
            dve(lambda e: e.tensor_tensor_scan(st, a_ap, d1, carry[:, mix, e_:e_ + 1], ALU.mult, ALU.add), [ea, ed1, e_carry[mix][e_]], [est])
            act(lambda e: e.activation(out=carry[:, mix, e_:e_ + 1], in_=st[:, T - 1:T], func=AF.Copy), [est], [e_carry[mix][e_]])
            ip = cpr[0] % 4; cpr[0] += 1
            pr_, epr = prb[ip], prb.e[ip]
            pool(lambda e: e.tensor_tensor(pr_, st, q_ap, ALU.mult), [est, eq], [epr])
            state[e_] = (pr_, epr)

        for step in range(nch + LAG):
            if step < nch:
                s1(step)
            if 0 <= step - 2 < nch:
                s2a(step - 2)
            if step >= LAG:
                e_ = step - LAG
                s2b(e_)
                st, est = state.pop(e_)
                out_of(e_, st, est)

    for blk in range(NB):
        tk = slice(blk * T, (blk + 1) * T)
        S.dma("sp", hb[:], hT.rearrange("(k p) t -> p k t", p=128)[:, :, tk], writes=[e_hb])
        S.dma("pool", posi[:], pos[tk].partition_broadcast(128), writes=[e_pos])
        for wt in range(NWT):
            i = wt % 2
            S.dma(nextq(), wa[i][:], Wm[:, wt * 128:(wt + 1) * 128].rearrange("(k p) c -> p k c", p=128), writes=[ewa[i]])
            ps, eps = gp("A")
            for k in range(16):
                pe(lambda e, k=k: e.matmul(ps[:], wa[i][:, k, :], hb[:, k, :], start=(k == 0), stop=(k == 15)), [ewa[i], e_hb], [eps], chain=(k > 0))
            if 2 <= wt <= 5:
                ci = wt - 2
                act(lambda e: e.activation(out=xe[:, ci, 3:T + 3], in_=ps[:], func=AF.Copy), [eps], [e_xe[ci]])
            else:
                act(lambda e: e.activation(out=P[wt], in_=ps[:], func=AF.Copy), [eps], [P.e[wt]])
        for ci in range(4):
            wt = ci + 2
            dve(lambda e: e.tensor_scalar(P[wt], xe[:, ci, 3:T + 3], pc(CW + 4 * ci + 3), None, ALU.mult), [e_xe[ci], ec], [P.e[wt]])
            for k in range(3):
                dve(lambda e, k=k: e.scalar_tensor_tensor(out=P[wt], in0=xe[:, ci, k:T + k], scalar=pc(CW + 4 * ci + k), in1=P[wt], op0=ALU.mult, op1=ALU.add),
                    [e_xe[ci], ec, P.e[wt]], [P.e[wt]])
            dve(lambda e: e.tensor_copy(xe[:, ci, 0:3], xe[:, ci, T:T + 3]), [e_xe[ci]], [e_xe[ci]])
            act(lambda e: e.activation(out=P[wt], in_=P[wt], func=AF.Silu, bias=pc(CB + ci)), [P.e[wt], ec], [P.e[wt]])
        act(lambda e: e.activation(out=P[6], in_=P[6], func=AF.Exp, bias=pc(DTB)), [P.e[6], ec], [P.e[6]])
        act(lambda e: e.activation(out=P[6], in_=P[6], func=AF.Ln, bias=1.0), [P.e[6]], [P.e[6]])
        arow, earow = Lg[4], Lg.e[4]
        act(lambda e: e.activation(out=arow, in_=P[6], func=AF.Exp, scale=nalog), [P.e[6], e_sm2], [earow])
        abc = []
        for r in range(4):
            pb, epb = gp("B")
            pe(lambda e: e.matmul(pb[:], ident[:, r:r + 1].to_broadcast([128, 128]), arow, start=True, stop=True), [ec, earow], [epb])
            t, et = Lg[r], Lg.e[r]
            act(lambda e: e.activation(out=t, in_=pb[:], func=AF.Copy), [epb], [et])
            abc.append((t, et))
        xdt = []
        for i in range(2):
            pb, epb = gp("B")
            pe(lambda e: e.matmul(pb[:], sel[:, i, :], P[6], start=True, stop=True), [ec, P.e[6]], [epb])
            t, et = Vb[i], Vb.e[i]
            dve(lambda e: e.tensor_tensor(t, P[2 + i], pb[:], ALU.mult), [P.e[2 + i], epb], [et])
            xdt.append((t, et))
        def ssd_out(e_, st, est):
            i, el = divmod(e_, 128)
            pe(lambda e: e.matmul(psY[i][:], wide_b[:, 127 - el:255 - el], st, start=(el == 0), stop=(el == 127)), [ec, est], [epsY[i]], chain=(el > 0))
        chan_scan(0, 256, P[4], P.e[4], P[5], P.e[5], lambda e_: abc[e_ // 64], lambda e_: (xdt[e_ // 128][0], xdt[e_ // 128][1], e_ % 128, None), ssd_out)
        for i in range(2):
            t, et = gt()
            dve(lambda e: e.scalar_tensor_tensor(out=t, in0=P[2 + i], scalar=pc(DSK + i), in1=psY[i][:], op0=ALU.mult, op1=ALU.add), [P.e[2 + i], ec, epsY[i]], [et])
            act(lambda e: e.activation(out=P[i], in_=P[i], func=AF.Silu), [P.e[i]], [P.e[i]])
            dve(lambda e: e.tensor_tensor(t, t, P[i], ALU.mult), [et, P.e[i]], [et])
            S.dma(nextq(), yT[0, i * 128:(i + 1) * 128, tk], t, reads=[et], writes=[eo])
        dve(lambda e: e.tensor_copy(posf[:], posi[:]), [e_pos], [e_pos])
        ang, eang = gt()
        dve(lambda e: e.tensor_scalar(ang, posf[:], pc(INVF), None, ALU.mult), [e_pos, ec], [eang])
        sn, esn = gt(); cs, ecs = gt()
        sincos(ang, eang, sn, esn, 0.0); sincos(ang, eang, cs, ecs, float(np.pi / 2))
        for wt, scale in ((9, 1.0), (10, 0.125)):
            pb, epb = gp("B")
            pe(lambda e: e.matmul(pb[:], rot[:], P[wt], start=True, stop=True), [ec, P.e[wt]], [epb])
            t, et = gt()
            dve(lambda e: e.tensor_tensor(t, pb[:], sn, ALU.mult), [epb, esn], [et])
            dve(lambda e: e.tensor_tensor(P[wt], P[wt], cs, ALU.mult), [P.e[wt], ecs], [P.e[wt]])
            dve(lambda e: e.tensor_tensor(P[wt], P[wt], t, ALU.add), [P.e[wt], et], [P.e[wt]])
            if scale != 1.0:
                dve(lambda e: e.tensor_scalar(P[wt], P[wt], scale, None, ALU.mult), [P.e[wt]], [P.e[wt]])
        gam_t, egam = Lg[0], Lg.e[0]
        dve(lambda e: e.tensor_scalar(gam_t, tau[:], 0.0, gam, ALU.mult, ALU.add), [ec, e_sm2], [egam])
        def ret_out(e_, st, est):
            for hh in range(2):
                pe(lambda e, hh=hh: e.matmul(psY[hh][:], wide_b[hh * 64:(hh + 1) * 64, 127 - e_:255 - e_], st[hh * 64:(hh + 1) * 64, :], start=(e_ == 0), stop=(e_ == 127)),
                   [ec, est], [epsY[hh]], chain=(e_ > 0))
        for i in range(2):
            act(lambda e, i=i: e.activation(out=Vb[i], in_=P[11 + i], func=AF.Copy), [P.e[11 + i]], [Vb.e[i]])
        chan_scan(1, 128, P[10], P.e[10], P[9], P.e[9], lambda e_: (gam_t, egam),
                  lambda e_: (None, None, e_, ((Vb[0], Vb.e[0]), (Vb[1], Vb.e[1]))), ret_out)
        for hh in range(2):
            y, ey = gt()
            act(lambda e: e.activation(out=y, in_=psY[hh][:], func=AF.Copy), [epsY[hh]], [ey])
            sq, esq = gt()
            act(lambda e: e.activation(out=sq, in_=y, func=AF.Square), [ey], [esq])
            pa, epa = gp("A"); pb, epb = gp("B")
            pe(lambda e: e.matmul(pa[:], ones[:], y, start=True, stop=True), [ec, ey], [epa])
            pe(lambda e: e.matmul(pb[:], ones[:], sq, start=True, stop=True), [ec, esq], [epb])
            mu, emu = gt(); rs_, ers = gt()
            act(lambda e: e.activation(out=mu, in_=pa[:], func=AF.Copy, scale=1.0 / 128), [epa], [emu])
            dve(lambda e: e.tensor_tensor(rs_, mu, mu, ALU.mult), [emu], [ers])
            dve(lambda e: e.scalar_tensor_tensor(out=rs_, in0=pb[:], scalar=1.0 / 128, in1=rs_, op0=ALU.mult, op1=ALU.subtract), [epb, ers], [ers])
            dve(lambda e: e.tensor_scalar(rs_, rs_, LN_EPS, None, ALU.add), [ers], [ers])
            act(lambda e: e.activation(out=rs_, in_=rs_, func=AF.Sqrt), [ers], [ers])
            dve(lambda e: e.reciprocal(rs_, rs_), [ers], [ers])
            dve(lambda e: e.tensor_tensor(y, y, mu, ALU.subtract), [ey, emu], [ey])
            dve(lambda e: e.scalar_tensor_tensor(out=y, in0=y, scalar=pc(RNW + hh), in1=rs_, op0=ALU.mult, op1=ALU.mult), [ey, ers, ec], [ey])
            act(lambda e: e.activation(out=P[13 + hh], in_=P[13 + hh], func=AF.Silu), [P.e[13 + hh]], [P.e[13 + hh]])
            dve(lambda e: e.tensor_tensor(y, y, P[13 + hh], ALU.mult), [ey, P.e[13 + hh]], [ey])
            S.dma(nextq(), yT[2, hh * 128:(hh + 1) * 128, tk], y, reads=[ey], writes=[eo])
        pb, epb = gp("B")
        pe(lambda e: e.matmul(pb[:], wal[:], P[21], start=True, stop=True), [ec, P.e[21]], [epb])
        al, eal = Lg[1], Lg.e[1]
        act(lambda e: e.activation(out=al, in_=pb[:], func=AF.Exp, scale=-1.0, bias=nbal), [epb, e_sm2], [eal])
        act(lambda e: e.activation(out=al, in_=al, func=AF.Ln, bias=1.0), [eal], [eal])
        act(lambda e: e.activation(out=al, in_=al, func=AF.Exp, scale=-1.0 / 16.0), [eal], [eal])
        dve(lambda e: e.tensor_scalar(P[15], P[15], float(128 ** -0.5), None, ALU.mult), [P.e[15]], [P.e[15]])
        def gla_out(e_, st, est):
            i, el = divmod(e_, 128)
            pe(lambda e: e.matmul(psY[i][:], wide_b[:, 127 - el:255 - el], st, start=(el == 0), stop=(el == 127)), [ec, est], [epsY[i]], chain=(el > 0))
        for i in range(2):
            act(lambda e, i=i: e.activation(out=Vb[i], in_=P[17 + i], func=AF.Copy), [P.e[17 + i]], [Vb.e[i]])
        chan_scan(2, 256, P[16], P.e[16], P[15], P.e[15], lambda e_: (al, eal), lambda e_: (Vb[e_ // 128], Vb.e[e_ // 128], e_ % 128, None), gla_out)
        ys_ = []
        pa, epa = gp("A")
        for i in range(2):
            y, ey = gt(); sq, esq = gt()
            act(lambda e: e.activation(out=y, in_=psY[i][:], func=AF.Copy), [epsY[i]], [ey])
            act(lambda e: e.activation(out=sq, in_=y, func=AF.Square), [ey], [esq])
            pe(lambda e, i=i: e.matmul(pa[:], ones[:], sq, start=(i == 0), stop=(i == 1)), [ec, esq], [epa], chain=(i > 0))
            ys_.append((y, ey))
        rs_, ers = gt()
        dve(lambda e: e.tensor_scalar(rs_, pa[:], 1.0 / 256, RMS_EPS, ALU.mult, ALU.add), [epa], [ers])
        act(lambda e: e.activation(out=rs_, in_=rs_, func=AF.Sqrt), [ers], [ers])
        dve(lambda e: e.reciprocal(rs_, rs_), [ers], [ers])
        for i in range(2):
            y, ey = ys_[i]
            dve(lambda e: e.scalar_tensor_tensor(out=y, in0=y, scalar=pc(GNW + i), in1=rs_, op0=ALU.mult, op1=ALU.mult), [ey, ers, ec], [ey])
            act(lambda e: e.activation(out=P[19 + i], in_=P[19 + i], func=AF.Silu), [P.e[19 + i]], [P.e[19 + i]])
            dve(lambda e: e.tensor_tensor(y, y, P[19 + i], ALU.mult), [ey, P.e[19 + i]], [ey])
            S.dma(nextq(), yT[3, i * 128:(i + 1) * 128, tk], y, reads=[ey], writes=[eo])
        for hf in range(2):
         hs = slice(hf * TS, (hf + 1) * TS)
         for i in range(2):
             for jj in range(4):
                 j = 4 * i + jj
                 xr, exr = gt(); xi, exi = gt(); xr = xr[:, 0:TS]; xi = xi[:, 0:TS]
                 for ri, (xx, exx) in enumerate(((xr, exr), (xi, exi))):
                     pb, epb = gp("B")
                     pe(lambda e: e.matmul(pb[:, 0:TS], Bl[:, ri, j, :], P.t[:, 7 + i, hs], start=True, stop=True), [ec, P.e[7 + i]], [epb])
                     act(lambda e: e.activation(out=xx, in_=pb[:, 0:TS], func=AF.Copy), [epb], [exx])
                 wr, ewr = gt(); wi, ewi = gt(); t1, et1 = gt(); wr = wr[:, 0:TS]; wi = wi[:, 0:TS]; t1 = t1[:, 0:TS]
                 dve(lambda e: e.tensor_tensor(wr, xr, Tn[:, 0, j, :], ALU.mult), [exr, e_tab], [ewr])
                 pool(lambda e: e.tensor_tensor(t1, xi, Tn[:, 1, j, :], ALU.mult), [exi, e_tab], [et1])
                 dve(lambda e: e.tensor_tensor(wr, wr, t1, ALU.subtract), [ewr, et1], [ewr])
                 pool(lambda e: e.tensor_tensor(wi, xr, Tn[:, 1, j, :], ALU.mult), [exr, e_tab], [ewi])
                 dve(lambda e: e.tensor_tensor(t1, xi, Tn[:, 0, j, :], ALU.mult), [exi, e_tab, ewr], [et1])
                 dve(lambda e: e.tensor_tensor(wi, wi, t1, ALU.add), [ewi, et1], [ewi])
                 dve(lambda e: e.tensor_tensor_scan(wr, ones[:, 0:1].to_broadcast([128, TS]), wr, s5c[:, 0, j:j + 1], ALU.mult, ALU.add), [ec, ewr, e_s5c], [ewr])
                 dve(lambda e: e.tensor_tensor_scan(wi, ones[:, 0:1].to_broadcast([128, TS]), wi, s5c[:, 1, j:j + 1], ALU.mult, ALU.add), [ec, ewi, e_s5c], [ewi])
                 hr, ehr = xr, exr; hi, ehi = xi, exi
                 dve(lambda e: e.tensor_tensor(hr, wr, Tp[:, 0, j, :], ALU.mult), [ewr, e_tab], [ehr])
                 pool(lambda e: e.tensor_tensor(t1, wi, Tp[:, 1, j, :], ALU.mult), [ewi, e_tab], [et1])
                 dve(lambda e: e.tensor_tensor(hr, hr, t1, ALU.subtract), [ehr, et1], [ehr])
                 pool(lambda e: e.tensor_tensor(hi, wr, Tp[:, 1, j, :], ALU.mult), [ewr, e_tab], [ehi])
                 dve(lambda e: e.tensor_tensor(t1, wi, Tp[:, 0, j, :], ALU.mult), [ewi, e_tab, ehr], [et1])
                 dve(lambda e: e.tensor_tensor(hi, hi, t1, ALU.add), [ehi, et1], [ehi])
                 c1 = sm2[:, 48:49]
                 dve(lambda e: e.tensor_scalar(c1, hi[:, TS - 1:TS], lbi[:, j:j + 1], None, ALU.mult), [ehi, e_sm2], [e_sm2])
                 dve(lambda e: e.scalar_tensor_tensor(out=s5c[:, 0, j:j + 1], in0=hr[:, TS - 1:TS], scalar=lbr[:, j:j + 1], in1=c1, op0=ALU.mult, op1=ALU.subtract), [ehr, e_sm2], [e_s5c])
                 dve(lambda e: e.tensor_scalar(c1, hi[:, TS - 1:TS], lbr[:, j:j + 1], None, ALU.mult), [ehi, e_sm2], [e_sm2])
                 dve(lambda e: e.scalar_tensor_tensor(out=s5c[:, 1, j:j + 1], in0=hr[:, TS - 1:TS], scalar=lbi[:, j:j + 1], in1=c1, op0=ALU.mult, op1=ALU.add), [ehr, e_sm2], [e_s5c])
                 pe(lambda e: e.matmul(psY[0][:, 0:TS], Cl[:, 0, j, :], hr, start=(jj == 0), stop=(jj == 3)), [ec, ehr], [epsY[0]], chain=(jj > 0))
                 pe(lambda e: e.matmul(psZ[0][:, 0:TS], Cl[:, 1, j, :], hi, start=(jj == 0), stop=(jj == 3)), [ec, ehi], [epsZ[0]], chain=(jj > 0))
             y, ey = gt(); y = y[:, 0:TS]
             act(lambda e: e.activation(out=y, in_=psZ[0][:, 0:TS], func=AF.Copy), [epsZ[0]], [ey])
             dve(lambda e: e.tensor_tensor(y, psY[0][:, 0:TS], y, ALU.subtract), [epsY[0], ey], [ey])
             dve(lambda e: e.scalar_tensor_tensor(out=y, in0=P.t[:, 7 + i, hs], scalar=pc(S5D + i), in1=y, op0=ALU.mult, op1=ALU.add), [P.e[7 + i], ec, ey], [ey])
             act(lambda e: e.activation(out=y, in_=y, func=AF.Gelu), [ey], [ey])
             S.dma(nextq(), yT[1, i * 128:(i + 1) * 128, blk * T + hf * TS:blk * T + (hf + 1) * TS], y, reads=[ey], writes=[eo])

    S.wait_all("sp", [eo]); S.wait_all("pool", [eo])
    cx.st.close()
    return nc


OFF = dict(z=0, xs=1024, B=2048, C=2304, dt=2560, u=2576, rq=3600, rk=4112, rv=4624, rg=5648, gq=6672, gk=7184, gv=7696, gr=8720, gc=9744, gate=9760)


def _mixer_consts(T=512):
    c = {}
    c["c_ident"] = np.eye(128, dtype=np.float32); c["c_ones"] = np.ones((128, 128), np.float32)
    w = np.zeros((128, 255), np.float32); w[:, 127] = 1.0; c["c_wide"] = w
    sel = np.zeros((128, 2, 128), np.float32)
    for i in range(2):
        for m in range(128):
            sel[2 * i + m // 64, i, m] = 1.0
    c["c_sel"] = sel
    rot = np.zeros((128, 128), np.float32)
    for m in range(128):
        if m % 64 < 32:
            rot[m + 32, m] = -1.0
        else:
            rot[m - 32, m] = 1.0
    c["c_rot"] = rot
    c["c_tau"] = np.tile(np.arange(T, dtype=np.float32)[None, :], (128, 1))
    return c


def _mixer_inputs(inp, l, j, T=512):
    g = j // 2
    w_in = inp["w_in"][l]
    Wm = np.zeros((D, NWT * 128), np.float32)
    def put(t, c0, n):
        Wm[:, t * 128:t * 128 + n] = w_in[:, c0:c0 + n]
    put(0, OFF["z"] + 256 * j, 256); put(2, OFF["xs"] + 256 * j, 256); put(4, OFF["B"] + 128 * g, 128); put(5, OFF["C"] + 128 * g, 128)
    put(6, OFF["dt"] + 4 * j, 4); put(7, OFF["u"] + 256 * j, 256); put(9, OFF["rq"] + 128 * j, 128); put(10, OFF["rk"] + 128 * j, 128)
    put(11, OFF["rv"] + 256 * j, 256); put(13, OFF["rg"] + 256 * j, 256); put(15, OFF["gq"] + 128 * j, 128); put(16, OFF["gk"] + 128 * j, 128)
    put(17, OFF["gv"] + 256 * j, 256); put(19, OFF["gr"] + 256 * j, 256); put(21, OFF["gc"], 16)
    par = np.zeros((128, 64), np.float32)
    p = np.arange(128)
    cw, cb = inp["ssd_conv_w"][l], inp["ssd_conv_b"][l]
    chans = [256 * j + p, 256 * j + 128 + p, 1024 + 128 * g + p, 1280 + 128 * g + p]
    for ci, ch in enumerate(chans):
        for k in range(4):
            par[:, 4 * ci + k] = cw[k, ch]
        par[:, 16 + ci] = cb[ch]
    par[0:4, 20] = inp["ssd_dt_bias"][l][4 * j:4 * j + 4]; par[0:4, 21] = inp["ssd_a_log"][l][4 * j:4 * j + 4]
    for i in range(2):
        par[:, 22 + i] = inp["ssd_d"][l][4 * j + 2 * i + p // 64]
        par[:, 24 + i] = inp["s5_d"][l][256 * j + 128 * i + p]
        par[:, 36 + i] = inp["ret_norm_w"][l][(2 * j + i) * 128 + p]
        par[:, 41 + i] = inp["gla_norm_w"][l][256 * j + 128 * i + p]
    par[:, 26] = inp["s5_lambda_re"][l][p % 64]; par[:, 27] = inp["s5_lambda_im"][l][p % 64]
    for jt in range(8):
        par[:, 28 + jt] = inp["s5_log_dt"][l][16 * j + 2 * jt + p // 64]
    inv_freq = (1.0 / (np.float32(10000.0) ** (np.arange(32, dtype=np.float32) / np.float32(32)))).astype(np.float32)
    par[:, 38] = inv_freq[(p % 64) % 32]
    lg = np.log1p(-np.exp2(-5.0 - np.arange(8, dtype=np.float32))).astype(np.float32)
    par[:, 39] = lg[2 * j + p // 64]
    par[:, 40] = inp["gla_b_alpha"][l][128 * j + p]
    wal = np.zeros((128, 128), np.float32); wal[0:16] = inp["gla_w_alpha"][l][:, 128 * j:128 * j + 128]
    s5B = np.zeros((2, 128, 8, 128), np.float32); s5C = np.zeros((2, 128, 8, 128), np.float32)
    for ri, (bn, cn) in enumerate((("s5_b_re", "s5_c_re"), ("s5_b_im", "s5_c_im"))):
        bb, cc = inp[bn][l], inp[cn][l]
        for jt in range(8):
            for g2 in range(2):
                gl = 2 * jt + g2
                G = 16 * j + gl
                k0 = (gl % 8) * 16
                s5B[ri, k0:k0 + 16, jt, g2 * 64:(g2 + 1) * 64] = bb[G].T
                s5C[ri, g2 * 64:(g2 + 1) * 64, jt, k0:k0 + 16] = cc[G].T
    d = dict(Wm=Wm, cpar=par, w_alpha=wal, s5B=s5B, s5C=s5C)
    d.update(_mixer_consts(T))
    return d


_PROGS = {}


def _prog(kind, n):
    key = (kind, n)
    if key not in _PROGS:
        _PROGS[key] = build_mixer(n) if kind == "m" else build_rest(n)
    return _PROGS[key]


def kernel(**inp):
    inp = {k: np.asarray(v) for k, v in inp.items()}
    x = inp["x"]; Bn, Sn, _ = x.shape
    NQ = 4; NT = Sn // NQ
    hT = [np.ascontiguousarray(x[b].T) for b in range(Bn)]
    rconst = dict(c_ones=np.ones((128, 128), np.float32), c_ident=np.eye(128, dtype=np.float32), c_sel=np.zeros((32, 32 * 128), np.float32))
    for l in range(2):
        mi = [_mixer_inputs(inp, l, j) for j in range(4)]
        ins = []
        for c in range(8):
            b, j = divmod(c, 4)
            d = dict(mi[j]); d["hT"] = hT[b]; d["pos"] = np.ascontiguousarray(inp["positions"][b].astype(np.int32))
            ins.append(d)
        res = run_bass_kernel_spmd(_prog("m", Sn), ins, core_ids=list(range(8)))
        ym = [r["yT"] for r in res.results]
        W = dict(w_gate=np.ascontiguousarray(inp["w_in"][l][:, OFF["gate"]:]), w_branch=inp["w_branch"][l], w_out=inp["w_out"][l],
                 ssd_norm_w=inp["ssd_norm_w"][l], s5_w_glu=inp["s5_w_glu"][l], s5_b_glu=inp["s5_b_glu"][l],
                 ln1_w=inp["ln1_w"][l], ln1_b=inp["ln1_b"][l], ln2_w=inp["ln2_w"][l], ln2_b=inp["ln2_b"][l], ln3_w=inp["ln3_w"][l], ln3_b=inp["ln3_b"][l],
                 router_w=inp["router_w"][l], router_b=inp["router_b"][l], moe_w_gate_up=inp["moe_w_gate_up"][l], moe_b_gate_up=inp["moe_b_gate_up"][l],
                 moe_w_down=inp["moe_w_down"][l], moe_b_down=inp["moe_b_down"][l], ple_w_gate=inp["ple_w_gate"][l], ple_w_proj=inp["ple_w_proj"][l])
        ins = []
        for c in range(8):
            b, q = divmod(c, 4)
            ts = slice(q * NT, (q + 1) * NT)
            ysT = np.concatenate([ym[b * 4 + j][:, :, ts] for j in range(4)], axis=1)
            d = dict(hT=np.ascontiguousarray(hT[b][:, ts]), ysT=np.ascontiguousarray(ysT), pT=np.ascontiguousarray(inp["p"][l, b, ts].T))
            d.update(W); d.update(rconst)
            ins.append(d)
        res = run_bass_kernel_spmd(_prog("r", NT), ins, core_ids=list(range(8)))
        hT = [np.concatenate([res.results[b * 4 + q]["oT"] for q in range(4)], axis=1) for b in range(Bn)]
    return np.ascontiguousarray(np.stack([h.T for h in hT], 0)).astype(np.float32)
```
